# Optimizing a Trainium2 kernel written in Bass

```python
import jax, jax.numpy as jnp
from jax import lax
import numpy as np

D_MODEL = 2048
BATCH = 8
SEQ = 4096
DEPTH = 2

CHUNK = 64
Q_BLOCK = 128
HEAD_DIM = 128
ROPE_THETA = 500000.0
PARTIAL_ROPE_DIM = HEAD_DIM // 4
NORM_EPS = 1e-6
D_MIX = D_MODEL
D_FF = 5632
N_MOD = 9

A_HEADS = D_MIX // (4 * HEAD_DIM)
IDX_HEADS = 16
IDX_DIM = 64
TOPK_MAX = 256
B_HEADS = D_MIX // (2 * HEAD_DIM)
MLA_Q_RANK = 448
MLA_KV_RANK = 128
MLA_NOPE = 128
MLA_ROPE = 64
MLA_V = HEAD_DIM
C_HEADS = D_MIX // (4 * HEAD_DIM)

SPLIT_SIZES = (
    A_HEADS * HEAD_DIM, A_HEADS * HEAD_DIM, A_HEADS * HEAD_DIM,
    IDX_HEADS * IDX_DIM, IDX_DIM, IDX_HEADS,
    MLA_Q_RANK, MLA_KV_RANK, MLA_ROPE,
    C_HEADS * HEAD_DIM, C_HEADS * HEAD_DIM, C_HEADS * HEAD_DIM,
)
N_IN = sum(SPLIT_SIZES)

kernel_name = "hybrid_dsa_mla_stickbreaking_macaron_block"


def rms_norm(x, g):
    xf = x.astype(jnp.float32)
    y = xf * lax.rsqrt(jnp.mean(xf * xf, axis=-1, keepdims=True) + NORM_EPS)
    return (y * g.astype(jnp.float32)).astype(x.dtype)


def rope_tables(seq, dim):
    inv = 1.0 / (ROPE_THETA ** (jnp.arange(0, dim, 2, dtype=jnp.float32) / dim))
    ang = jnp.arange(seq, dtype=jnp.float32)[:, None] * inv[None, :]
    return jnp.cos(ang), jnp.sin(ang)


def apply_rope(x, cos, sin):
    x1, x2 = jnp.split(x, 2, axis=-1)
    c = cos[None, :, None, :].astype(x.dtype)
    s = sin[None, :, None, :].astype(x.dtype)
    return jnp.concatenate([x1 * c - x2 * s, x1 * s + x2 * c], axis=-1)


def partial_rope(x, cos, sin):
    r = PARTIAL_ROPE_DIM
    return jnp.concatenate([apply_rope(x[..., :r], cos, sin), x[..., r:]], axis=-1)


def chunk_mask(start, seq):
    q_pos = start + jnp.arange(Q_BLOCK)
    k_pos = jnp.arange(seq)
    return (k_pos[None, :] // CHUNK) <= (q_pos[:, None] // CHUNK)


def sweep_query_blocks(fn, *qs):
    b, s = qs[0].shape[:2]
    nb = s // Q_BLOCK
    blk = tuple(jnp.moveaxis(q.reshape((b, nb, Q_BLOCK) + q.shape[2:]), 1, 0) for q in qs)
    starts = jnp.arange(nb, dtype=jnp.int32) * Q_BLOCK
    out = lax.map(lambda a: fn(*a[0], a[1]), (blk, starts))
    return jnp.moveaxis(out, 0, 1).reshape((b, s) + out.shape[3:])


def dsa_attention(q, k, v, q_idx, k_idx, w_idx, topk):
    seq = k.shape[1]
    scale = HEAD_DIM ** -0.5

    def block(qb, qib, wib, start):
        adm = chunk_mask(start, seq)
        logits = jnp.einsum('bqhd,bsd->bqhs', qib, k_idx) * (IDX_DIM ** -0.5)
        score = jnp.einsum('bqh,bqhs->bqs', wib * (IDX_HEADS ** -0.5), jax.nn.relu(logits))
        score = jnp.where(adm[None], score.astype(jnp.float32), -jnp.inf)
        top_val, top_idx = lax.top_k(score, topk)
        valid = jnp.isfinite(top_val)
        k_sel = jax.vmap(lambda kb, ib: kb[ib])(k, top_idx)
        v_sel = jax.vmap(lambda vb, ib: vb[ib])(v, top_idx)
        att = jnp.einsum('bqhd,bqkhd->bhqk', qb, k_sel).astype(jnp.float32) * scale
        att = jnp.where(valid[:, None], att, -jnp.inf)
        p = jax.nn.softmax(att, axis=-1).astype(v.dtype)
        return jnp.einsum('bhqk,bqkhd->bqhd', p, v_sel)

    return sweep_query_blocks(block, q, q_idx, w_idx)


def mla_attention(q_nope, q_rope, k_nope, k_rope, v):
    seq = k_nope.shape[1]
    scale = (MLA_NOPE + MLA_ROPE) ** -0.5

    def block(qn, qr, start):
        adm = chunk_mask(start, seq)
        sc = (jnp.einsum('bqhd,bshd->bhqs', qn, k_nope)
              + jnp.einsum('bqhr,bsr->bhqs', qr, k_rope)).astype(jnp.float32) * scale
        sc = jnp.where(adm[None, None], sc, -jnp.inf)
        p = jax.nn.softmax(sc, axis=-1).astype(v.dtype)
        return jnp.einsum('bhqs,bshd->bqhd', p, v)

    return sweep_query_blocks(block, q_nope, q_rope)


def stick_breaking_attention(q, k, v):
    seq = k.shape[1]
    scale = HEAD_DIM ** -0.5
    k_pos = jnp.arange(seq)

    def block(qb, start):
        q_pos = start + jnp.arange(Q_BLOCK)
        before = k_pos[None, :] < q_pos[:, None]
        z = jnp.einsum('bqhd,bshd->bhqs', qb, k).astype(jnp.float32) * scale
        log_stay = jnp.where(before, jax.nn.log_sigmoid(-z), 0.0)
        later = lax.cumsum(log_stay, axis=log_stay.ndim - 1, reverse=True) - log_stay
        w = jnp.where(before, jnp.exp(jax.nn.log_sigmoid(z) + later), 0.0)
        return jnp.einsum('bhqs,bshd->bqhd', w.astype(v.dtype), v)

    return sweep_query_blocks(block, q)


def swiglu(h, w_gate, w_up, w_down):
    return (jax.nn.silu(h @ w_gate) * (h @ w_up)) @ w_down


def modulate(h, shift, scale):
    return h * (1.0 + scale[:, None, :]) + shift[:, None, :]


def setup_inputs(seed: int = 0) -> dict:
    key = jax.random.key(seed)
    ks = jax.random.split(key, 32)
    L, D = DEPTH, D_MODEL

    def dense(k, shape, fan_in, gain=1.0):
        return jax.random.normal(k, shape, jnp.float32) * (gain * fan_in ** -0.5)

    def gain(k, shape):
        return 1.0 + 0.05 * jax.random.normal(k, shape, jnp.float32)

    return {
        "x": jax.random.normal(ks[0], (BATCH, SEQ, D), jnp.float32),
        "c": jax.random.normal(ks[1], (BATCH, D), jnp.float32),
        "w_ada": dense(ks[2], (L, D, N_MOD * D), D, 0.1),
        "b_ada": 0.01 * jax.random.normal(ks[3], (L, N_MOD * D), jnp.float32),
        "g_ffn1": gain(ks[4], (L, D)),
        "w1_gate": dense(ks[5], (L, D, D_FF), D),
        "w1_up": dense(ks[6], (L, D, D_FF), D),
        "w1_down": dense(ks[7], (L, D_FF, D), D_FF),
        "g_mix": gain(ks[8], (L, D)),
        "w_in": dense(ks[9], (L, D, N_IN), D),
        "g_qa": gain(ks[10], (L, HEAD_DIM)),
        "g_ka": gain(ks[11], (L, HEAD_DIM)),
        "g_cq": gain(ks[12], (L, MLA_Q_RANK)),
        "g_ckv": gain(ks[13], (L, MLA_KV_RANK)),
        "w_uq": dense(ks[14], (L, MLA_Q_RANK, B_HEADS * (MLA_NOPE + MLA_ROPE)), MLA_Q_RANK),
        "w_ukv": dense(ks[15], (L, MLA_KV_RANK, B_HEADS * (MLA_NOPE + MLA_V)), MLA_KV_RANK),
        "g_q_nope": gain(ks[16], (L, MLA_NOPE)),
        "g_k_nope": gain(ks[17], (L, MLA_NOPE)),
        "g_q_rope": gain(ks[18], (L, MLA_ROPE)),
        "g_k_rope": gain(ks[19], (L, MLA_ROPE)),
        "w_out": dense(ks[20], (L, D_MIX, D), D_MIX),
        "g_ffn2": gain(ks[21], (L, D)),
        "w2_gate": dense(ks[22], (L, D, D_FF), D),
        "w2_up": dense(ks[23], (L, D, D_FF), D),
        "w2_down": dense(ks[24], (L, D_FF, D), D_FF),
    }


def reference(x, c, w_ada, b_ada, g_ffn1, w1_gate, w1_up, w1_down, g_mix, w_in,
              g_qa, g_ka, g_cq, g_ckv, w_uq, w_ukv, g_q_nope, g_k_nope, g_q_rope, g_k_rope,
              w_out, g_ffn2, w2_gate, w2_up, w2_down):
    b, s, _ = x.shape
    topk = min(TOPK_MAX, s // 4)
    cos_p, sin_p = rope_tables(s, PARTIAL_ROPE_DIM)
    cos_m, sin_m = rope_tables(s, MLA_ROPE)
    split_at = [int(v) for v in np.cumsum(SPLIT_SIZES)[:-1]]
    c_act = jax.nn.silu(c)

    for l in range(DEPTH):
        mod = c_act @ w_ada[l] + b_ada[l]
        (sh1, sc1, ga1, shm, scm, gam, sh2, sc2, ga2) = jnp.split(mod, N_MOD, axis=-1)

        h = modulate(rms_norm(x, g_ffn1[l]), sh1, sc1)
        x = x + 0.5 * (1.0 + ga1[:, None, :]) * swiglu(h, w1_gate[l], w1_up[l], w1_down[l])

        h = modulate(rms_norm(x, g_mix[l]), shm, scm)
        (qa, ka, va, qi, ki, wi, cq, ckv, kr, qc, kc, vc) = jnp.split(h @ w_in[l], split_at, axis=-1)

        qa = partial_rope(rms_norm(qa.reshape(b, s, A_HEADS, HEAD_DIM), g_qa[l]), cos_p, sin_p)
        ka = partial_rope(rms_norm(ka.reshape(b, s, A_HEADS, HEAD_DIM), g_ka[l]), cos_p, sin_p)
        va = va.reshape(b, s, A_HEADS, HEAD_DIM)
        out_a = dsa_attention(qa, ka, va, qi.reshape(b, s, IDX_HEADS, IDX_DIM), ki, wi, topk)

        qb = (rms_norm(cq, g_cq[l]) @ w_uq[l]).reshape(b, s, B_HEADS, MLA_NOPE + MLA_ROPE)
        kvb = (rms_norm(ckv, g_ckv[l]) @ w_ukv[l]).reshape(b, s, B_HEADS, MLA_NOPE + MLA_V)
        q_nope = rms_norm(qb[..., :MLA_NOPE], g_q_nope[l])
        q_rope = apply_rope(rms_norm(qb[..., MLA_NOPE:], g_q_rope[l]), cos_m, sin_m)
        k_nope = rms_norm(kvb[..., :MLA_NOPE], g_k_nope[l])
        vb = kvb[..., MLA_NOPE:]
        k_rope = apply_rope(rms_norm(kr, g_k_rope[l])[:, :, None, :], cos_m, sin_m)[:, :, 0]
        out_b = mla_attention(q_nope, q_rope, k_nope, k_rope, vb)

        out_c = stick_breaking_attention(qc.reshape(b, s, C_HEADS, HEAD_DIM),
                                         kc.reshape(b, s, C_HEADS, HEAD_DIM),
                                         vc.reshape(b, s, C_HEADS, HEAD_DIM))

        mixed = jnp.concatenate([out_a.reshape(b, s, -1), out_b.reshape(b, s, -1),
                                 out_c.reshape(b, s, -1)], axis=-1) @ w_out[l]
        x = x + (1.0 + gam[:, None, :]) * mixed

        h = modulate(rms_norm(x, g_ffn2[l]), sh2, sc2)
        x = x + 0.5 * (1.0 + ga2[:, None, :]) * swiglu(h, w2_gate[l], w2_up[l], w2_down[l])

    return x
```

```python
import numpy as np
from contextlib import ExitStack
import concourse.bass as bass
import concourse.mybir as mybir
from concourse.bass_utils import run_bass_kernel_spmd

F32 = mybir.dt.float32
BF16 = mybir.dt.bfloat16
AF = mybir.ActivationFunctionType
ALU = mybir.AluOpType
AX = mybir.AxisListType

S = 4096
D = 2048
DFF = 5632
NL = 2
NT = S // 128
NG = S // 512
EPS = 1e-6
NIN = 4816
TM_COLS = 2704
FM_COLS = 2176
BIGNEG = -30000.0
GT = 1216
NBIS = 22

ENGS = ("pe", "act", "dve", "pool", "sp")
CHUNK = 12000
NPOOL = 40


class Op:
    __slots__ = ("eng", "fn", "deps", "sig", "seq", "dma", "dsem", "dval", "name")

    def __init__(self, eng, fn, dma=False, name=""):
        self.eng = eng
        self.fn = fn
        self.deps = []
        self.sig = False
        self.seq = 0
        self.dma = dma
        self.dsem = -1
        self.dval = 0
        self.name = name


class Prog:
    def __init__(self, nc):
        self.nc = nc
        self.q = {e: [] for e in ENGS}
        self.last_w = {}
        self.readers = {}
        self.dma_cnt = [0] * NPOOL
        self.dma_rr = 0
        self.live_dma = []
        self.last_real = {}

    def _add_dep(self, op, d, raw):
        if d is None or d is op:
            return
        if (not d.dma) and (not op.dma) and d.eng == op.eng and not raw:
            return
        if d in op.deps:
            return
        op.deps.append(d)
        d.sig = True

    def _track(self, op, reads, writes):
        for r in reads:
            self._add_dep(op, self.last_w.get(r), True)
        for w in writes:
            self._add_dep(op, self.last_w.get(w), False)
            for rd in self.readers.get(w, ()):
                self._add_dep(op, rd, False)
        for r in reads:
            lst = self.readers.setdefault(r, [])
            if not op.dma:
                lst[:] = [o for o in lst if o.dma or o.eng != op.eng]
            lst.append(op)
        for w in writes:
            self.last_w[w] = op
            self.readers[w] = []

    def op(self, eng, fn, reads=(), writes=(), name=""):
        o = Op(eng, fn, name=name)
        self._track(o, reads, writes)
        self.q[eng].append(o)
        self.last_real[eng] = o
        return o

    def dma(self, queue, out, in_, reads=(), writes=(), **kw):
        o = Op(queue, lambda e: e.dma_start(out=out, in_=in_, **kw), dma=True)
        i = self.dma_rr
        self.dma_rr = (self.dma_rr + 1) % NPOOL
        self.dma_cnt[i] += 1
        o.dsem = i
        o.dval = 16 * self.dma_cnt[i]
        self._track(o, reads, writes)
        self.q[queue].append(o)
        self.live_dma.append(o)
        return o

    def barrier(self):
        lasts = list(self.last_real.values())
        dmas = list(self.live_dma)
        self.live_dma = []
        for e in ENGS:
            b = Op(e, None, name="barrier")
            for d in lasts:
                if d.eng != e:
                    b.deps.append(d)
                    d.sig = True
            for d in dmas:
                b.deps.append(d)
            self.q[e].append(b)
        self.last_w = {}
        self.readers = {}

    def emit(self, es):
        nc = self.nc
        nsig = {}
        for e in ENGS:
            n = 0
            for o in self.q[e]:
                if o.sig and not o.dma:
                    n += 1
                    o.seq = n
            nsig[e] = n
        sems = {}
        for e in ENGS:
            k = (nsig[e] + CHUNK - 1) // CHUNK
            sems[e] = [es.enter_context(nc.semaphore(f"s_{e}{i}")) for i in range(k)]
        dsems = [es.enter_context(nc.semaphore(f"s_dma{i}")) for i in range(NPOOL)]
        self.nsig = nsig

        def run(ename):
            def body(eng):
                seen_eng = {}
                seen_dma = {}
                for o in self.q[ename]:
                    for d in o.deps:
                        if d.dma:
                            if seen_dma.get(d.dsem, 0) >= d.dval:
                                continue
                            eng.wait_ge(dsems[d.dsem], d.dval)
                            seen_dma[d.dsem] = d.dval
                        else:
                            if seen_eng.get(d.eng, 0) >= d.seq:
                                continue
                            c = (d.seq - 1) // CHUNK
                            eng.wait_ge(sems[d.eng][c], d.seq - c * CHUNK)
                            seen_eng[d.eng] = d.seq
                    if o.fn is None:
                        continue
                    if o.dma:
                        prev = o.dval - 16
                        if prev > 0 and seen_dma.get(o.dsem, 0) < prev:
                            eng.wait_ge(dsems[o.dsem], prev)
                            seen_dma[o.dsem] = prev
                        ins = o.fn(eng)
                        ins.then_inc(dsems[o.dsem], 16)
                    else:
                        ins = o.fn(eng)
                        if o.sig:
                            c = (o.seq - 1) // CHUNK
                            ins.then_inc(sems[ename][c], 1)
            return body

        with nc.Block() as block:
            block.tensor(run("pe"))
            block.scalar(run("act"))
            block.vector(run("dve"))
            block.gpsimd(run("pool"))
            block.sync(run("sp"))


class Alloc:
    def __init__(self, big, nbytes):
        self.big = big
        self.nbytes = nbytes
        self.off = 0

    def take(self, cols, dtype=F32, parts=128):
        esz = 4 if dtype == F32 else 2
        nb = (cols * esz + 63) // 64 * 64
        assert self.off + nb <= self.nbytes, (self.off, nb, self.nbytes)
        v = self.big[0:parts, self.off // 4:(self.off + nb) // 4]
        self.off += nb
        if dtype != F32:
            v = v.bitcast(dtype)
        return v[:, 0:cols]


class Builder:
    def __init__(self, nc, cfg):
        self.nc = nc
        self.cfg = cfg
        self.uid = 0

    def dram_in(self, name, shape, dt=F32):
        return self.nc.dram_tensor(name, list(shape), dt, kind="ExternalInput").ap()

    def dram_scr(self, name, shape, dt):
        kind = "ExternalOutput" if name in self.cfg.get("dbg", ()) else "Internal"
        return self.nc.dram_tensor(name, list(shape), dt, kind=kind).ap()

    def declare(self):
        nl = self.cfg["nl"]
        d = {}
        d["x"] = self.dram_in("x", [S, D])
        d["cT"] = self.dram_in("cT", [128, 16])
        d["wada"] = self.dram_in("wada", [NL * 36, 128, 16 * 512])
        d["bada"] = self.dram_in("bada", [NL, 9 * D])
        d["gcol"] = self.dram_in("gcol", [128, NL * 3 * 16])
        d["wg"] = self.dram_in("wg", [NL * 2 * 22, 128, 16 * 256])
        d["wu"] = self.dram_in("wu", [NL * 2 * 22, 128, 16 * 256])
        d["wd"] = self.dram_in("wd", [NL * 2 * 8, 128, 22 * 512])
        d["ident"] = self.dram_in("ident", [128, 128])
        d["wtm"] = self.dram_in("wtm", [NL, 128, 16 * TM_COLS])
        d["wfm"] = self.dram_in("wfm", [NL, 128, 16 * FM_COLS])
        d["wuq"] = self.dram_in("wuq", [NL, 128, 4 * 1536])
        d["wukv"] = self.dram_in("wukv", [NL, 128, 2048])
        d["wout"] = self.dram_in("wout", [NL, 128, 16 * D])
        d["gtm"] = self.dram_in("gtm", [128, NL * GT])
        d["ropeA"] = self.dram_in("ropeA", [S, 128])
        d["ropeQ"] = self.dram_in("ropeQ", [S, 512])
        d["ropeK"] = self.dram_in("ropeK", [S, 64])
        d["cmaskB"] = self.dram_in("cmaskB", [128, 4 * 512])
        d["tri"] = self.dram_in("tri", [128, 4 * 512])
        d["ustrict"] = self.dram_in("ustrict", [128, 128])
        d["pow2"] = self.dram_in("pow2", [128, NBIS + 1])
        d["y"] = self.nc.dram_tensor("y", [S, D], F32, kind="ExternalOutput").ap()
        d["modv"] = self.dram_scr("modv", [NL, 9 * D], F32)
        d["wg_b"] = self.dram_scr("wg_b", [NL * 2 * 22, 128, 16 * 256], BF16)
        d["wu_b"] = self.dram_scr("wu_b", [NL * 2 * 22, 128, 16 * 256], BF16)
        d["wd_b"] = self.dram_scr("wd_b", [NL * 2 * 8, 128, 22 * 512], BF16)
        d["wtm_b"] = self.dram_scr("wtm_b", [NL, 128, 16 * TM_COLS], BF16)
        d["wfm_b"] = self.dram_scr("wfm_b", [NL, 128, 16 * FM_COLS], BF16)
        d["wuq_b"] = self.dram_scr("wuq_b", [NL, 128, 4 * 1536], BF16)
        d["wukv_b"] = self.dram_scr("wukv_b", [NL, 128, 2048], BF16)
        d["wout_b"] = self.dram_scr("wout_b", [NL, 128, 16 * D], BF16)
        for nm, shp in (("qaT", [4, 128, S]), ("kaT", [4, 128, S]), ("qiT", [8, 128, S]), ("kiT", [1, 128, S]),
                        ("qnT", [8, 128, S]), ("qrT", [8, 64, S]), ("knT", [8, 128, S]), ("krT", [1, 64, S]),
                        ("qcT", [4, 128, S]), ("kcT", [4, 128, S]), ("mixT", [16, 128, S]),
                        ("va", [S, 4 * 129]), ("vb", [S, 8 * 129]), ("vc", [S, 512])):
            d[nm] = self.dram_scr(nm + "_d", shp, BF16)
        d["wi"] = self.dram_scr("wi_d", [S, 16], F32)
        self.d = d

    def cast_tiles(self, P, src, dst, tiles, cols, bufs, step=4096):
        i = self.uid
        for t in tiles:
            for c0 in range(0, cols, step):
                c1 = min(cols, c0 + step)
                fb, bb = bufs[i % len(bufs)]
                kf = ("castf", i % len(bufs))
                kb = ("castb", i % len(bufs))
                P.dma("sp", fb[:, 0:c1 - c0], src[t, :, c0:c1], writes=[kf])
                o_, i_ = bb[:, 0:c1 - c0], fb[:, 0:c1 - c0]
                if i % 2 == 0:
                    P.op("dve", lambda e, o_=o_, i_=i_: e.tensor_copy(out=o_, in_=i_), reads=[kf], writes=[kb])
                else:
                    P.op("act", lambda e, o_=o_, i_=i_: e.copy(out=o_, in_=i_), reads=[kf], writes=[kb])
                P.dma("pool", dst[t, :, c0:c1], bb[:, 0:c1 - c0], reads=[kb], writes=[])
                i += 1
        self.uid = i

    def phase_prep(self, P, A):
        d = self.d
        nl = self.cfg["nl"]
        A.off = self.base_off
        bufs = [(A.take(4096, F32), A.take(4096, BF16)) for _ in range(4)]
        for l in range(nl):
            for f in range(2):
                if not self.cfg["ffn"][f]:
                    continue
                t0 = (l * 2 + f) * 22
                self.cast_tiles(P, d["wg"], d["wg_b"], range(t0, t0 + 22), 16 * 256, bufs)
                self.cast_tiles(P, d["wu"], d["wu_b"], range(t0, t0 + 22), 16 * 256, bufs)
                t0 = (l * 2 + f) * 8
                self.cast_tiles(P, d["wd"], d["wd_b"], range(t0, t0 + 8), 22 * 512, bufs, step=2816)
        if self.cfg.get("mix", False):
            for l in range(nl):
                for nm, cols in (("wtm", 16 * TM_COLS), ("wfm", 16 * FM_COLS), ("wuq", 4 * 1536), ("wukv", 2048), ("wout", 16 * D)):
                    self.cast_tiles(P, d[nm], d[nm + "_b"], [l], cols, bufs)
        P.barrier()

    def phase_adaln(self, P, A):
        d = self.d
        nl = self.cfg["nl"]
        ps = self.ps
        A.off = self.base_off
        cact = A.take(16)
        wt = [A.take(16 * 512) for _ in range(2)]
        bt = [A.take(512, parts=1) for _ in range(2)]
        rt = [A.take(512, parts=1) for _ in range(2)]
        P.dma("sp", cact, d["cT"], writes=["cact"])
        P.op("act", lambda e: e.activation(out=cact, in_=cact, func=AF.Silu), reads=["cact"], writes=["cact"])
        for l in range(nl):
            for j in range(36):
                i = l * 36 + j
                w = wt[i % 2]
                w3 = w.rearrange("p (k c) -> p k c", k=16)
                P.dma("sp", w, d["wada"][i], writes=[("wt", i % 2)])
                P.dma("pool", bt[i % 2], d["bada"][l:l + 1, j * 512:(j + 1) * 512], writes=[("bt", i % 2)])
                pso = ps[0:1, (i % 2) * 512:(i % 2) * 512 + 512]
                for k in range(16):
                    P.op("pe", lambda e, k=k, pso=pso, w3=w3: e.matmul(pso, lhsT=cact[:, k:k + 1], rhs=w3[:, k, :],
                                                                         start=(k == 0), stop=(k == 15)),
                         reads=["cact", ("wt", i % 2)], writes=[("psb", i % 2)])
                r = rt[i % 2]
                b = bt[i % 2]
                P.op("dve", lambda e, r=r, pso=pso, b=b: e.tensor_tensor(out=r, in0=pso, in1=b, op=ALU.add),
                     reads=[("psb", i % 2), ("bt", i % 2)], writes=[("rt", i % 2), ("psb", i % 2)])
                P.dma("pool", d["modv"][l:l + 1, j * 512:(j + 1) * 512], r, reads=[("rt", i % 2)], writes=["modv"])
        P.barrier()

    def sublayer_setup(self, P, A, l, sub, gate_scale, want_cols=True):
        d = self.d
        shc = A.take(16)
        scc = A.take(16)
        if gate_scale is None:
            gb = None
        else:
            gb = A.take(D)
        mv = d["modv"]
        P.dma("sp", shc, mv[l, (3 * sub) * D:(3 * sub + 1) * D].rearrange("(k p) -> p k", p=128),
              writes=["shc"], allow_slow_non_contiguous=True)
        P.dma("sp", scc, mv[l, (3 * sub + 1) * D:(3 * sub + 2) * D].rearrange("(k p) -> p k", p=128),
              writes=["scc"], allow_slow_non_contiguous=True)
        if gb is not None:
            P.dma("sp", gb, mv[l, (3 * sub + 2) * D:(3 * sub + 3) * D].partition_broadcast(128), writes=["gb"])
        gc = self.gcol[:, (l * 3 + sub) * 16:(l * 3 + sub + 1) * 16]
        P.op("dve", lambda e: e.scalar_tensor_tensor(out=scc, in0=scc, scalar=1.0, in1=gc, op0=ALU.add, op1=ALU.mult),
             reads=["scc", "gcol"], writes=["scc"])
        if gb is not None:
            P.op("dve", lambda e: e.tensor_scalar(out=gb, in0=gb, scalar1=1.0, scalar2=gate_scale, op0=ALU.add, op1=ALU.mult),
                 reads=["gb"], writes=["gb"])
        return shc, scc, gb

    def norm_loads(self, P, src, g, xb):
        for s in range(4):
            t = g * 4 + s
            P.dma("pool", xb[s % len(xb)], src[t * 128:(t + 1) * 128, :], reads=[("xr", t, n) for n in range(4)], writes=[("xb", s % len(xb))])

    def norm_group(self, P, src, g, hT, shc, gsc, xb, junk, st, inline_load=False, junk_key="junk"):
        ps = self.ps
        for s in range(4):
            t = g * 4 + s
            xt = xb[s % len(xb)]
            kx = ("xb", s % len(xb))
            if inline_load:
                P.dma("pool", xt, src[t * 128:(t + 1) * 128, :], reads=[("xr", t, n) for n in range(4)], writes=[kx])
            ss = st[t % 2]
            kss = ("st", t % 2)
            P.op("act", lambda e, xt=xt, ss=ss: e.activation(out=junk, in_=xt, func=AF.Square, accum_out=ss[:, 0:1]),
                 reads=[kx], writes=[junk_key, kss])
            P.op("dve", lambda e, ss=ss: e.tensor_scalar(out=ss[:, 1:2], in0=ss[:, 0:1], scalar1=1.0 / D, scalar2=EPS,
                                                         op0=ALU.mult, op1=ALU.add), reads=[kss], writes=[kss])
            P.op("act", lambda e, ss=ss: e.activation(out=ss[:, 2:3], in_=ss[:, 1:2], func=AF.Sqrt), reads=[kss], writes=[kss])
            P.op("dve", lambda e, ss=ss: e.reciprocal(out=ss[:, 3:4], in_=ss[:, 2:3]), reads=[kss], writes=[kss])
            P.op("dve", lambda e, xt=xt, ss=ss: e.tensor_scalar(out=xt, in0=xt, scalar1=ss[:, 3:4], scalar2=None, op0=ALU.mult),
                 reads=[kx, kss], writes=[kx])
            for kg in range(4):
                bank = kg % 2
                kp = ("psb", bank)
                for q in range(4):
                    k = kg * 4 + q
                    pt = ps[:, bank * 512 + q * 128: bank * 512 + (q + 1) * 128]
                    P.op("pe", lambda e, pt=pt, xt=xt, k=k: e.transpose(out=pt, in_=xt[:, k * 128:(k + 1) * 128], identity=self.ident),
                         reads=[kx, "ident"], writes=[kp])
                for q in range(4):
                    k = kg * 4 + q
                    pt = ps[:, bank * 512 + q * 128: bank * 512 + (q + 1) * 128]
                    P.op("act", lambda e, pt=pt, k=k, s=s: e.activation(out=hT[:, k, s * 128:(s + 1) * 128], in_=pt, func=AF.Identity,
                                                                         scale=gsc[:, k:k + 1], bias=shc[:, k:k + 1]),
                         reads=[kp, "scc", "shc"], writes=[("hT", s), kp])


    def headnorm(self, P, x3, H, dh, gain, tmp3, stat, kx):
        kt, ks = ("hn_tmp", kx), ("hn_stat", kx)
        P.op("dve", lambda e: e.tensor_tensor(out=tmp3, in0=x3, in1=x3, op=ALU.mult), reads=[kx], writes=[kt])
        yield
        P.op("dve", lambda e: e.tensor_reduce(out=stat[:, 0:H], in_=tmp3, axis=AX.X, op=ALU.add), reads=[kt], writes=[ks])
        yield
        P.op("dve", lambda e: e.tensor_scalar(out=stat[:, H:2 * H], in0=stat[:, 0:H], scalar1=1.0 / dh, scalar2=EPS,
                                              op0=ALU.mult, op1=ALU.add), reads=[ks], writes=[ks])
        yield
        P.op("act", lambda e: e.activation(out=stat[:, H:2 * H], in_=stat[:, H:2 * H], func=AF.Sqrt), reads=[ks], writes=[ks])
        yield
        P.op("dve", lambda e: e.reciprocal(out=stat[:, 2 * H:3 * H], in_=stat[:, H:2 * H]), reads=[ks], writes=[ks])
        yield
        P.op("dve", lambda e: e.tensor_tensor(out=x3, in0=x3, in1=stat[:, 2 * H:3 * H].unsqueeze(2).to_broadcast([128, H, dh]),
                                              op=ALU.mult), reads=[kx, ks], writes=[kx])
        yield
        P.op("dve", lambda e: e.tensor_tensor(out=x3, in0=x3, in1=gain.unsqueeze(1).to_broadcast([128, H, dh]), op=ALU.mult),
             reads=[kx, "gt"], writes=[kx])
        yield

    def rope(self, P, x1, x2, cos3, sin3, rt, kx, krope):
        H, r2 = x1.shape[1], x1.shape[2]
        t = [r[:, 0:H * r2].rearrange("p (h r) -> p h r", h=H) for r in rt]
        kr = ("rope_tmp", kx)
        P.op("dve", lambda e: e.tensor_tensor(out=t[0], in0=x1, in1=cos3, op=ALU.mult), reads=[kx, krope], writes=[kr])
        P.op("dve", lambda e: e.tensor_tensor(out=t[1], in0=x2, in1=sin3, op=ALU.mult), reads=[kx, krope], writes=[kr])
        yield
        P.op("dve", lambda e: e.tensor_tensor(out=t[2], in0=x1, in1=sin3, op=ALU.mult), reads=[kx, krope], writes=[kr])
        P.op("dve", lambda e: e.tensor_tensor(out=t[3], in0=x2, in1=cos3, op=ALU.mult), reads=[kx, krope], writes=[kr])
        yield
        P.op("dve", lambda e: e.tensor_tensor(out=x1, in0=t[0], in1=t[1], op=ALU.subtract), reads=[kr], writes=[kx])
        P.op("dve", lambda e: e.tensor_tensor(out=x2, in0=t[2], in1=t[3], op=ALU.add), reads=[kr], writes=[kx])
        yield

    def phase_mixproj(self, P, A, l, src):
        d = self.d
        ps = self.ps
        psb = self.ps.bitcast(BF16)
        A.off = self.base_off
        shc, gsc, _ = self.sublayer_setup(P, A, l, 1, None)
        xb = [A.take(D) for _ in range(2)]
        st = [A.take(4) for _ in range(2)]
        hT = A.take(16 * 512, BF16).rearrange("p (k t) -> p k t", k=16)
        wblk = [A.take(16 * 512, BF16) for _ in range(2)]
        wuq = A.take(4 * 1536, BF16).rearrange("p (j c) -> p j c", j=4)
        wukv = A.take(2048, BF16)
        gt = A.take(GT)
        identb = A.take(128, BF16)
        rAg = A.take(4 * 128).rearrange("p (s c) -> p s c", s=4)
        rQg = A.take(4 * 512).rearrange("p (s c) -> p s c", s=4)
        rKg = A.take(4 * 64).rearrange("p (s c) -> p s c", s=4)
        xsL = [A.take(1536) for _ in range(4)]
        tmpL = [A.take(1024) for _ in range(4)]
        statL = [A.take(32) for _ in range(4)]
        xbfL = [A.take(1536, BF16) for _ in range(4)]
        cqTL = [A.take(4 * 128, BF16).rearrange("p (j t) -> p j t", j=4) for _ in range(4)]
        ckvTL = [A.take(128, BF16) for _ in range(4)]
        qaS = A.take(4 * 512, BF16).rearrange("p (h t) -> p h t", h=4)
        kaS = A.take(4 * 512, BF16).rearrange("p (h t) -> p h t", h=4)
        qnS_flat = A.take(8 * 512, BF16)
        qnS = qnS_flat.rearrange("p (h t) -> p h t", h=8)
        junk = qnS_flat[:, 0:D]
        qrS = A.take(8 * 512, BF16).rearrange("p (h t) -> p h t", h=8)
        knS = A.take(8 * 512, BF16).rearrange("p (h t) -> p h t", h=8)
        krS = A.take(512, BF16)
        fmS = [A.take(512, BF16) for _ in range(2)]
        vaS = [A.take(4 * 129, BF16).rearrange("p (h c) -> p h c", h=4) for _ in range(4)]
        vbS = [A.take(8 * 129, BF16).rearrange("p (h c) -> p h c", h=8) for _ in range(4)]
        vcS = [A.take(512, BF16) for _ in range(4)]
        wiS = [A.take(16) for _ in range(4)]

        P.dma("sp", wuq, d["wuq_b"][l].rearrange("p (j c) -> p j c", j=4), writes=["wuq"])
        P.dma("sp", wukv, d["wukv_b"][l], writes=["wukv"])
        P.dma("sp", gt, d["gtm"][:, l * GT:(l + 1) * GT], writes=["gt"])
        P.op("dve", lambda e: e.tensor_copy(out=identb, in_=self.ident), reads=["ident"], writes=["identb"])
        for b in range(4):
            P.op("dve", lambda e, b=b: e.memset(vaS[b][:, :, 128:129], 1.0), writes=[("vaS", b)])
            P.op("dve", lambda e, b=b: e.memset(vbS[b][:, :, 128:129], 1.0), writes=[("vbS", b)])
        g_qa, g_ka = gt[:, 0:128], gt[:, 128:256]
        g_cq, g_ckv = gt[:, 256:704], gt[:, 704:832]
        g_qn, g_kn = gt[:, 832:960], gt[:, 960:1088]
        g_qr, g_kr = gt[:, 1088:1152], gt[:, 1152:1216]
        hTk = [("hT", s) for s in range(4)]
        tm_blocks = [(0, 512), (512, 1024), (1024, 1536), (1536, 2048), (2048, 2560), (2560, 2704)]
        wtm_v = d["wtm_b"][l].rearrange("p (k c) -> p k c", k=16)
        wfm_v = d["wfm_b"][l].rearrange("p (k c) -> p k c", k=16)
        fm_tiles = [(0, 512), (512, 1024), (1024, 1536), (1536, 2048), (2048, 2176)]
        fm_dest = ([("qiT", i) for i in range(8)] + [("kiT", 0)] + [("qcT", i) for i in range(4)] + [("kcT", i) for i in range(4)])
        cnt = {"w": 0, "pb": 0, "fm": 0}

        ng = self.cfg.get("ng", NG)
        for g in range(ng):
            self.norm_group(P, src, g, hT, shc, gsc, xb, junk, st, inline_load=True, junk_key="qnS")
            rows = slice(g * 512, (g + 1) * 512)
            P.dma("pool", rAg, d["ropeA"][rows, :].rearrange("(s p) c -> p s c", p=128), writes=["ropeA"])
            P.dma("pool", rQg, d["ropeQ"][rows, :].rearrange("(s p) c -> p s c", p=128), writes=["ropeQ"])
            P.dma("pool", rKg, d["ropeK"][rows, :].rearrange("(s p) c -> p s c", p=128), writes=["ropeK"])

            def tm_chain(bi, s, w3, wb, nc_):
                t = g * 4 + s
                trows = slice(t * 128, (t + 1) * 128)
                tcol = slice(s * 128, (s + 1) * 128)
                xs, tmp, stat, xbf, cqT, ckvT = xsL[s], tmpL[s], statL[s], xbfL[s], cqTL[s], ckvTL[s]
                rt = [tmp[:, 256 * q:256 * (q + 1)] for q in range(4)]
                kxs, kxbf = ("xs", s), ("xbf", s)
                pb = s
                tb = 7

                def evac_xs(pbank, ncols, off=0):
                    P.op("act", lambda e: e.copy(out=xs[:, off:off + ncols], in_=ps[:, pbank * 512:pbank * 512 + ncols]),
                         reads=[("psb", pbank)], writes=[kxs, ("psb", pbank)])

                def tr(items):
                    for in_ap, off, w in items:
                        P.op("pe", lambda e, in_ap=in_ap, off=off, w=w: e.transpose(out=psb[0:w, tb * 1024 + off:tb * 1024 + off + 128], in_=in_ap,
                                                                                    identity=identb),
                             reads=[kxbf, "identb"], writes=[("psb", tb)])

                def evac7(out_ap, parts, off, ncols, wkey, view=None):
                    i_ = psb[0:parts, tb * 1024 + off:tb * 1024 + off + ncols]
                    if view is not None:
                        i_ = i_.rearrange("p (h t) -> p h t", h=view)
                    P.op("act", lambda e: e.copy(out=out_ap, in_=i_), reads=[("psb", tb)], writes=[wkey, ("psb", tb)])

                for k in range(16):
                    P.op("pe", lambda e, k=k: e.matmul(ps[:, pb * 512:pb * 512 + nc_], lhsT=hT[:, k, tcol], rhs=w3[:, k, :],
                                                       start=(k == 0), stop=(k == 15)),
                         reads=[("wblk", wb), ("hT", s)], writes=[("psb", pb)])
                yield
                if bi in (0, 1):
                    evac_xs(pb, 512)
                    yield
                    x3 = xs[:, 0:512].rearrange("p (h c) -> p h c", h=4)
                    yield from self.headnorm(P, x3, 4, 128, g_qa if bi == 0 else g_ka, tmp[:, 0:512].rearrange("p (h c) -> p h c", h=4), stat, kxs)
                    cos3 = rAg[:, s, 0:64].rearrange("p (h r) -> p h r", h=4)
                    sin3 = rAg[:, s, 64:128].rearrange("p (h r) -> p h r", h=4)
                    yield from self.rope(P, x3[:, :, 0:16], x3[:, :, 16:32], cos3, sin3, rt, kxs, "ropeA")
                    P.op("dve", lambda e: e.tensor_copy(out=xbf[:, 0:512], in_=xs[:, 0:512]), reads=[kxs], writes=[kxbf])
                    yield
                    tr([(xbf[:, h * 128:(h + 1) * 128], h * 128, 128) for h in range(4)])
                    evac7((qaS if bi == 0 else kaS)[:, :, tcol], 128, 0, 512, "qaS" if bi == 0 else "kaS", view=4)
                    yield
                elif bi == 2:
                    P.op("act", lambda e: e.copy(out=vaS[s][:, :, 0:128], in_=ps[:, pb * 512:(pb + 1) * 512].rearrange("p (h c) -> p h c", h=4)),
                         reads=[("psb", pb)], writes=[("vaS", s), ("psb", pb)])
                    P.dma("pool", d["va"][trows, :].rearrange("p (h c) -> p h c", h=4), vaS[s], reads=[("vaS", s)])
                    yield
                elif bi == 3:
                    P.op("act", lambda e: e.copy(out=vcS[s], in_=ps[:, pb * 512:(pb + 1) * 512]),
                         reads=[("psb", pb)], writes=[("vcS", s), ("psb", pb)])
                    P.dma("pool", d["vc"][trows, :], vcS[s], reads=[("vcS", s)])
                    yield
                elif bi == 4:
                    evac_xs(pb, 512)
                    yield
                    yield from self.headnorm(P, xs[:, 0:448].unsqueeze(1), 1, 448, g_cq, tmp[:, 0:448].unsqueeze(1), stat, kxs)
                    xk = xs[:, 448:512].unsqueeze(1)
                    yield from self.headnorm(P, xk, 1, 64, g_kr, tmp[:, 448:512].unsqueeze(1), stat, kxs)
                    yield from self.rope(P, xk[:, :, 0:32], xk[:, :, 32:64], rKg[:, s, 0:32].unsqueeze(1), rKg[:, s, 32:64].unsqueeze(1),
                                         rt, kxs, "ropeK")
                    P.op("dve", lambda e: e.tensor_copy(out=xbf[:, 0:512], in_=xs[:, 0:512]), reads=[kxs], writes=[kxbf])
                    yield
                    tr([(xbf[:, 0:128], 0, 128), (xbf[:, 128:256], 128, 128), (xbf[:, 256:384], 256, 128),
                        (xbf[:, 384:448], 384, 64), (xbf[:, 448:512], 512, 64)])
                    kcq = ("cqT", s)
                    evac7(cqT[:, 0:3, :], 128, 0, 384, kcq, view=3)
                    evac7(cqT[0:64, 3, :], 64, 384, 128, kcq)
                    evac7(krS[0:64, tcol], 64, 512, 128, "krS")
                    yield
                    for nb in range(3):
                        for j in range(4):
                            kk = 128 if j < 3 else 64
                            P.op("pe", lambda e, nb=nb, j=j, kk=kk: e.matmul(
                                ps[:, (4 + nb) * 512:(5 + nb) * 512], lhsT=cqT[0:kk, j, :], rhs=wuq[0:kk, j, nb * 512:(nb + 1) * 512],
                                start=(j == 0), stop=(j == 3)), reads=[kcq, "wuq"], writes=[("psb", 4 + nb)])
                        evac_xs(4 + nb, 512, off=nb * 512)
                        yield
                    q3 = xs[:, 0:1536].rearrange("p (h c) -> p h c", h=8)
                    yield from self.headnorm(P, q3[:, :, 0:128], 8, 128, g_qn, tmp[:, 0:1024].rearrange("p (h c) -> p h c", h=8), stat, kxs)
                    yield from self.headnorm(P, q3[:, :, 128:192], 8, 64, g_qr, tmp[:, 0:512].rearrange("p (h c) -> p h c", h=8), stat, kxs)
                    cosq = rQg[:, s, 0:256].rearrange("p (h r) -> p h r", h=8)
                    sinq = rQg[:, s, 256:512].rearrange("p (h r) -> p h r", h=8)
                    yield from self.rope(P, q3[:, :, 128:160], q3[:, :, 160:192], cosq, sinq, rt, kxs, "ropeQ")
                    P.op("dve", lambda e: e.tensor_copy(out=xbf[:, 0:1536], in_=xs[:, 0:1536]), reads=[kxs], writes=[kxbf])
                    yield
                    tr([(xbf[:, h * 192:h * 192 + 128], h * 128, 128) for h in range(8)])
                    evac7(qnS[:, :, tcol], 128, 0, 1024, "qnS", view=8)
                    yield
                    tr([(xbf[:, h * 192 + 128:h * 192 + 192], h * 128, 64) for h in range(8)])
                    evac7(qrS[0:64, :, tcol], 64, 0, 1024, "qrS", view=8)
                    yield
                else:
                    evac_xs(pb, 144)
                    yield
                    P.op("dve", lambda e: e.tensor_scalar(out=wiS[s], in0=xs[:, 128:144], scalar1=1.0 / 32.0, scalar2=None, op0=ALU.mult),
                         reads=[kxs], writes=[("wiS", s)])
                    P.dma("pool", d["wi"][trows, :], wiS[s], reads=[("wiS", s)])
                    yield
                    yield from self.headnorm(P, xs[:, 0:128].unsqueeze(1), 1, 128, g_ckv, tmp[:, 0:128].unsqueeze(1), stat, kxs)
                    P.op("dve", lambda e: e.tensor_copy(out=xbf[:, 0:128], in_=xs[:, 0:128]), reads=[kxs], writes=[kxbf])
                    yield
                    tr([(xbf[:, 0:128], 0, 128)])
                    kckv = ("ckvT", s)
                    evac7(ckvT, 128, 0, 128, kckv)
                    yield
                    for hh in range(2):
                        for nb in range(2):
                            P.op("pe", lambda e, hh=hh, nb=nb: e.matmul(
                                ps[:, (4 + nb) * 512:(5 + nb) * 512], lhsT=ckvT, rhs=wukv[:, hh * 1024 + nb * 512:hh * 1024 + (nb + 1) * 512],
                                start=True, stop=True), reads=[kckv, "wukv"], writes=[("psb", 4 + nb)])
                            evac_xs(4 + nb, 512, off=nb * 512)
                        yield
                        kv3 = xs[:, 0:1024].rearrange("p (h c) -> p h c", h=4)
                        P.op("dve", lambda e, hh=hh: e.tensor_copy(out=vbS[s][:, hh * 4:(hh + 1) * 4, 0:128], in_=kv3[:, :, 128:256]),
                             reads=[kxs], writes=[("vbS", s)])
                        yield
                        yield from self.headnorm(P, kv3[:, :, 0:128], 4, 128, g_kn, tmp[:, 0:512].rearrange("p (h c) -> p h c", h=4), stat, kxs)
                        P.op("dve", lambda e: e.tensor_copy(out=xbf[:, 0:512].rearrange("p (h c) -> p h c", h=4), in_=kv3[:, :, 0:128]),
                             reads=[kxs], writes=[kxbf])
                        yield
                        tr([(xbf[:, h * 128:(h + 1) * 128], h * 128, 128) for h in range(4)])
                        evac7(knS[:, hh * 4:(hh + 1) * 4, tcol], 128, 0, 512, "knS", view=4)
                        yield
                    P.dma("pool", d["vb"][trows, :].rearrange("p (h c) -> p h c", h=8), vbS[s], reads=[("vbS", s)])
                    yield

            for bi, (c0, c1) in enumerate(tm_blocks):
                nc_ = c1 - c0
                wb = cnt["w"] % 2
                cnt["w"] += 1
                w3 = wblk[wb][:, 0:16 * nc_].rearrange("p (k c) -> p k c", k=16)
                P.dma("sp", w3, wtm_v[:, :, c0:c1], writes=[("wblk", wb)])
                gens = [tm_chain(bi, s, w3, wb, nc_) for s in range(4)]
                while gens:
                    for g_ in list(gens):
                        try:
                            next(g_)
                        except StopIteration:
                            gens.remove(g_)
            ci = 0
            for (c0, c1) in fm_tiles:
                nc_ = c1 - c0
                wb = cnt["w"] % 2
                cnt["w"] += 1
                w3 = wblk[wb][:, 0:16 * nc_].rearrange("p (k c) -> p k c", k=16)
                P.dma("sp", w3, wfm_v[:, :, c0:c1], writes=[("wblk", wb)])
                for cc in range(nc_ // 128):
                    pb = cnt["pb"] % 4
                    cnt["pb"] += 1
                    for k in range(16):
                        P.op("pe", lambda e, pb=pb, w3=w3, k=k, cc=cc: e.matmul(
                            ps[:, pb * 512:(pb + 1) * 512], lhsT=w3[:, k, cc * 128:(cc + 1) * 128], rhs=hT[:, k, :],
                            start=(k == 0), stop=(k == 15)), reads=[("wblk", wb)] + hTk, writes=[("psb", pb)])
                    fb = cnt["fm"] % 2
                    cnt["fm"] += 1
                    P.op("act", lambda e, pb=pb, fb=fb: e.copy(out=fmS[fb], in_=ps[:, pb * 512:(pb + 1) * 512]),
                         reads=[("psb", pb)], writes=[("fmS", fb), ("psb", pb)])
                    nm, idx = fm_dest[ci]
                    P.dma("pool", d[nm][idx, :, rows], fmS[fb], reads=[("fmS", fb)])
                    ci += 1
            P.dma("pool", d["qaT"][:, :, rows].rearrange("h p t -> p h t"), qaS, reads=["qaS"])
            P.dma("pool", d["kaT"][:, :, rows].rearrange("h p t -> p h t"), kaS, reads=["kaS"])
            P.dma("pool", d["qnT"][:, :, rows].rearrange("h p t -> p h t"), qnS, reads=["qnS"])
            P.dma("pool", d["knT"][:, :, rows].rearrange("h p t -> p h t"), knS, reads=["knS"])
            P.dma("pool", d["qrT"][:, :, rows].rearrange("h p t -> p h t"), qrS[0:64], reads=["qrS"])
            P.dma("pool", d["krT"][0, :, rows], krS[0:64], reads=["krS"])
        P.barrier()


    def attn_core(self, P, G, heads, s_mms, mask_rhs, v_ap, scale, pT, o_tm, identb, rs, rkeys):
        ps = self.ps
        nkb = 4 * G + 4
        seq = [(h, kb) for h in range(heads) for kb in range(nkb)]

        def emit_S(idx):
            h, kb = seq[idx]
            sb_ = idx % 2
            pS = ps[:, sb_ * 512:(sb_ + 1) * 512]
            mms = list(s_mms(h, kb))
            m = mask_rhs(kb)
            if m is not None:
                mms.append((identb, m))
            for i, (lt, rh) in enumerate(mms):
                P.op("pe", lambda e, pS=pS, lt=lt, rh=rh, i=i, n=len(mms): e.matmul(pS, lhsT=lt, rhs=rh, start=(i == 0), stop=(i == n - 1)),
                     reads=rkeys, writes=[("psb", sb_)])

        emit_S(0)
        for idx, (h, kb) in enumerate(seq):
            sb_ = idx % 2
            pS = ps[:, sb_ * 512:(sb_ + 1) * 512]
            if idx + 1 < len(seq):
                emit_S(idx + 1)
            p_ = pT[sb_]
            P.op("act", lambda e, p_=p_, pS=pS: e.activation(out=p_, in_=pS, func=AF.Exp, scale=scale),
                 reads=[("psb", sb_)], writes=[("pT", sb_), ("psb", sb_)])
            for i in range(4):
                last = 4 * G + i
                if kb > last:
                    continue
                P.op("pe", lambda e, i=i, p_=p_, h=h, kb=kb, last=last: e.matmul(
                    ps[:, (4 + i) * 512:(4 + i) * 512 + 129], lhsT=p_[:, i * 128:(i + 1) * 128], rhs=v_ap(h, kb),
                    start=(kb == 0), stop=(kb == last)), reads=[("pT", sb_)] + rkeys, writes=[("psb", 4 + i)])
            if kb == nkb - 1:
                for i in range(4):
                    acc = ps[:, (4 + i) * 512:(4 + i) * 512 + 129]
                    r_ = rs[:, i:i + 1]
                    P.op("dve", lambda e, acc=acc, r_=r_: e.reciprocal(out=r_, in_=acc[:, 128:129]), reads=[("psb", 4 + i)], writes=[("rs", i), ("psb", 4 + i)])
                    P.op("dve", lambda e, acc=acc, i=i, h=h, r_=r_: e.tensor_scalar(out=o_tm[:, i, h * 128:(h + 1) * 128], in0=acc[:, 0:128],
                                                                                     scalar1=r_, scalar2=None, op0=ALU.mult),
                         reads=[("psb", 4 + i), ("rs", i)], writes=["o_tm", ("psb", 4 + i)])

    def store_mixT(self, P, G, o_tm, nch, c0, stage, identb):
        psb = self.ps.bitcast(BF16)
        d = self.d
        rows = slice(G * 512, (G + 1) * 512)
        for c in range(nch):
            bank = 2 + c % 2
            for i in range(4):
                P.op("pe", lambda e, c=c, i=i, bank=bank: e.transpose(out=psb[:, bank * 1024 + i * 128:bank * 1024 + (i + 1) * 128],
                                                                       in_=o_tm[:, i, c * 128:(c + 1) * 128], identity=identb),
                     reads=["o_tm", "identb"], writes=[("psb", bank)])
            P.op("act", lambda e, c=c, bank=bank: e.copy(out=stage[:, c, :], in_=psb[:, bank * 1024:bank * 1024 + 512]),
                 reads=[("psb", bank)], writes=["stage", ("psb", bank)])
        P.dma("pool", d["mixT"][c0:c0 + nch, :, rows].rearrange("c p t -> p c t"), stage[:, 0:nch, :], reads=["stage"], writes=[])

    def phase_dsa(self, P, A, l):
        d = self.d
        ps = self.ps
        psb = self.ps.bitcast(BF16)
        A.off = self.base_off
        identb = A.take(128, BF16)
        kiT = A.take(S, BF16)
        kaT = A.take(4 * S, BF16).rearrange("p (h t) -> p h t", h=4)
        va = A.take(32 * 516, BF16).rearrange("p (k h c) -> p k h c", k=32, h=4)
        qi_g = A.take(8 * 512, BF16).rearrange("p (c t) -> p c t", c=8)
        qa_g = A.take(4 * 512, BF16).rearrange("p (h t) -> p h t", h=4)
        wi_g = A.take(64).rearrange("p (s h) -> p s h", s=4)
        scores = [A.take(S) for _ in range(2)]
        rl = [[A.take(512, BF16) for _ in range(3)] for _ in range(2)]
        dg = [A.take(16 * 128, BF16).rearrange("p (h q) -> p h q", h=16) for _ in range(2)]
        junkb = A.take(S, BF16)
        mks = [A.take(S, BF16) for _ in range(2)]
        mkT = A.take(32 * 512, BF16).rearrange("p (k t) -> p k t", k=32)
        pT = [A.take(512, BF16) for _ in range(2)]
        o_tm = A.take(4 * 512, BF16).rearrange("p (s c) -> p s c", s=4)
        stage = A.take(4 * 512, BF16).rearrange("p (c t) -> p c t", c=4)
        pw2 = A.take(NBIS + 1)
        Ws = [A.take(NBIS + 1) for _ in range(2)]
        W2s = [A.take(NBIS + 1) for _ in range(2)]
        sms = [A.take(16) for _ in range(2)]
        rs = A.take(4)
        P.op("dve", lambda e: e.tensor_copy(out=identb, in_=self.ident), reads=["ident"], writes=["identb"])
        P.dma("sp", kiT, d["kiT"][0], writes=["kiT"])
        P.dma("sp", kaT, d["kaT"].rearrange("h p t -> p h t"), writes=["kaT"])
        P.dma("sp", va, d["va"].rearrange("(k p) (h c) -> p k h c", p=128, h=4), writes=["va"])
        P.dma("sp", pw2, d["pow2"], writes=["pw2"])
        a_scale = 128.0 ** -0.5
        cnt = {"s": 0, "l": 0}
        for G in range(self.cfg.get("ng", NG)):
            rows = slice(G * 512, (G + 1) * 512)
            P.dma("sp", qi_g, d["qiT"][:, :, rows].rearrange("c p t -> p c t"), writes=["qi_g"])
            P.dma("sp", qa_g, d["qaT"][:, :, rows].rearrange("h p t -> p h t"), writes=["qa_g"])
            P.dma("sp", wi_g, d["wi"][rows, :].rearrange("(s p) h -> p s h", p=128), writes=["wi_g"])
            P.op("pool", lambda e, G=G: e.memset(mkT[:, 4 * G:4 * G + 4, :], BIGNEG), writes=[("mkT", i_) for i_ in range(4)])
            def qchain(i, sl):
                qt = 4 * G + i
                n2 = 128 * (qt + 1)
                n1 = n2 - 64
                score = scores[sl]
                sm = sms[sl]
                W, W2 = Ws[sl], W2s[sl]
                lo, w0, mid, cn, upd, m8 = sm[:, 0:1], sm[:, 2:3], sm[:, 3:4], sm[:, 4:5], sm[:, 5:6], sm[:, 8:16]
                ksc, ksm, kW = ("score", sl), ("sm", sl), ("W", sl)
                lb0 = 4 * sl
                sbank = lb0 + 2
                tbank = lb0 + 3
                dg_ = dg[sl]
                kdg = ("dg", sl)
                P.op("pool", lambda e: e.tensor_tensor(out=dg_, in0=identb.unsqueeze(1).to_broadcast([128, 16, 128]),
                                                       in1=wi_g[:, i, :].unsqueeze(2).to_broadcast([128, 16, 128]), op=ALU.mult),
                     reads=["identb", "wi_g"], writes=[kdg])
                yield
                for k5 in range((n2 + 511) // 512):
                    wd_ = min(512, n2 - k5 * 512)
                    pacc = ps[:, sbank * 512:sbank * 512 + wd_]

                    def logit(h, wd_=wd_, k5=k5):
                        lb = lb0 + h % 2
                        base = (h % 2) * 64
                        P.op("pe", lambda e, lb=lb, base=base, h=h: e.matmul(
                            ps[:, lb * 512:lb * 512 + wd_], lhsT=qi_g[base:base + 64, h // 2, i * 128:(i + 1) * 128],
                            rhs=kiT[base:base + 64, k5 * 512:k5 * 512 + wd_], start=True, stop=True),
                             reads=["qi_g", "kiT"], writes=[("psb", lb)])

                    logit(0)
                    for h in range(16):
                        if h + 1 < 16:
                            logit(h + 1)
                        lb = lb0 + h % 2
                        rb = (sl, h % 3)
                        r_ = rl[sl][h % 3][:, 0:wd_]
                        pin = ps[:, lb * 512:lb * 512 + wd_]
                        if h % 3 != 2:
                            P.op("act", lambda e, r_=r_, pin=pin: e.activation(out=r_, in_=pin, func=AF.Relu),
                                 reads=[("psb", lb)], writes=[("rl", rb), ("psb", lb)])
                        else:
                            P.op("dve", lambda e, r_=r_, pin=pin: e.tensor_scalar(out=r_, in0=pin, scalar1=0.0, scalar2=None, op0=ALU.max),
                                 reads=[("psb", lb)], writes=[("rl", rb), ("psb", lb)])
                        P.op("pe", lambda e, pacc=pacc, h=h, r_=r_: e.matmul(pacc, lhsT=dg_[:, h, :], rhs=r_, start=(h == 0), stop=(h == 15)),
                             reads=[kdg, ("rl", rb)], writes=[("psb", sbank)])
                        yield
                    sc_blk = score[:, k5 * 512:k5 * 512 + wd_]
                    P.op("act", lambda e, sc_blk=sc_blk, pacc=pacc: e.copy(out=sc_blk, in_=pacc),
                         reads=[("psb", sbank)], writes=[ksc, ("psb", sbank)])
                    yield
                sc = score[:, 0:n2]
                P.op("dve", lambda e: e.tensor_reduce(out=lo, in_=sc, axis=AX.X, op=ALU.min), reads=[ksc], writes=[ksm])
                yield
                P.op("dve", lambda e: e.memset(score[0:64, n1:n2], BIGNEG), reads=[ksm], writes=[ksc])
                yield
                if qt >= 2:
                    P.op("dve", lambda e: e.max(out=m8, in_=sc), reads=[ksc], writes=[ksm])
                    yield
                    P.op("dve", lambda e: e.tensor_tensor(out=w0, in0=m8[:, 0:1], in1=lo, op=ALU.subtract), reads=[ksm], writes=[ksm])
                    yield
                    P.op("dve", lambda e: e.tensor_scalar(out=W, in0=pw2, scalar1=w0, scalar2=None, op0=ALU.mult), reads=[ksm, "pw2"], writes=[kW])
                    P.op("dve", lambda e: e.tensor_scalar(out=W2, in0=pw2, scalar1=w0, scalar2=2.0, op0=ALU.mult, op1=ALU.mult), reads=[ksm, "pw2"], writes=[kW])
                    yield
                    P.op("dve", lambda e: e.tensor_tensor(out=mid, in0=lo, in1=W[:, 0:1], op=ALU.add), reads=[ksm, kW], writes=[ksm])
                    yield
                    for k in range(NBIS):
                        P.op("dve", lambda e: e.tensor_scalar(out=junkb[:, 0:n2], in0=sc, scalar1=mid, scalar2=None, op0=ALU.is_ge,
                                                              op1=ALU.add, accum_out=cn), reads=[ksc, ksm], writes=[ksm])
                        yield
                        P.op("dve", lambda e, k=k: e.tensor_scalar(out=upd, in0=cn, scalar1=255.5, scalar2=W2[:, k + 1:k + 2], op0=ALU.is_ge, op1=ALU.mult),
                             reads=[ksm, kW], writes=[ksm])
                        yield
                        P.op("dve", lambda e, k=k: e.scalar_tensor_tensor(out=mid, in0=upd, scalar=W[:, k + 1:k + 2], in1=mid, op0=ALU.subtract, op1=ALU.add),
                             reads=[ksm, kW], writes=[ksm])
                        yield
                    P.op("dve", lambda e: e.tensor_tensor(out=lo, in0=mid, in1=W[:, NBIS:NBIS + 1], op=ALU.subtract), reads=[ksm, kW], writes=[ksm])
                    yield
                mk_ = mks[sl]
                kmk = ("mk", sl)
                P.op("dve", lambda e: e.tensor_scalar(out=mk_[:, 0:n2], in0=sc, scalar1=lo, scalar2=1.0, op0=ALU.is_ge, op1=ALU.subtract),
                     reads=[ksc, ksm], writes=[kmk])
                yield
                for kb0 in range(0, qt + 1, 8):
                    nb_ = min(8, qt + 1 - kb0)
                    for j in range(nb_):
                        kb = kb0 + j
                        P.op("pe", lambda e, j=j, kb=kb: e.transpose(out=psb[:, tbank * 1024 + j * 128:tbank * 1024 + (j + 1) * 128],
                                                                      in_=mk_[:, kb * 128:(kb + 1) * 128], identity=identb),
                             reads=[kmk, "identb"], writes=[("psb", tbank)])
                    P.op("act", lambda e, nb_=nb_, kb0=kb0: e.activation(
                        out=mkT[:, kb0:kb0 + nb_, i * 128:(i + 1) * 128],
                        in_=psb[:, tbank * 1024:tbank * 1024 + nb_ * 128].rearrange("p (k t) -> p k t", k=nb_), func=AF.Copy, scale=-BIGNEG),
                         reads=[("psb", tbank)], writes=[("mkT", i), ("psb", tbank)])
                    yield

            for pair in ((0, 1), (2, 3)):
                gens = [qchain(pair[0], 0), qchain(pair[1], 1)]
                while gens:
                    for g_ in list(gens):
                        try:
                            next(g_)
                        except StopIteration:
                            gens.remove(g_)
            self.attn_core(P, G, 4,
                           lambda h, kb: [(kaT[:, h, kb * 128:(kb + 1) * 128], qa_g[:, h, :])],
                           lambda kb: mkT[:, kb, :],
                           lambda h, kb: va[:, kb, h, :],
                           a_scale, pT, o_tm, identb, rs, ["kaT", "qa_g", "va", "identb"] + [("mkT", i_) for i_ in range(4)])
            self.store_mixT(P, G, o_tm, 4, 0, stage, identb)
        P.barrier()

    def phase_mla(self, P, A, l):
        d = self.d
        A.off = self.base_off
        identb = A.take(128, BF16)
        knT = A.take(8 * S, BF16).rearrange("p (h t) -> p h t", h=8)
        krT = A.take(S, BF16)
        vb = A.take(32 * 8 * 129, BF16).rearrange("p (k h c) -> p k h c", k=32, h=8)
        qn_g = A.take(8 * 512, BF16).rearrange("p (h t) -> p h t", h=8)
        qr_g = A.take(8 * 512, BF16).rearrange("p (h t) -> p h t", h=8)
        cmf = A.take(4 * 512)
        cm = A.take(4 * 512, BF16).rearrange("p (j t) -> p j t", j=4)
        pT = [A.take(512, BF16) for _ in range(2)]
        o_tm = A.take(4 * 1024, BF16).rearrange("p (s c) -> p s c", s=4)
        stage = A.take(8 * 512, BF16).rearrange("p (c t) -> p c t", c=8)
        rs = A.take(4)
        P.op("dve", lambda e: e.tensor_copy(out=identb, in_=self.ident), reads=["ident"], writes=["identb"])
        P.dma("sp", knT, d["knT"].rearrange("h p t -> p h t"), writes=["knT"])
        P.dma("sp", krT[0:64], d["krT"][0], writes=["krT"])
        P.dma("sp", vb, d["vb"].rearrange("(k p) (h c) -> p k h c", p=128, h=8), writes=["vb"])
        P.dma("sp", cmf, d["cmaskB"], writes=["cmf"])
        P.op("dve", lambda e: e.tensor_copy(out=cm, in_=cmf.rearrange("p (j t) -> p j t", j=4)), reads=["cmf"], writes=["cm"])
        b_scale = 192.0 ** -0.5
        for G in range(self.cfg.get("ng", NG)):
            rows = slice(G * 512, (G + 1) * 512)
            P.dma("sp", qn_g, d["qnT"][:, :, rows].rearrange("h p t -> p h t"), writes=["qn_g"])
            P.dma("sp", qr_g[0:64], d["qrT"][:, :, rows].rearrange("h p t -> p h t"), writes=["qr_g"])
            self.attn_core(P, G, 8,
                           lambda h, kb: [(knT[:, h, kb * 128:(kb + 1) * 128], qn_g[:, h, :]),
                                          (krT[0:64, kb * 128:(kb + 1) * 128], qr_g[0:64, h, :])],
                           lambda kb, G=G: (cm[:, kb - 4 * G, :] if kb >= 4 * G else None),
                           lambda h, kb: vb[:, kb, h, :],
                           b_scale, pT, o_tm, identb, rs, ["knT", "krT", "qn_g", "qr_g", "cm", "vb", "identb"])
            self.store_mixT(P, G, o_tm, 8, 4, stage, identb)
        P.barrier()

    def phase_sb(self, P, A, l):
        d = self.d
        ps = self.ps
        A.off = self.base_off
        identb = A.take(128, BF16)
        kcT = A.take(4 * S, BF16).rearrange("p (h t) -> p h t", h=4)
        vc = A.take(32 * 512, BF16).rearrange("p (k c) -> p k c", k=32)
        qc_g = A.take(4 * 512, BF16).rearrange("p (h t) -> p h t", h=4)
        tri = A.take(4 * 512).rearrange("p (j t) -> p j t", j=4)
        trib = A.take(4 * 512, BF16).rearrange("p (j t) -> p j t", j=4)
        ustr = A.take(128)
        ones = A.take(128)
        eb = [A.take(512) for _ in range(3)]
        spb = [A.take(512) for _ in range(3)]
        t1b = [A.take(512) for _ in range(3)]
        acc = A.take(512)
        wT = [A.take(512, BF16) for _ in range(3)]
        o_tm = A.take(4 * 512, BF16).rearrange("p (s c) -> p s c", s=4)
        stage = A.take(4 * 512, BF16).rearrange("p (c t) -> p c t", c=4)
        P.op("dve", lambda e: e.tensor_copy(out=identb, in_=self.ident), reads=["ident"], writes=["identb"])
        P.dma("sp", kcT, d["kcT"].rearrange("h p t -> p h t"), writes=["kcT"])
        P.dma("sp", vc, d["vc"].rearrange("(k p) c -> p k c", p=128), writes=["vc"])
        P.dma("sp", tri, d["tri"].rearrange("p (j t) -> p j t", j=4), writes=["tri"])
        P.dma("sp", ustr, d["ustrict"], writes=["ustr"])
        P.op("dve", lambda e: e.tensor_copy(out=trib, in_=tri), reads=["tri"], writes=["trib"])
        P.op("dve", lambda e: e.memset(ones, 1.0), writes=["ones"])
        c_scale = 128.0 ** -0.5
        gi = 0
        for G in range(self.cfg.get("ng", NG)):
            rows = slice(G * 512, (G + 1) * 512)
            P.dma("sp", qc_g, d["qcT"][:, :, rows].rearrange("h p t -> p h t"), writes=["qc_g"])
            seq = [(h, kb) for h in range(4) for kb in range(4 * G + 3, -1, -1)]
            n = len(seq)

            def bufs(idx):
                g3 = (gi + idx) % 3
                g2 = (gi + idx) % 2
                return g3, g2

            def stageA(idx):
                h, kb = seq[idx]
                g3, g2 = bufs(idx)
                pz = ps[:, g2 * 512:(g2 + 1) * 512]
                e_, sp_, t1_ = eb[g3], spb[g3], t1b[g3]
                j = kb - 4 * G
                P.op("pe", lambda e, pz=pz, h=h, kb=kb: e.matmul(pz, lhsT=kcT[:, h, kb * 128:(kb + 1) * 128], rhs=qc_g[:, h, :], start=True, stop=True),
                     reads=["kcT", "qc_g"], writes=[("psb", g2)])
                P.op("act", lambda e, e_=e_, pz=pz: e.activation(out=e_, in_=pz, func=AF.Exp, scale=c_scale),
                     reads=[("psb", g2)], writes=[("eb", g3), ("psb", g2)])
                P.op("act", lambda e, e_=e_, sp_=sp_: e.activation(out=sp_, in_=e_, func=AF.Ln, bias=1.0),
                     reads=[("eb", g3)], writes=[("sp", g3)])
                P.op("dve", lambda e, t1_=t1_, pz=pz, sp_=sp_: e.scalar_tensor_tensor(out=t1_, in0=pz, scalar=c_scale, in1=sp_, op0=ALU.mult, op1=ALU.subtract),
                     reads=[("psb", g2), ("sp", g3)], writes=[("t1", g3), ("psb", g2)])
                if j >= 0:
                    P.op("pool", lambda e, sp_=sp_, j=j: e.tensor_tensor(out=sp_, in0=sp_, in1=tri[:, j, :], op=ALU.mult),
                         reads=[("sp", g3), "tri"], writes=[("sp", g3)])

            def stageB(idx):
                h, kb = seq[idx]
                g3, g2 = bufs(idx)
                first = (kb == 4 * G + 3)
                pl = ps[:, (2 + g2) * 512:(3 + g2) * 512]
                sp_, t1_, w_ = spb[g3], t1b[g3], wT[g3]
                j = kb - 4 * G
                P.op("pe", lambda e, pl=pl, sp_=sp_, first=first: e.matmul(pl, lhsT=ustr, rhs=sp_, start=True, stop=first),
                     reads=["ustr", ("sp", g3)], writes=[("psb", 2 + g2)])
                if not first:
                    P.op("pe", lambda e, pl=pl: e.matmul(pl, lhsT=ones, rhs=acc, start=False, stop=True),
                         reads=["ones", "acc"], writes=[("psb", 2 + g2)])
                P.op("dve", lambda e, t1_=t1_, pl=pl: e.tensor_tensor(out=t1_, in0=t1_, in1=pl, op=ALU.subtract),
                     reads=[("t1", g3), ("psb", 2 + g2)], writes=[("t1", g3), ("psb", 2 + g2)])
                if first:
                    P.op("pool", lambda e, sp_=sp_: e.tensor_copy(out=acc, in_=sp_), reads=[("sp", g3)], writes=["acc"])
                else:
                    P.op("pool", lambda e, sp_=sp_: e.tensor_tensor(out=acc, in0=acc, in1=sp_, op=ALU.add), reads=[("sp", g3), "acc"], writes=["acc"])
                P.op("act", lambda e, w_=w_, t1_=t1_: e.activation(out=w_, in_=t1_, func=AF.Exp), reads=[("t1", g3)], writes=[("wT", g3)])
                if j >= 0:
                    P.op("pool", lambda e, w_=w_, j=j: e.tensor_tensor(out=w_, in0=w_, in1=trib[:, j, :], op=ALU.mult),
                         reads=[("wT", g3), "trib"], writes=[("wT", g3)])

            def stageC(idx):
                h, kb = seq[idx]
                g3, g2 = bufs(idx)
                w_ = wT[g3]
                for i in range(4):
                    if kb > 4 * G + i:
                        continue
                    P.op("pe", lambda e, i=i, w_=w_, kb=kb, h=h, G=G: e.matmul(
                        ps[:, (4 + i) * 512:(4 + i) * 512 + 128], lhsT=w_[:, i * 128:(i + 1) * 128], rhs=vc[:, kb, h * 128:(h + 1) * 128],
                        start=(kb == 4 * G + i), stop=(kb == 0)), reads=[("wT", g3), "vc"], writes=[("psb", 4 + i)])
                if kb == 0:
                    for i in range(4):
                        P.op("act", lambda e, i=i, h=h: e.copy(out=o_tm[:, i, h * 128:(h + 1) * 128], in_=ps[:, (4 + i) * 512:(4 + i) * 512 + 128]),
                             reads=[("psb", 4 + i)], writes=["o_tm", ("psb", 4 + i)])

            for it in range(n + 2):
                if it < n:
                    stageA(it)
                if 0 <= it - 1 < n:
                    stageB(it - 1)
                if 0 <= it - 2 < n:
                    stageC(it - 2)
            gi += n
            self.store_mixT(P, G, o_tm, 4, 12, stage, identb)
        P.barrier()

    def phase_outproj(self, P, A, l, src):
        d = self.d
        ps = self.ps
        A.off = self.base_off
        _, _, gb = self.sublayer_setup(P, A, l, 1, 1.0)
        wo = A.take(16 * D, BF16).rearrange("p (k n) -> p k n", k=16)
        mx = [A.take(16 * 512, BF16).rearrange("p (k t) -> p k t", k=16) for _ in range(2)]
        xr = [A.take(512) for _ in range(8)]
        y = d["y"]
        P.dma("sp", wo, d["wout_b"][l].rearrange("p (k n) -> p k n", k=16), writes=["wo"])
        pi = 0
        for G in range(self.cfg.get("ng", NG)):
            rows = slice(G * 512, (G + 1) * 512)
            m_ = mx[G % 2]
            P.dma("sp", m_, d["mixT"][:, :, rows].rearrange("c p t -> p c t"), writes=[("mx", G % 2)])
            for s in range(4):
                t = G * 4 + s
                for n in range(4):
                    pb = pi % 8
                    xp = xr[pi % 8]
                    kxp = ("xrb", pi % 8)
                    pi += 1
                    P.dma("pool", xp, src[t * 128:(t + 1) * 128, n * 512:(n + 1) * 512], reads=[("xr", t, n)], writes=[kxp])
                    pd = ps[:, pb * 512:(pb + 1) * 512]
                    for k in range(16):
                        P.op("pe", lambda e, pd=pd, m_=m_, k=k, s=s, n=n: e.matmul(pd, lhsT=m_[:, k, s * 128:(s + 1) * 128], rhs=wo[:, k, n * 512:(n + 1) * 512],
                                                                                    start=(k == 0), stop=(k == 15)),
                             reads=[("mx", G % 2), "wo"], writes=[("psb", pb)])
                    P.op("dve", lambda e, pd=pd, n=n: e.tensor_tensor(out=pd, in0=pd, in1=gb[:, n * 512:(n + 1) * 512], op=ALU.mult),
                         reads=[("psb", pb), "gb"], writes=[("psb", pb)])
                    P.op("dve", lambda e, pd=pd, xp=xp: e.tensor_tensor(out=xp, in0=pd, in1=xp, op=ALU.add),
                         reads=[("psb", pb), kxp], writes=[kxp, ("psb", pb)])
                    P.dma("pool", y[t * 128:(t + 1) * 128, n * 512:(n + 1) * 512], xp, reads=[kxp], writes=[("xr", t, n)])
        P.barrier()

    def phase_ffn(self, P, A, l, f, src):
        d = self.d
        ps = self.ps
        A.off = self.base_off
        sub = 0 if f == 0 else 2
        shc, gsc, gb = self.sublayer_setup(P, A, l, sub, 0.5)
        xb = [A.take(D) for _ in range(4)]
        xr = [A.take(512) for _ in range(8)]
        junk = A.take(D, BF16)
        st = [A.take(4) for _ in range(2)]
        hT = A.take(16 * 512, BF16).rearrange("p (k t) -> p k t", k=16)
        actT = A.take(44 * 512, BF16).rearrange("p (c t) -> p c t", c=44)
        wgb = [A.take(16 * 256, BF16).rearrange("p (k c) -> p k c", k=16) for _ in range(2)]
        wub = [A.take(16 * 256, BF16).rearrange("p (k c) -> p k c", k=16) for _ in range(2)]
        wdb = [A.take(22 * 512, BF16).rearrange("p (c n) -> p c n", c=22) for _ in range(2)]
        sg = [A.take(512) for _ in range(2)]
        wt0 = (l * 2 + f) * 22
        wd0 = (l * 2 + f) * 8
        hTk = [("hT", s) for s in range(4)]
        y = d["y"]
        pi = 0
        ng = self.cfg.get("ng", NG)
        stop = self.cfg.get("ffn_stop", 9)
        if stop <= 1:
            P.barrier()
            return
        self.norm_loads(P, src, 0, xb)
        self.norm_group(P, src, 0, hT, shc, gsc, xb, junk, st)
        if stop <= 2:
            P.barrier()
            return
        for g in range(ng):
            for jb in range(22):
                b = jb % 2
                P.dma("sp", wgb[b], d["wg_b"][wt0 + jb].rearrange("p (k c) -> p k c", k=16), writes=[("wg", b)])
                P.dma("sp", wub[b], d["wu_b"][wt0 + jb].rearrange("p (k c) -> p k c", k=16), writes=[("wu", b)])
                for cc in range(2):
                    c = jb * 2 + cc
                    pb = c % 2
                    pg = ps[:, (0 + pb) * 512:(1 + pb) * 512]
                    pu = ps[:, (2 + pb) * 512:(3 + pb) * 512]
                    for k in range(16):
                        P.op("pe", lambda e, pg=pg, b=b, k=k, cc=cc: e.matmul(pg, lhsT=wgb[b][:, k, cc * 128:(cc + 1) * 128], rhs=hT[:, k, :],
                                                                              start=(k == 0), stop=(k == 15)),
                             reads=[("wg", b)] + hTk, writes=[("psb", pb)])
                    for k in range(16):
                        P.op("pe", lambda e, pu=pu, b=b, k=k, cc=cc: e.matmul(pu, lhsT=wub[b][:, k, cc * 128:(cc + 1) * 128], rhs=hT[:, k, :],
                                                                              start=(k == 0), stop=(k == 15)),
                             reads=[("wu", b)] + hTk, writes=[("psb", 2 + pb)])
                    sgt = sg[c % 2]
                    P.op("act", lambda e, sgt=sgt, pg=pg: e.activation(out=sgt, in_=pg, func=AF.Silu),
                         reads=[("psb", pb)], writes=[("sg", c % 2), ("psb", pb)])
                    P.op("dve", lambda e, sgt=sgt, pu=pu, c=c: e.tensor_tensor(out=actT[:, c, :], in0=pu, in1=sgt, op=ALU.mult),
                         reads=[("psb", 2 + pb), ("sg", c % 2)], writes=[("actT", c), ("psb", 2 + pb)])
            if stop <= 3:
                continue
            if g + 1 < ng:
                self.norm_loads(P, src, g + 1, xb)
            for n in range(4):
                if n == 2 and g + 1 < ng:
                    self.norm_group(P, src, g + 1, hT, shc, gsc, xb, junk, st)
                xps = []
                for s in range(4):
                    t = g * 4 + s
                    xp = xr[pi % 8]
                    kxp = ("xrb", pi % 8)
                    pi += 1
                    xps.append((xp, kxp))
                    P.dma("pool", xp, src[t * 128:(t + 1) * 128, n * 512:(n + 1) * 512], reads=[("xr", t, n)], writes=[kxp])
                for hf in range(2):
                    wi = n * 2 + hf
                    b = wi % 2
                    P.dma("sp", wdb[b], d["wd_b"][wd0 + wi].rearrange("p (c n) -> p c n", c=22), writes=[("wd", b)])
                    for s in range(4):
                        pd = ps[:, (4 + s) * 512:(5 + s) * 512]
                        for c in range(22):
                            ca = hf * 22 + c
                            P.op("pe", lambda e, pd=pd, b=b, c=c, ca=ca, s=s, hf=hf: e.matmul(
                                pd, lhsT=actT[:, ca, s * 128:(s + 1) * 128], rhs=wdb[b][:, c, :],
                                start=(hf == 0 and c == 0), stop=(hf == 1 and c == 21)),
                                 reads=[("wd", b), ("actT", ca)], writes=[("psb", 4 + s)])
                for s in range(4):
                    t = g * 4 + s
                    pd = ps[:, (4 + s) * 512:(5 + s) * 512]
                    xp, kxp = xps[s]
                    P.op("dve", lambda e, pd=pd, n=n: e.tensor_tensor(out=pd, in0=pd, in1=gb[:, n * 512:(n + 1) * 512], op=ALU.mult),
                         reads=[("psb", 4 + s), "gb"], writes=[("psb", 4 + s)])
                    P.op("dve", lambda e, pd=pd, xp=xp: e.tensor_tensor(out=xp, in0=pd, in1=xp, op=ALU.add),
                         reads=[("psb", 4 + s), kxp], writes=[kxp, ("psb", 4 + s)])
                    P.dma("pool", y[t * 128:(t + 1) * 128, n * 512:(n + 1) * 512], xp, reads=[kxp], writes=[("xr", t, n)])
        P.barrier()

    def build(self):
        nc = self.nc
        self.declare()
        d = self.d
        with ExitStack() as es:
            NB = 211968
            big = es.enter_context(nc.sbuf_tensor("big", [128, NB // 4], F32))
            self.ps = es.enter_context(nc.psum_tensor("ps", [128, 4096], F32))
            A = Alloc(big, NB)
            P = Prog(nc)
            self.ident = A.take(128)
            self.gcol = A.take(NL * 3 * 16)
            P.dma("sp", self.ident, d["ident"], writes=["ident"])
            P.dma("sp", self.gcol, d["gcol"], writes=["gcol"])
            self.base_off = A.off
            P.barrier()
            if self.cfg.get("do_adaln", True):
                self.phase_adaln(P, A)
            if self.cfg.get("do_prep", True):
                self.phase_prep(P, A)
            src = d["x"]
            for l in range(self.cfg["nl"] if self.cfg.get("do_ffn", True) else 0):
                if self.cfg["ffn"][0]:
                    self.phase_ffn(P, A, l, 0, src)
                    src = d["y"]
                if self.cfg.get("mix", False):
                    mp = self.cfg.get("mixparts", "pabco")
                    if "p" in mp:
                        self.phase_mixproj(P, A, l, src)
                    if "a" in mp:
                        self.phase_dsa(P, A, l)
                    if "b" in mp:
                        self.phase_mla(P, A, l)
                    if "c" in mp:
                        self.phase_sb(P, A, l)
                    if "o" in mp:
                        self.phase_outproj(P, A, l, src)
                        src = d["y"]
                if self.cfg["ffn"][1]:
                    self.phase_ffn(P, A, l, 1, src)
                    src = d["y"]
            P.barrier()
            P.emit(es)
            self.P = P
        return nc


def host_layout(inp):
    sh = {}
    w_ada = np.asarray(inp["w_ada"], dtype=np.float32)
    sh["wada"] = np.ascontiguousarray(
        w_ada.reshape(NL, 16, 128, 36, 512).transpose(0, 3, 2, 1, 4)).reshape(NL * 36, 128, 16 * 512)
    sh["bada"] = np.ascontiguousarray(np.asarray(inp["b_ada"], dtype=np.float32))
    g = np.stack([np.asarray(inp[k], dtype=np.float32) for k in ("g_ffn1", "g_mix", "g_ffn2")], axis=1)
    sh["gcol"] = np.ascontiguousarray(g.reshape(NL, 3, 16, 128).transpose(3, 0, 1, 2)).reshape(128, NL * 3 * 16)

    def gu(a, b):
        w = np.stack([np.asarray(inp[a], dtype=np.float32), np.asarray(inp[b], dtype=np.float32)], axis=1)
        w = w.reshape(NL, 2, 16, 128, 22, 256).transpose(0, 1, 4, 3, 2, 5)
        return np.ascontiguousarray(w).reshape(NL * 2 * 22, 128, 16 * 256)

    sh["wg"] = gu("w1_gate", "w2_gate")
    sh["wu"] = gu("w1_up", "w2_up")
    w = np.stack([np.asarray(inp["w1_down"], dtype=np.float32), np.asarray(inp["w2_down"], dtype=np.float32)], axis=1)
    w = w.reshape(NL, 2, 2, 22, 128, 4, 512).transpose(0, 1, 5, 2, 4, 3, 6)
    sh["wd"] = np.ascontiguousarray(w).reshape(NL * 2 * 8, 128, 22 * 512)
    sh["ident"] = np.eye(128, dtype=np.float32)
    w_in = np.asarray(inp["w_in"], dtype=np.float32)
    sp = np.cumsum([0, 512, 512, 512, 1024, 64, 16, 448, 128, 64, 512, 512, 512])
    qa, ka, va, qi, ki, wi, cq, ckv, kr, qc, kc, vc = [np.arange(sp[i], sp[i + 1]) for i in range(12)]
    tm = np.concatenate([qa, ka, va, vc, cq, kr, ckv, wi])
    fm = np.concatenate([qi, ki, ki, qc, kc])
    sh["wtm"] = np.ascontiguousarray(w_in[:, :, tm].reshape(NL, 16, 128, TM_COLS).transpose(0, 2, 1, 3)).reshape(NL, 128, 16 * TM_COLS)
    sh["wfm"] = np.ascontiguousarray(w_in[:, :, fm].reshape(NL, 16, 128, FM_COLS).transpose(0, 2, 1, 3)).reshape(NL, 128, 16 * FM_COLS)
    wuq = np.zeros((NL, 512, 1536), np.float32)
    wuq[:, :448] = np.asarray(inp["w_uq"], dtype=np.float32)
    sh["wuq"] = np.ascontiguousarray(wuq.reshape(NL, 4, 128, 1536).transpose(0, 2, 1, 3)).reshape(NL, 128, 4 * 1536)
    sh["wukv"] = np.ascontiguousarray(np.asarray(inp["w_ukv"], dtype=np.float32))
    sh["wout"] = np.ascontiguousarray(np.asarray(inp["w_out"], dtype=np.float32).reshape(NL, 16, 128, D).transpose(0, 2, 1, 3)).reshape(NL, 128, 16 * D)
    gt = np.concatenate([np.asarray(inp[k], dtype=np.float32) for k in
                         ("g_qa", "g_ka", "g_cq", "g_ckv", "g_q_nope", "g_k_nope", "g_q_rope", "g_k_rope")], axis=1)
    sh["gtm"] = np.ascontiguousarray(np.broadcast_to(gt.reshape(1, NL * GT), (128, NL * GT)))
    def tables(dim):
        inv = (1.0 / (np.float32(500000.0) ** (np.arange(0, dim, 2, dtype=np.float32) / np.float32(dim)))).astype(np.float32)
        ang = (np.arange(S, dtype=np.float32)[:, None] * inv[None, :]).astype(np.float32)
        return np.cos(ang).astype(np.float32), np.sin(ang).astype(np.float32)
    ca, sa = tables(32)
    cm_, sm_ = tables(64)
    sh["ropeA"] = np.ascontiguousarray(np.concatenate([np.tile(ca, (1, 4)), np.tile(sa, (1, 4))], axis=1))
    sh["ropeQ"] = np.ascontiguousarray(np.concatenate([np.tile(cm_, (1, 8)), np.tile(sm_, (1, 8))], axis=1))
    sh["ropeK"] = np.ascontiguousarray(np.concatenate([cm_, sm_], axis=1))
    cmask = np.zeros((128, 4, 4, 128), np.float32)
    tri = np.zeros((128, 4, 4, 128), np.float32)
    kk = np.arange(128)[:, None]
    qq = np.arange(128)[None, :]
    for j in range(4):
        for i in range(4):
            if i < j:
                cmask[:, j, i, :] = BIGNEG
            elif i == j:
                cmask[:, j, i, :] = np.where((kk // 64) <= (qq // 64), 0.0, BIGNEG)
                tri[:, j, i, :] = (kk < qq)
            else:
                tri[:, j, i, :] = 1.0
    sh["cmaskB"] = cmask.reshape(128, 2048)
    sh["tri"] = tri.reshape(128, 2048)
    sh["ustrict"] = (np.arange(128)[:, None] > np.arange(128)[None, :]).astype(np.float32)
    sh["pow2"] = np.ascontiguousarray(np.broadcast_to((0.5 ** np.arange(1, NBIS + 2)).astype(np.float32)[None, :], (128, NBIS + 1)))
    return sh


def core_inputs(inp, sh, b):
    m = dict(sh)
    m["x"] = np.ascontiguousarray(np.asarray(inp["x"][b], dtype=np.float32))
    m["cT"] = np.ascontiguousarray(np.asarray(inp["c"][b], dtype=np.float32).reshape(16, 128).T)
    return m


DEFAULT_CFG = {"nl": 2, "ffn": (True, True), "mix": True}


def kernel(**inputs):
    nc = bass.Bass("TRN2", target_bir_lowering=False)
    bld = Builder(nc, DEFAULT_CFG)
    bld.build()
    sh = host_layout(inputs)
    in_maps = [core_inputs(inputs, sh, b) for b in range(8)]
    res = run_bass_kernel_spmd(nc, in_maps, core_ids=list(range(8)))
    return np.stack([np.asarray(r["y"], dtype=np.float32) for r in res.results], axis=0)
```

```python
import numpy as np
from contextlib import ExitStack
import concourse.bass as bass
import concourse.mybir as mybir
from concourse.bass_utils import run_bass_kernel_spmd

F32 = mybir.dt.float32
BF16 = mybir.dt.bfloat16
AF = mybir.ActivationFunctionType
ALU = mybir.AluOpType
AX = mybir.AxisListType

S = 4096
D = 2048
DFF = 5632
NL = 2
NT = S // 128
NG = S // 512
EPS = 1e-6
NIN = 4816
TM_COLS = 2704
FM_COLS = 2176
BIGNEG = -30000.0
GT = 1216
NBIS = 20

ENGS = ("pe", "act", "dve", "pool", "sp")
CHUNK = 12000
NPOOL = 40


class Op:
    __slots__ = ("eng", "fn", "deps", "sig", "seq", "dma", "dsem", "dval", "name")

    def __init__(self, eng, fn, dma=False, name=""):
        self.eng = eng
        self.fn = fn
        self.deps = []
        self.sig = False
        self.seq = 0
        self.dma = dma
        self.dsem = -1
        self.dval = 0
        self.name = name


class Prog:
    def __init__(self, nc):
        self.nc = nc
        self.q = {e: [] for e in ENGS}
        self.last_w = {}
        self.readers = {}
        self.dma_cnt = [0] * NPOOL
        self.dma_rr = 0
        self.live_dma = []
        self.last_real = {}

    def _add_dep(self, op, d, raw):
        if d is None or d is op:
            return
        if (not d.dma) and (not op.dma) and d.eng == op.eng and not raw:
            return
        if d in op.deps:
            return
        op.deps.append(d)
        d.sig = True

    def _track(self, op, reads, writes):
        for r in reads:
            self._add_dep(op, self.last_w.get(r), True)
        for w in writes:
            self._add_dep(op, self.last_w.get(w), False)
            for rd in self.readers.get(w, ()):
                self._add_dep(op, rd, False)
        for r in reads:
            lst = self.readers.setdefault(r, [])
            if not op.dma:
                lst[:] = [o for o in lst if o.dma or o.eng != op.eng]
            lst.append(op)
        for w in writes:
            self.last_w[w] = op
            self.readers[w] = []

    def op(self, eng, fn, reads=(), writes=(), name=""):
        o = Op(eng, fn, name=name)
        self._track(o, reads, writes)
        self.q[eng].append(o)
        self.last_real[eng] = o
        return o

    def dma(self, queue, out, in_, reads=(), writes=(), **kw):
        o = Op(queue, lambda e: e.dma_start(out=out, in_=in_, **kw), dma=True)
        i = self.dma_rr
        self.dma_rr = (self.dma_rr + 1) % NPOOL
        self.dma_cnt[i] += 1
        o.dsem = i
        o.dval = 16 * self.dma_cnt[i]
        self._track(o, reads, writes)
        self.q[queue].append(o)
        self.live_dma.append(o)
        return o

    def barrier(self):
        lasts = list(self.last_real.values())
        dmas = list(self.live_dma)
        self.live_dma = []
        for e in ENGS:
            b = Op(e, None, name="barrier")
            for d in lasts:
                if d.eng != e:
                    b.deps.append(d)
                    d.sig = True
            for d in dmas:
                b.deps.append(d)
            self.q[e].append(b)
        self.last_w = {}
        self.readers = {}

    def emit(self, es):
        nc = self.nc
        nsig = {}
        for e in ENGS:
            n = 0
            for o in self.q[e]:
                if o.sig and not o.dma:
                    n += 1
                    o.seq = n
            nsig[e] = n
        sems = {}
        for e in ENGS:
            k = (nsig[e] + CHUNK - 1) // CHUNK
            sems[e] = [es.enter_context(nc.semaphore(f"s_{e}{i}")) for i in range(k)]
        dsems = [es.enter_context(nc.semaphore(f"s_dma{i}")) for i in range(NPOOL)]
        self.nsig = nsig

        def run(ename):
            def body(eng):
                seen_eng = {}
                seen_dma = {}
                for o in self.q[ename]:
                    for d in o.deps:
                        if d.dma:
                            if seen_dma.get(d.dsem, 0) >= d.dval:
                                continue
                            eng.wait_ge(dsems[d.dsem], d.dval)
                            seen_dma[d.dsem] = d.dval
                        else:
                            if seen_eng.get(d.eng, 0) >= d.seq:
                                continue
                            c = (d.seq - 1) // CHUNK
                            eng.wait_ge(sems[d.eng][c], d.seq - c * CHUNK)
                            seen_eng[d.eng] = d.seq
                    if o.fn is None:
                        continue
                    if o.dma:
                        prev = o.dval - 16
                        if prev > 0 and seen_dma.get(o.dsem, 0) < prev:
                            eng.wait_ge(dsems[o.dsem], prev)
                            seen_dma[o.dsem] = prev
                        ins = o.fn(eng)
                        ins.then_inc(dsems[o.dsem], 16)
                    else:
                        ins = o.fn(eng)
                        if o.sig:
                            c = (o.seq - 1) // CHUNK
                            ins.then_inc(sems[ename][c], 1)
            return body

        with nc.Block() as block:
            block.tensor(run("pe"))
            block.scalar(run("act"))
            block.vector(run("dve"))
            block.gpsimd(run("pool"))
            block.sync(run("sp"))


class Alloc:
    def __init__(self, big, nbytes):
        self.big = big
        self.nbytes = nbytes
        self.off = 0
        self.registry = {}
        self.keep = []

    def take(self, cols, dtype=F32, parts=128):
        esz = 4 if dtype == F32 else 2
        nb = (cols * esz + 63) // 64 * 64
        assert self.off + nb <= self.nbytes, (self.off, nb, self.nbytes)
        v = self.big[0:parts, self.off // 4:(self.off + nb) // 4]
        off0 = self.off
        self.off += nb
        if dtype != F32:
            v = v.bitcast(dtype)
        r = v[:, 0:cols]
        if self.registry is not None:
            self.registry[id(r)] = off0
            self.keep.append(r)
        return r


class Builder:
    def __init__(self, nc, cfg):
        self.nc = nc
        self.cfg = cfg
        self.uid = 0

    def big_flat(self, ap0, ncols):
        off = self._alloc_offsets[id(ap0)]
        return self.A.big[:, off // 4:off // 4 + ncols]

    def dram_in(self, name, shape, dt=F32):
        return self.nc.dram_tensor(name, list(shape), dt, kind="ExternalInput").ap()

    def dram_scr(self, name, shape, dt):
        kind = "ExternalOutput" if name in self.cfg.get("dbg", ()) else "Internal"
        return self.nc.dram_tensor(name, list(shape), dt, kind=kind).ap()

    def declare(self):
        nl = self.cfg["nl"]
        d = {}
        d["x"] = self.dram_in("x", [S, D])
        d["cT"] = self.dram_in("cT", [128, 16])
        d["wada"] = self.dram_in("wada", [NL * 36, 128, 16 * 512])
        d["bada"] = self.dram_in("bada", [NL, 9 * D])
        d["gcol"] = self.dram_in("gcol", [128, NL * 3 * 16])
        d["wg"] = self.dram_in("wg", [NL * 2 * 22, 128, 16 * 256])
        d["wu"] = self.dram_in("wu", [NL * 2 * 22, 128, 16 * 256])
        d["wd"] = self.dram_in("wd", [NL * 2 * 8, 128, 22 * 512])
        d["ident"] = self.dram_in("ident", [128, 128])
        d["wtm"] = self.dram_in("wtm", [NL, 128, 16 * TM_COLS])
        d["wfm"] = self.dram_in("wfm", [NL, 128, 16 * FM_COLS])
        d["wuq"] = self.dram_in("wuq", [NL, 128, 4 * 1536])
        d["wukv"] = self.dram_in("wukv", [NL, 128, 2048])
        d["wout"] = self.dram_in("wout", [NL, 128, 16 * D])
        d["gtm"] = self.dram_in("gtm", [128, NL * GT])
        d["ropeA"] = self.dram_in("ropeA", [S, 128])
        d["ropeQ"] = self.dram_in("ropeQ", [S, 512])
        d["ropeK"] = self.dram_in("ropeK", [S, 64])
        d["cmaskB"] = self.dram_in("cmaskB", [128, 4 * 512])
        d["tri"] = self.dram_in("tri", [128, 4 * 512])
        d["ustrict"] = self.dram_in("ustrict", [128, 128])
        d["pow2"] = self.dram_in("pow2", [128, NBIS + 1])
        d["y"] = self.nc.dram_tensor("y", [S, D], F32, kind="ExternalOutput").ap()
        d["modv"] = self.dram_scr("modv", [NL, 9 * D], F32)
        d["wg_b"] = self.dram_scr("wg_b", [NL * 2 * 22, 128, 16 * 256], BF16)
        d["wu_b"] = self.dram_scr("wu_b", [NL * 2 * 22, 128, 16 * 256], BF16)
        d["wd_b"] = self.dram_scr("wd_b", [NL * 2 * 8, 128, 22 * 512], BF16)
        d["wtm_b"] = self.dram_scr("wtm_b", [NL, 128, 16 * TM_COLS], BF16)
        d["wfm_b"] = self.dram_scr("wfm_b", [NL, 128, 16 * FM_COLS], BF16)
        d["wuq_b"] = self.dram_scr("wuq_b", [NL, 128, 4 * 1536], BF16)
        d["wukv_b"] = self.dram_scr("wukv_b", [NL, 128, 2048], BF16)
        d["wout_b"] = self.dram_scr("wout_b", [NL, 128, 16 * D], BF16)
        for nm, shp in (("qaT", [4, 128, S]), ("kaT", [4, 128, S]), ("qiT", [8, 128, S]), ("kiT", [1, 128, S]),
                        ("qnT", [8, 128, S]), ("qrT", [8, 64, S]), ("knT", [8, 128, S]), ("krT", [1, 64, S]),
                        ("qcT", [4, 128, S]), ("kcT", [4, 128, S]), ("mixT", [16, 128, S]),
                        ("va", [S, 4 * 129]), ("vb", [S, 8 * 129]), ("vc", [S, 512])):
            d[nm] = self.dram_scr(nm + "_d", shp, BF16)
        d["wi"] = self.dram_scr("wi_d", [S, 16], F32)
        self.d = d

    def cast_tiles(self, P, src, dst, tiles, cols, bufs, step=4096):
        i = self.uid
        for t in tiles:
            for c0 in range(0, cols, step):
                c1 = min(cols, c0 + step)
                fb, bb = bufs[i % len(bufs)]
                kf = ("castf", i % len(bufs))
                kb = ("castb", i % len(bufs))
                P.dma("sp", fb[:, 0:c1 - c0], src[t, :, c0:c1], writes=[kf])
                o_, i_ = bb[:, 0:c1 - c0], fb[:, 0:c1 - c0]
                if i % 2 == 0:
                    P.op("dve", lambda e, o_=o_, i_=i_: e.tensor_copy(out=o_, in_=i_), reads=[kf], writes=[kb])
                else:
                    P.op("act", lambda e, o_=o_, i_=i_: e.copy(out=o_, in_=i_), reads=[kf], writes=[kb])
                P.dma("pool", dst[t, :, c0:c1], bb[:, 0:c1 - c0], reads=[kb], writes=[])
                i += 1
        self.uid = i

    def phase_prep(self, P, A):
        d = self.d
        nl = self.cfg["nl"]
        A.off = self.base_off
        bufs = [(A.take(4096, F32), A.take(4096, BF16)) for _ in range(4)]
        for l in range(nl):
            for f in range(2):
                if not self.cfg["ffn"][f]:
                    continue
                t0 = (l * 2 + f) * 22
                self.cast_tiles(P, d["wg"], d["wg_b"], range(t0, t0 + 22), 16 * 256, bufs)
                self.cast_tiles(P, d["wu"], d["wu_b"], range(t0, t0 + 22), 16 * 256, bufs)
                t0 = (l * 2 + f) * 8
                self.cast_tiles(P, d["wd"], d["wd_b"], range(t0, t0 + 8), 22 * 512, bufs, step=2816)
        if self.cfg.get("mix", False):
            for l in range(nl):
                for nm, cols in (("wtm", 16 * TM_COLS), ("wfm", 16 * FM_COLS), ("wuq", 4 * 1536), ("wukv", 2048), ("wout", 16 * D)):
                    self.cast_tiles(P, d[nm], d[nm + "_b"], [l], cols, bufs)
        P.barrier()

    def phase_adaln(self, P, A):
        d = self.d
        nl = self.cfg["nl"]
        ps = self.ps
        A.off = self.base_off
        cact = A.take(16)
        wt = [A.take(16 * 512) for _ in range(2)]
        bt = [A.take(512, parts=1) for _ in range(2)]
        rt = [A.take(512, parts=1) for _ in range(2)]
        P.dma("sp", cact, d["cT"], writes=["cact"])
        P.op("act", lambda e: e.activation(out=cact, in_=cact, func=AF.Silu), reads=["cact"], writes=["cact"])
        for l in range(nl):
            for j in range(36):
                i = l * 36 + j
                w = wt[i % 2]
                w3 = w.rearrange("p (k c) -> p k c", k=16)
                P.dma("sp", w, d["wada"][i], writes=[("wt", i % 2)])
                P.dma("pool", bt[i % 2], d["bada"][l:l + 1, j * 512:(j + 1) * 512], writes=[("bt", i % 2)])
                pso = ps[0:1, (i % 2) * 512:(i % 2) * 512 + 512]
                for k in range(16):
                    P.op("pe", lambda e, k=k, pso=pso, w3=w3: e.matmul(pso, lhsT=cact[:, k:k + 1], rhs=w3[:, k, :],
                                                                         start=(k == 0), stop=(k == 15)),
                         reads=["cact", ("wt", i % 2)], writes=[("psb", i % 2)])
                r = rt[i % 2]
                b = bt[i % 2]
                P.op("dve", lambda e, r=r, pso=pso, b=b: e.tensor_tensor(out=r, in0=pso, in1=b, op=ALU.add),
                     reads=[("psb", i % 2), ("bt", i % 2)], writes=[("rt", i % 2), ("psb", i % 2)])
                P.dma("pool", d["modv"][l:l + 1, j * 512:(j + 1) * 512], r, reads=[("rt", i % 2)], writes=["modv"])
        P.barrier()

    def sublayer_setup(self, P, A, l, sub, gate_scale, want_cols=True):
        d = self.d
        shc = A.take(16)
        scc = A.take(16)
        if gate_scale is None:
            gb = None
        else:
            gb = A.take(D)
        mv = d["modv"]
        P.dma("sp", shc, mv[l, (3 * sub) * D:(3 * sub + 1) * D].rearrange("(k p) -> p k", p=128),
              writes=["shc"], allow_slow_non_contiguous=True)
        P.dma("sp", scc, mv[l, (3 * sub + 1) * D:(3 * sub + 2) * D].rearrange("(k p) -> p k", p=128),
              writes=["scc"], allow_slow_non_contiguous=True)
        if gb is not None:
            P.dma("sp", gb, mv[l, (3 * sub + 2) * D:(3 * sub + 3) * D].partition_broadcast(128), writes=["gb"])
        gc = self.gcol[:, (l * 3 + sub) * 16:(l * 3 + sub + 1) * 16]
        P.op("dve", lambda e: e.scalar_tensor_tensor(out=scc, in0=scc, scalar=1.0, in1=gc, op0=ALU.add, op1=ALU.mult),
             reads=["scc", "gcol"], writes=["scc"])
        if gb is not None:
            P.op("dve", lambda e: e.tensor_scalar(out=gb, in0=gb, scalar1=1.0, scalar2=gate_scale, op0=ALU.add, op1=ALU.mult),
                 reads=["gb"], writes=["gb"])
        return shc, scc, gb

    def norm_loads(self, P, src, g, xb):
        for s in range(4):
            t = g * 4 + s
            P.dma("pool", xb[s % len(xb)], src[t * 128:(t + 1) * 128, :], reads=[("xr", t, n) for n in range(4)], writes=[("xb", s % len(xb))])

    def norm_group(self, P, src, g, hT, shc, gsc, xb, junk, st, inline_load=False, junk_key="junk"):
        ps = self.ps
        for s in range(4):
            t = g * 4 + s
            xt = xb[s % len(xb)]
            kx = ("xb", s % len(xb))
            if inline_load:
                P.dma("pool", xt, src[t * 128:(t + 1) * 128, :], reads=[("xr", t, n) for n in range(4)], writes=[kx])
            ss = st[t % 2]
            kss = ("st", t % 2)
            P.op("act", lambda e, xt=xt, ss=ss: e.activation(out=junk, in_=xt, func=AF.Square, accum_out=ss[:, 0:1]),
                 reads=[kx], writes=[junk_key, kss])
            P.op("dve", lambda e, ss=ss: e.tensor_scalar(out=ss[:, 1:2], in0=ss[:, 0:1], scalar1=1.0 / D, scalar2=EPS,
                                                         op0=ALU.mult, op1=ALU.add), reads=[kss], writes=[kss])
            P.op("act", lambda e, ss=ss: e.activation(out=ss[:, 2:3], in_=ss[:, 1:2], func=AF.Sqrt), reads=[kss], writes=[kss])
            P.op("dve", lambda e, ss=ss: e.reciprocal(out=ss[:, 3:4], in_=ss[:, 2:3]), reads=[kss], writes=[kss])
            P.op("dve", lambda e, xt=xt, ss=ss: e.tensor_scalar(out=xt, in0=xt, scalar1=ss[:, 3:4], scalar2=None, op0=ALU.mult),
                 reads=[kx, kss], writes=[kx])
            for kg in range(4):
                bank = kg % 2
                kp = ("psb", bank)
                for q in range(4):
                    k = kg * 4 + q
                    pt = ps[:, bank * 512 + q * 128: bank * 512 + (q + 1) * 128]
                    P.op("pe", lambda e, pt=pt, xt=xt, k=k: e.transpose(out=pt, in_=xt[:, k * 128:(k + 1) * 128], identity=self.ident),
                         reads=[kx, "ident"], writes=[kp])
                for q in range(4):
                    k = kg * 4 + q
                    pt = ps[:, bank * 512 + q * 128: bank * 512 + (q + 1) * 128]
                    P.op("act", lambda e, pt=pt, k=k, s=s: e.activation(out=hT[:, k, s * 128:(s + 1) * 128], in_=pt, func=AF.Identity,
                                                                         scale=gsc[:, k:k + 1], bias=shc[:, k:k + 1]),
                         reads=[kp, "scc", "shc"], writes=[("hT", s), kp])


    def headnorm(self, P, x3, H, dh, gain, tmp3, stat, kx):
        kt, ks = ("hn_tmp", kx), ("hn_stat", kx)
        P.op("dve", lambda e: e.tensor_tensor(out=tmp3, in0=x3, in1=x3, op=ALU.mult), reads=[kx], writes=[kt])
        yield
        P.op("dve", lambda e: e.tensor_reduce(out=stat[:, 0:H], in_=tmp3, axis=AX.X, op=ALU.add), reads=[kt], writes=[ks])
        yield
        P.op("dve", lambda e: e.tensor_scalar(out=stat[:, H:2 * H], in0=stat[:, 0:H], scalar1=1.0 / dh, scalar2=EPS,
                                              op0=ALU.mult, op1=ALU.add), reads=[ks], writes=[ks])
        yield
        P.op("act", lambda e: e.activation(out=stat[:, H:2 * H], in_=stat[:, H:2 * H], func=AF.Sqrt), reads=[ks], writes=[ks])
        yield
        P.op("dve", lambda e: e.reciprocal(out=stat[:, 2 * H:3 * H], in_=stat[:, H:2 * H]), reads=[ks], writes=[ks])
        yield
        P.op("dve", lambda e: e.tensor_tensor(out=x3, in0=x3, in1=stat[:, 2 * H:3 * H].unsqueeze(2).to_broadcast([128, H, dh]),
                                              op=ALU.mult), reads=[kx, ks], writes=[kx])
        yield
        P.op("dve", lambda e: e.tensor_tensor(out=x3, in0=x3, in1=gain.unsqueeze(1).to_broadcast([128, H, dh]), op=ALU.mult),
             reads=[kx, "gt"], writes=[kx])
        yield

    def rope(self, P, x1, x2, cos3, sin3, rt, kx, krope):
        H, r2 = x1.shape[1], x1.shape[2]
        t = [r[:, 0:H * r2].rearrange("p (h r) -> p h r", h=H) for r in rt]
        kr = ("rope_tmp", kx)
        P.op("dve", lambda e: e.tensor_tensor(out=t[0], in0=x1, in1=cos3, op=ALU.mult), reads=[kx, krope], writes=[kr])
        P.op("dve", lambda e: e.tensor_tensor(out=t[1], in0=x2, in1=sin3, op=ALU.mult), reads=[kx, krope], writes=[kr])
        yield
        P.op("dve", lambda e: e.tensor_tensor(out=t[2], in0=x1, in1=sin3, op=ALU.mult), reads=[kx, krope], writes=[kr])
        P.op("dve", lambda e: e.tensor_tensor(out=t[3], in0=x2, in1=cos3, op=ALU.mult), reads=[kx, krope], writes=[kr])
        yield
        P.op("dve", lambda e: e.tensor_tensor(out=x1, in0=t[0], in1=t[1], op=ALU.subtract), reads=[kr], writes=[kx])
        P.op("dve", lambda e: e.tensor_tensor(out=x2, in0=t[2], in1=t[3], op=ALU.add), reads=[kr], writes=[kx])
        yield

    def phase_mixproj(self, P, A, l, src):
        d = self.d
        ps = self.ps
        psb = self.ps.bitcast(BF16)
        A.off = self.base_off
        shc, gsc, _ = self.sublayer_setup(P, A, l, 1, None)
        xb = [A.take(D) for _ in range(2)]
        st = [A.take(4) for _ in range(2)]
        hT = A.take(16 * 512, BF16).rearrange("p (k t) -> p k t", k=16)
        wblk = [A.take(16 * 512, BF16) for _ in range(2)]
        wblk.append(self.big_flat(xb[0], 2 * D).bitcast(BF16)[:, 0:16 * 512])
        wuq = A.take(4 * 1536, BF16).rearrange("p (j c) -> p j c", j=4)
        wukv = A.take(2048, BF16)
        gt = A.take(GT)
        identb = A.take(128, BF16)
        rAg = A.take(4 * 128).rearrange("p (s c) -> p s c", s=4)
        rQg = A.take(4 * 512).rearrange("p (s c) -> p s c", s=4)
        rKg = A.take(4 * 64).rearrange("p (s c) -> p s c", s=4)
        xsL = [A.take(1536) for _ in range(4)]
        tmpL = [A.take(1024) for _ in range(4)]
        statL = [A.take(32) for _ in range(4)]
        xbfL = [A.take(1536, BF16) for _ in range(4)]
        cqTL = [A.take(4 * 128, BF16).rearrange("p (j t) -> p j t", j=4) for _ in range(4)]
        ckvTL = [A.take(128, BF16) for _ in range(4)]
        qaS = A.take(4 * 512, BF16).rearrange("p (h t) -> p h t", h=4)
        kaS = A.take(4 * 512, BF16).rearrange("p (h t) -> p h t", h=4)
        qnS_flat = A.take(8 * 512, BF16)
        qnS = qnS_flat.rearrange("p (h t) -> p h t", h=8)
        junk = qnS_flat[:, 0:D]
        qrS = A.take(8 * 512, BF16).rearrange("p (h t) -> p h t", h=8)
        knS = A.take(8 * 512, BF16).rearrange("p (h t) -> p h t", h=8)
        krS = A.take(512, BF16)
        fmS = [A.take(512, BF16) for _ in range(2)]
        vaS = [A.take(4 * 129, BF16).rearrange("p (h c) -> p h c", h=4) for _ in range(4)]
        vbS = [A.take(8 * 129, BF16).rearrange("p (h c) -> p h c", h=8) for _ in range(4)]
        vcS = [A.take(512, BF16) for _ in range(4)]
        wiS = [A.take(16) for _ in range(4)]

        P.dma("sp", wuq, d["wuq_b"][l].rearrange("p (j c) -> p j c", j=4), writes=["wuq"])
        P.dma("sp", wukv, d["wukv_b"][l], writes=["wukv"])
        P.dma("sp", gt, d["gtm"][:, l * GT:(l + 1) * GT], writes=["gt"])
        P.op("dve", lambda e: e.tensor_copy(out=identb, in_=self.ident), reads=["ident"], writes=["identb"])
        for b in range(4):
            P.op("dve", lambda e, b=b: e.memset(vaS[b][:, :, 128:129], 1.0), writes=[("vaS", b)])
            P.op("dve", lambda e, b=b: e.memset(vbS[b][:, :, 128:129], 1.0), writes=[("vbS", b)])
        g_qa, g_ka = gt[:, 0:128], gt[:, 128:256]
        g_cq, g_ckv = gt[:, 256:704], gt[:, 704:832]
        g_qn, g_kn = gt[:, 832:960], gt[:, 960:1088]
        g_qr, g_kr = gt[:, 1088:1152], gt[:, 1152:1216]
        hTk = [("hT", s) for s in range(4)]
        tm_blocks = [(0, 512), (512, 1024), (1024, 1536), (1536, 2048), (2048, 2560), (2560, 2704)]
        wtm_v = d["wtm_b"][l].rearrange("p (k c) -> p k c", k=16)
        wfm_v = d["wfm_b"][l].rearrange("p (k c) -> p k c", k=16)
        fm_tiles = [(0, 512), (512, 1024), (1024, 1536), (1536, 2048), (2048, 2176)]
        fm_dest = ([("qiT", i) for i in range(8)] + [("kiT", 0)] + [("qcT", i) for i in range(4)] + [("kcT", i) for i in range(4)])
        cnt = {"w": 0, "pb": 0, "fm": 0}

        def wkeys(wb):
            return [("wblk", wb)] + ([("xb", 0), ("xb", 1)] if wb == 2 else [])

        ng = self.cfg.get("ng", NG)
        for g in range(ng):
            self.norm_group(P, src, g, hT, shc, gsc, xb, junk, st, inline_load=True, junk_key="qnS")
            rows = slice(g * 512, (g + 1) * 512)
            P.dma("pool", rAg, d["ropeA"][rows, :].rearrange("(s p) c -> p s c", p=128), writes=["ropeA"])
            P.dma("pool", rQg, d["ropeQ"][rows, :].rearrange("(s p) c -> p s c", p=128), writes=["ropeQ"])
            P.dma("pool", rKg, d["ropeK"][rows, :].rearrange("(s p) c -> p s c", p=128), writes=["ropeK"])

            def tm_chain(bi, s, w3, wb, nc_):
                t = g * 4 + s
                trows = slice(t * 128, (t + 1) * 128)
                tcol = slice(s * 128, (s + 1) * 128)
                xs, tmp, stat, xbf, cqT, ckvT = xsL[s], tmpL[s], statL[s], xbfL[s], cqTL[s], ckvTL[s]
                rt = [tmp[:, 256 * q:256 * (q + 1)] for q in range(4)]
                kxs, kxbf = ("xs", s), ("xbf", s)
                pb = s
                tb = 7

                def evac_xs(pbank, ncols, off=0):
                    P.op("act", lambda e: e.copy(out=xs[:, off:off + ncols], in_=ps[:, pbank * 512:pbank * 512 + ncols]),
                         reads=[("psb", pbank)], writes=[kxs, ("psb", pbank)])

                def tr(items):
                    for in_ap, off, w in items:
                        P.op("pe", lambda e, in_ap=in_ap, off=off, w=w: e.transpose(out=psb[0:w, tb * 1024 + off:tb * 1024 + off + 128], in_=in_ap,
                                                                                    identity=identb),
                             reads=[kxbf, "identb"], writes=[("psb", tb)])

                def evac7(out_ap, parts, off, ncols, wkey, view=None):
                    i_ = psb[0:parts, tb * 1024 + off:tb * 1024 + off + ncols]
                    if view is not None:
                        i_ = i_.rearrange("p (h t) -> p h t", h=view)
                    P.op("act", lambda e: e.copy(out=out_ap, in_=i_), reads=[("psb", tb)], writes=[wkey, ("psb", tb)])

                for k in range(16):
                    P.op("pe", lambda e, k=k: e.matmul(ps[:, pb * 512:pb * 512 + nc_], lhsT=hT[:, k, tcol], rhs=w3[:, k, :],
                                                       start=(k == 0), stop=(k == 15)),
                         reads=wkeys(wb) + [("hT", s)], writes=[("psb", pb)])
                yield
                if bi in (0, 1):
                    evac_xs(pb, 512)
                    yield
                    x3 = xs[:, 0:512].rearrange("p (h c) -> p h c", h=4)
                    yield from self.headnorm(P, x3, 4, 128, g_qa if bi == 0 else g_ka, tmp[:, 0:512].rearrange("p (h c) -> p h c", h=4), stat, kxs)
                    cos3 = rAg[:, s, 0:64].rearrange("p (h r) -> p h r", h=4)
                    sin3 = rAg[:, s, 64:128].rearrange("p (h r) -> p h r", h=4)
                    yield from self.rope(P, x3[:, :, 0:16], x3[:, :, 16:32], cos3, sin3, rt, kxs, "ropeA")
                    P.op("dve", lambda e: e.tensor_copy(out=xbf[:, 0:512], in_=xs[:, 0:512]), reads=[kxs], writes=[kxbf])
                    yield
                    tr([(xbf[:, h * 128:(h + 1) * 128], h * 128, 128) for h in range(4)])
                    evac7((qaS if bi == 0 else kaS)[:, :, tcol], 128, 0, 512, "qaS" if bi == 0 else "kaS", view=4)
                    yield
                elif bi == 2:
                    P.op("act", lambda e: e.copy(out=vaS[s][:, :, 0:128], in_=ps[:, pb * 512:(pb + 1) * 512].rearrange("p (h c) -> p h c", h=4)),
                         reads=[("psb", pb)], writes=[("vaS", s), ("psb", pb)])
                    P.dma("pool", d["va"][trows, :].rearrange("p (h c) -> p h c", h=4), vaS[s], reads=[("vaS", s)])
                    yield
                elif bi == 3:
                    P.op("act", lambda e: e.copy(out=vcS[s], in_=ps[:, pb * 512:(pb + 1) * 512]),
                         reads=[("psb", pb)], writes=[("vcS", s), ("psb", pb)])
                    P.dma("pool", d["vc"][trows, :], vcS[s], reads=[("vcS", s)])
                    yield
                elif bi == 4:
                    evac_xs(pb, 512)
                    yield
                    yield from self.headnorm(P, xs[:, 0:448].unsqueeze(1), 1, 448, g_cq, tmp[:, 0:448].unsqueeze(1), stat, kxs)
                    xk = xs[:, 448:512].unsqueeze(1)
                    yield from self.headnorm(P, xk, 1, 64, g_kr, tmp[:, 448:512].unsqueeze(1), stat, kxs)
                    yield from self.rope(P, xk[:, :, 0:32], xk[:, :, 32:64], rKg[:, s, 0:32].unsqueeze(1), rKg[:, s, 32:64].unsqueeze(1),
                                         rt, kxs, "ropeK")
                    P.op("dve", lambda e: e.tensor_copy(out=xbf[:, 0:512], in_=xs[:, 0:512]), reads=[kxs], writes=[kxbf])
                    yield
                    tr([(xbf[:, 0:128], 0, 128), (xbf[:, 128:256], 128, 128), (xbf[:, 256:384], 256, 128),
                        (xbf[:, 384:448], 384, 64), (xbf[:, 448:512], 512, 64)])
                    kcq = ("cqT", s)
                    evac7(cqT[:, 0:3, :], 128, 0, 384, kcq, view=3)
                    evac7(cqT[0:64, 3, :], 64, 384, 128, kcq)
                    evac7(krS[0:64, tcol], 64, 512, 128, "krS")
                    yield
                    for nb in range(3):
                        for j in range(4):
                            kk = 128 if j < 3 else 64
                            P.op("pe", lambda e, nb=nb, j=j, kk=kk: e.matmul(
                                ps[:, (4 + nb) * 512:(5 + nb) * 512], lhsT=cqT[0:kk, j, :], rhs=wuq[0:kk, j, nb * 512:(nb + 1) * 512],
                                start=(j == 0), stop=(j == 3)), reads=[kcq, "wuq"], writes=[("psb", 4 + nb)])
                        evac_xs(4 + nb, 512, off=nb * 512)
                        yield
                    q3 = xs[:, 0:1536].rearrange("p (h c) -> p h c", h=8)
                    yield from self.headnorm(P, q3[:, :, 0:128], 8, 128, g_qn, tmp[:, 0:1024].rearrange("p (h c) -> p h c", h=8), stat, kxs)
                    yield from self.headnorm(P, q3[:, :, 128:192], 8, 64, g_qr, tmp[:, 0:512].rearrange("p (h c) -> p h c", h=8), stat, kxs)
                    cosq = rQg[:, s, 0:256].rearrange("p (h r) -> p h r", h=8)
                    sinq = rQg[:, s, 256:512].rearrange("p (h r) -> p h r", h=8)
                    yield from self.rope(P, q3[:, :, 128:160], q3[:, :, 160:192], cosq, sinq, rt, kxs, "ropeQ")
                    P.op("dve", lambda e: e.tensor_copy(out=xbf[:, 0:1536], in_=xs[:, 0:1536]), reads=[kxs], writes=[kxbf])
                    yield
                    tr([(xbf[:, h * 192:h * 192 + 128], h * 128, 128) for h in range(8)])
                    evac7(qnS[:, :, tcol], 128, 0, 1024, "qnS", view=8)
                    yield
                    tr([(xbf[:, h * 192 + 128:h * 192 + 192], h * 128, 64) for h in range(8)])
                    evac7(qrS[0:64, :, tcol], 64, 0, 1024, "qrS", view=8)
                    yield
                else:
                    evac_xs(pb, 144)
                    yield
                    P.op("dve", lambda e: e.tensor_scalar(out=wiS[s], in0=xs[:, 128:144], scalar1=1.0 / 32.0, scalar2=None, op0=ALU.mult),
                         reads=[kxs], writes=[("wiS", s)])
                    P.dma("pool", d["wi"][trows, :], wiS[s], reads=[("wiS", s)])
                    yield
                    yield from self.headnorm(P, xs[:, 0:128].unsqueeze(1), 1, 128, g_ckv, tmp[:, 0:128].unsqueeze(1), stat, kxs)
                    P.op("dve", lambda e: e.tensor_copy(out=xbf[:, 0:128], in_=xs[:, 0:128]), reads=[kxs], writes=[kxbf])
                    yield
                    tr([(xbf[:, 0:128], 0, 128)])
                    kckv = ("ckvT", s)
                    evac7(ckvT, 128, 0, 128, kckv)
                    yield
                    for hh in range(2):
                        for nb in range(2):
                            P.op("pe", lambda e, hh=hh, nb=nb: e.matmul(
                                ps[:, (4 + nb) * 512:(5 + nb) * 512], lhsT=ckvT, rhs=wukv[:, hh * 1024 + nb * 512:hh * 1024 + (nb + 1) * 512],
                                start=True, stop=True), reads=[kckv, "wukv"], writes=[("psb", 4 + nb)])
                            evac_xs(4 + nb, 512, off=nb * 512)
                        yield
                        kv3 = xs[:, 0:1024].rearrange("p (h c) -> p h c", h=4)
                        P.op("dve", lambda e, hh=hh: e.tensor_copy(out=vbS[s][:, hh * 4:(hh + 1) * 4, 0:128], in_=kv3[:, :, 128:256]),
                             reads=[kxs], writes=[("vbS", s)])
                        yield
                        yield from self.headnorm(P, kv3[:, :, 0:128], 4, 128, g_kn, tmp[:, 0:512].rearrange("p (h c) -> p h c", h=4), stat, kxs)
                        P.op("dve", lambda e: e.tensor_copy(out=xbf[:, 0:512].rearrange("p (h c) -> p h c", h=4), in_=kv3[:, :, 0:128]),
                             reads=[kxs], writes=[kxbf])
                        yield
                        tr([(xbf[:, h * 128:(h + 1) * 128], h * 128, 128) for h in range(4)])
                        evac7(knS[:, hh * 4:(hh + 1) * 4, tcol], 128, 0, 512, "knS", view=4)
                        yield
                    P.dma("pool", d["vb"][trows, :].rearrange("p (h c) -> p h c", h=8), vbS[s], reads=[("vbS", s)])
                    yield

            def fm_gen(fi, banks):
                c0, c1 = fm_tiles[fi]
                nc_ = c1 - c0
                wb = cnt["w"] % 3
                cnt["w"] += 1
                w3 = wblk[wb][:, 0:16 * nc_].rearrange("p (k c) -> p k c", k=16)
                P.dma("sp", w3, wfm_v[:, :, c0:c1], writes=wkeys(wb))
                yield
                for cc in range(nc_ // 128):
                    ci = fi * 4 + cc
                    pb = banks[cnt["pb"] % len(banks)]
                    cnt["pb"] += 1
                    for k in range(16):
                        P.op("pe", lambda e, pb=pb, k=k, cc=cc: e.matmul(
                            ps[:, pb * 512:(pb + 1) * 512], lhsT=w3[:, k, cc * 128:(cc + 1) * 128], rhs=hT[:, k, :],
                            start=(k == 0), stop=(k == 15)), reads=wkeys(wb) + hTk, writes=[("psb", pb)])
                        if k % 4 == 3:
                            yield
                    fb = cnt["fm"] % 2
                    cnt["fm"] += 1
                    P.op("act", lambda e, pb=pb, fb=fb: e.copy(out=fmS[fb], in_=ps[:, pb * 512:(pb + 1) * 512]),
                         reads=[("psb", pb)], writes=[("fmS", fb), ("psb", pb)])
                    nm, idx = fm_dest[ci]
                    P.dma("pool", d[nm][idx, :, rows], fmS[fb], reads=[("fmS", fb)])
                    yield

            for bi, (c0, c1) in enumerate(tm_blocks):
                nc_ = c1 - c0
                wb = cnt["w"] % 3
                cnt["w"] += 1
                w3 = wblk[wb][:, 0:16 * nc_].rearrange("p (k c) -> p k c", k=16)
                P.dma("sp", w3, wtm_v[:, :, c0:c1], writes=wkeys(wb))
                gens = [tm_chain(bi, s, w3, wb, nc_) for s in range(4)]
                if bi < 4:
                    gens.append(fm_gen(bi, (4, 5)))
                while gens:
                    for g_ in list(gens):
                        try:
                            next(g_)
                        except StopIteration:
                            gens.remove(g_)
            for _ in fm_gen(4, (0, 1, 2, 3)):
                pass
            P.dma("pool", d["qaT"][:, :, rows].rearrange("h p t -> p h t"), qaS, reads=["qaS"])
            P.dma("pool", d["kaT"][:, :, rows].rearrange("h p t -> p h t"), kaS, reads=["kaS"])
            P.dma("pool", d["qnT"][:, :, rows].rearrange("h p t -> p h t"), qnS, reads=["qnS"])
            P.dma("pool", d["knT"][:, :, rows].rearrange("h p t -> p h t"), knS, reads=["knS"])
            P.dma("pool", d["qrT"][:, :, rows].rearrange("h p t -> p h t"), qrS[0:64], reads=["qrS"])
            P.dma("pool", d["krT"][0, :, rows], krS[0:64], reads=["krS"])
        P.barrier()


    def attn_core(self, P, G, heads, s_mms, mask_rhs, v_ap, scale, pT, o_tm, identb, rs, rkeys):
        ps = self.ps
        nkb = 4 * G + 4
        seq = [(h, kb) for h in range(heads) for kb in range(nkb)]

        def emit_S(idx):
            h, kb = seq[idx]
            sb_ = idx % 2
            pS = ps[:, sb_ * 512:(sb_ + 1) * 512]
            mms = list(s_mms(h, kb))
            m = mask_rhs(kb)
            if m is not None:
                mms.append((identb, m))
            for i, (lt, rh) in enumerate(mms):
                P.op("pe", lambda e, pS=pS, lt=lt, rh=rh, i=i, n=len(mms): e.matmul(pS, lhsT=lt, rhs=rh, start=(i == 0), stop=(i == n - 1)),
                     reads=rkeys, writes=[("psb", sb_)])

        emit_S(0)
        for idx, (h, kb) in enumerate(seq):
            sb_ = idx % 2
            pS = ps[:, sb_ * 512:(sb_ + 1) * 512]
            if idx + 1 < len(seq):
                emit_S(idx + 1)
            p_ = pT[sb_]
            P.op("act", lambda e, p_=p_, pS=pS: e.activation(out=p_, in_=pS, func=AF.Exp, scale=scale),
                 reads=[("psb", sb_)], writes=[("pT", sb_), ("psb", sb_)])
            for i in range(4):
                last = 4 * G + i
                if kb > last:
                    continue
                P.op("pe", lambda e, i=i, p_=p_, h=h, kb=kb, last=last: e.matmul(
                    ps[:, (4 + i) * 512:(4 + i) * 512 + 129], lhsT=p_[:, i * 128:(i + 1) * 128], rhs=v_ap(h, kb),
                    start=(kb == 0), stop=(kb == last)), reads=[("pT", sb_)] + rkeys, writes=[("psb", 4 + i)])
            if kb == nkb - 1:
                for i in range(4):
                    acc = ps[:, (4 + i) * 512:(4 + i) * 512 + 129]
                    r_ = rs[:, i:i + 1]
                    P.op("dve", lambda e, acc=acc, r_=r_: e.reciprocal(out=r_, in_=acc[:, 128:129]), reads=[("psb", 4 + i)], writes=[("rs", i), ("psb", 4 + i)])
                    P.op("dve", lambda e, acc=acc, i=i, h=h, r_=r_: e.tensor_scalar(out=o_tm[:, i, h * 128:(h + 1) * 128], in0=acc[:, 0:128],
                                                                                     scalar1=r_, scalar2=None, op0=ALU.mult),
                         reads=[("psb", 4 + i), ("rs", i)], writes=["o_tm", ("psb", 4 + i)])

    def store_mixT(self, P, G, o_tm, nch, c0, stage, identb):
        psb = self.ps.bitcast(BF16)
        d = self.d
        rows = slice(G * 512, (G + 1) * 512)
        for c in range(nch):
            bank = 2 + c % 2
            for i in range(4):
                P.op("pe", lambda e, c=c, i=i, bank=bank: e.transpose(out=psb[:, bank * 1024 + i * 128:bank * 1024 + (i + 1) * 128],
                                                                       in_=o_tm[:, i, c * 128:(c + 1) * 128], identity=identb),
                     reads=["o_tm", "identb"], writes=[("psb", bank)])
            P.op("act", lambda e, c=c, bank=bank: e.copy(out=stage[:, c, :], in_=psb[:, bank * 1024:bank * 1024 + 512]),
                 reads=[("psb", bank)], writes=["stage", ("psb", bank)])
        P.dma("pool", d["mixT"][c0:c0 + nch, :, rows].rearrange("c p t -> p c t"), stage[:, 0:nch, :], reads=["stage"], writes=[])

    def phase_dsa(self, P, A, l):
        d = self.d
        ps = self.ps
        psb = self.ps.bitcast(BF16)
        A.off = self.base_off
        identb = A.take(128, BF16)
        kiT = A.take(S, BF16)
        kaT = A.take(4 * S, BF16).rearrange("p (h t) -> p h t", h=4)
        va = A.take(32 * 516, BF16).rearrange("p (k h c) -> p k h c", k=32, h=4)
        qi_g = A.take(8 * 512, BF16).rearrange("p (c t) -> p c t", c=8)
        qa_g = A.take(4 * 512, BF16).rearrange("p (h t) -> p h t", h=4)
        wi_g = A.take(64).rearrange("p (s h) -> p s h", s=4)
        scores = [A.take(S) for _ in range(2)]
        rl = [[A.take(512, BF16) for _ in range(3)] for _ in range(2)]
        dg = [A.take(16 * 128, BF16).rearrange("p (h q) -> p h q", h=16) for _ in range(2)]
        mks = [A.take(S, BF16) for _ in range(2)]
        mkT = A.take(32 * 512, BF16).rearrange("p (k t) -> p k t", k=32)
        pT = [A.take(512, BF16) for _ in range(2)]
        o_tm = A.take(4 * 512, BF16).rearrange("p (s c) -> p s c", s=4)
        stage = A.take(4 * 512, BF16).rearrange("p (c t) -> p c t", c=4)
        pw2 = A.take(NBIS + 1)
        Ws = [A.take(NBIS + 1) for _ in range(2)]
        W2s = [A.take(NBIS + 1) for _ in range(2)]
        sms = [A.take(16) for _ in range(2)]
        rs = A.take(4)
        P.op("dve", lambda e: e.tensor_copy(out=identb, in_=self.ident), reads=["ident"], writes=["identb"])
        P.dma("sp", kiT, d["kiT"][0], writes=["kiT"])
        P.dma("sp", kaT, d["kaT"].rearrange("h p t -> p h t"), writes=["kaT"])
        P.dma("sp", va, d["va"].rearrange("(k p) (h c) -> p k h c", p=128, h=4), writes=["va"])
        P.dma("sp", pw2, d["pow2"], writes=["pw2"])
        a_scale = 128.0 ** -0.5
        cnt = {"s": 0, "l": 0}
        for G in range(self.cfg.get("ng", NG)):
            rows = slice(G * 512, (G + 1) * 512)
            P.dma("sp", qi_g, d["qiT"][:, :, rows].rearrange("c p t -> p c t"), writes=["qi_g"])
            P.dma("sp", qa_g, d["qaT"][:, :, rows].rearrange("h p t -> p h t"), writes=["qa_g"])
            P.dma("sp", wi_g, d["wi"][rows, :].rearrange("(s p) h -> p s h", p=128), writes=["wi_g"])
            P.op("pool", lambda e, G=G: e.memset(mkT[:, 4 * G:4 * G + 4, :], BIGNEG), writes=[("mkT", i_) for i_ in range(4)])
            def qchain(i, sl):
                qt = 4 * G + i
                n2 = 128 * (qt + 1)
                n1 = n2 - 64
                score = scores[sl]
                sm = sms[sl]
                W, W2 = Ws[sl], W2s[sl]
                lo, w0, mid, cn, upd, m8 = sm[:, 0:1], sm[:, 2:3], sm[:, 3:4], sm[:, 4:5], sm[:, 5:6], sm[:, 8:16]
                ksc, ksm, kW = ("score", sl), ("sm", sl), ("W", sl)
                lb0 = 4 * sl
                sbank = lb0 + 2
                tbank = lb0 + 3
                dg_ = dg[sl]
                kdg = ("dg", sl)
                P.op("pool", lambda e: e.tensor_tensor(out=dg_, in0=identb.unsqueeze(1).to_broadcast([128, 16, 128]),
                                                       in1=wi_g[:, i, :].unsqueeze(2).to_broadcast([128, 16, 128]), op=ALU.mult),
                     reads=["identb", "wi_g"], writes=[kdg])
                yield
                for k5 in range((n2 + 511) // 512):
                    wd_ = min(512, n2 - k5 * 512)
                    pacc = ps[:, sbank * 512:sbank * 512 + wd_]

                    def logit(h, wd_=wd_, k5=k5):
                        lb = lb0 + h % 2
                        base = (h % 2) * 64
                        P.op("pe", lambda e, lb=lb, base=base, h=h: e.matmul(
                            ps[:, lb * 512:lb * 512 + wd_], lhsT=qi_g[base:base + 64, h // 2, i * 128:(i + 1) * 128],
                            rhs=kiT[base:base + 64, k5 * 512:k5 * 512 + wd_], start=True, stop=True),
                             reads=["qi_g", "kiT"], writes=[("psb", lb)])

                    logit(0)
                    for h in range(16):
                        if h + 1 < 16:
                            logit(h + 1)
                        lb = lb0 + h % 2
                        rb = (sl, h % 3)
                        r_ = rl[sl][h % 3][:, 0:wd_]
                        pin = ps[:, lb * 512:lb * 512 + wd_]
                        if h not in (1, 3, 6, 8, 10, 13, 15):
                            P.op("act", lambda e, r_=r_, pin=pin: e.activation(out=r_, in_=pin, func=AF.Relu),
                                 reads=[("psb", lb)], writes=[("rl", rb), ("psb", lb)])
                        else:
                            P.op("dve", lambda e, r_=r_, pin=pin: e.tensor_scalar(out=r_, in0=pin, scalar1=0.0, scalar2=None, op0=ALU.max),
                                 reads=[("psb", lb)], writes=[("rl", rb), ("psb", lb)])
                        P.op("pe", lambda e, pacc=pacc, h=h, r_=r_: e.matmul(pacc, lhsT=dg_[:, h, :], rhs=r_, start=(h == 0), stop=(h == 15)),
                             reads=[kdg, ("rl", rb)], writes=[("psb", sbank)])
                        yield
                    sc_blk = score[:, k5 * 512:k5 * 512 + wd_]
                    P.op("act", lambda e, sc_blk=sc_blk, pacc=pacc: e.copy(out=sc_blk, in_=pacc),
                         reads=[("psb", sbank)], writes=[ksc, ("psb", sbank)])
                    yield
                sc = score[:, 0:n2]
                P.op("dve", lambda e: e.tensor_reduce(out=lo, in_=sc, axis=AX.X, op=ALU.min), reads=[ksc], writes=[ksm])
                yield
                P.op("dve", lambda e: e.memset(score[0:64, n1:n2], BIGNEG), reads=[ksm], writes=[ksc])
                yield
                if qt >= 2:
                    P.op("dve", lambda e: e.max(out=m8, in_=sc), reads=[ksc], writes=[ksm])
                    yield
                    P.op("dve", lambda e: e.tensor_tensor(out=w0, in0=m8[:, 0:1], in1=lo, op=ALU.subtract), reads=[ksm], writes=[ksm])
                    yield
                    P.op("dve", lambda e: e.tensor_scalar(out=W, in0=pw2, scalar1=w0, scalar2=None, op0=ALU.mult), reads=[ksm, "pw2"], writes=[kW])
                    jk = mks[sl][:, 0:n2]
                    if sl == 0:
                        P.op("dve", lambda e: e.tensor_scalar(out=W2, in0=pw2, scalar1=w0, scalar2=2.0, op0=ALU.mult, op1=ALU.mult), reads=[ksm, "pw2"], writes=[kW])
                        yield
                        P.op("dve", lambda e: e.tensor_tensor(out=mid, in0=lo, in1=W[:, 0:1], op=ALU.add), reads=[ksm, kW], writes=[ksm])
                        yield
                        for k in range(NBIS):
                            P.op("dve", lambda e: e.tensor_scalar(out=jk, in0=sc, scalar1=mid, scalar2=None, op0=ALU.is_ge,
                                                                  op1=ALU.add, accum_out=cn), reads=[ksc, ksm], writes=[ksm, ("mk", sl)])
                            yield
                            P.op("dve", lambda e, k=k: e.tensor_scalar(out=upd, in0=cn, scalar1=255.5, scalar2=W2[:, k + 1:k + 2], op0=ALU.is_ge, op1=ALU.mult),
                                 reads=[ksm, kW], writes=[ksm])
                            yield
                            P.op("dve", lambda e, k=k: e.scalar_tensor_tensor(out=mid, in0=upd, scalar=W[:, k + 1:k + 2], in1=mid, op0=ALU.subtract, op1=ALU.add),
                                 reads=[ksm, kW], writes=[ksm])
                            yield
                        P.op("dve", lambda e: e.tensor_tensor(out=lo, in0=mid, in1=W[:, NBIS:NBIS + 1], op=ALU.subtract), reads=[ksm, kW], writes=[ksm])
                        yield
                    else:
                        P.op("dve", lambda e: e.tensor_scalar(out=W2, in0=pw2, scalar1=w0, scalar2=-2.0, op0=ALU.mult, op1=ALU.mult), reads=[ksm, "pw2"], writes=[kW])
                        yield
                        P.op("dve", lambda e: e.tensor_scalar(out=mid, in0=lo, scalar1=W[:, 0:1], scalar2=-1.0, op0=ALU.add, op1=ALU.mult), reads=[ksm, kW], writes=[ksm])
                        yield
                        cthr = float(511 - n2)
                        for k in range(NBIS):
                            P.op("act", lambda e: e.activation(out=jk, in_=sc, func=AF.Sign, bias=mid, scale=1.0, accum_out=cn),
                                 reads=[ksc, ksm], writes=[ksm, ("mk", sl)])
                            yield
                            P.op("dve", lambda e, k=k: e.tensor_scalar(out=upd, in0=cn, scalar1=cthr, scalar2=W2[:, k + 1:k + 2], op0=ALU.is_ge, op1=ALU.mult),
                                 reads=[ksm, kW], writes=[ksm])
                            yield
                            P.op("dve", lambda e, k=k: e.scalar_tensor_tensor(out=mid, in0=upd, scalar=W[:, k + 1:k + 2], in1=mid, op0=ALU.add, op1=ALU.add),
                                 reads=[ksm, kW], writes=[ksm])
                            yield
                        P.op("dve", lambda e: e.tensor_scalar(out=lo, in0=mid, scalar1=-1.0, scalar2=W[:, NBIS:NBIS + 1], op0=ALU.mult, op1=ALU.subtract),
                             reads=[ksm, kW], writes=[ksm])
                        yield
                mk_ = mks[sl]
                kmk = ("mk", sl)
                P.op("dve", lambda e: e.tensor_scalar(out=mk_[:, 0:n2], in0=sc, scalar1=lo, scalar2=1.0, op0=ALU.is_ge, op1=ALU.subtract),
                     reads=[ksc, ksm], writes=[kmk])
                yield
                for kb0 in range(0, qt + 1, 8):
                    nb_ = min(8, qt + 1 - kb0)
                    for j in range(nb_):
                        kb = kb0 + j
                        P.op("pe", lambda e, j=j, kb=kb: e.transpose(out=psb[:, tbank * 1024 + j * 128:tbank * 1024 + (j + 1) * 128],
                                                                      in_=mk_[:, kb * 128:(kb + 1) * 128], identity=identb),
                             reads=[kmk, "identb"], writes=[("psb", tbank)])
                    P.op("act", lambda e, nb_=nb_, kb0=kb0: e.activation(
                        out=mkT[:, kb0:kb0 + nb_, i * 128:(i + 1) * 128],
                        in_=psb[:, tbank * 1024:tbank * 1024 + nb_ * 128].rearrange("p (k t) -> p k t", k=nb_), func=AF.Copy, scale=-BIGNEG),
                         reads=[("psb", tbank)], writes=[("mkT", i), ("psb", tbank)])
                    yield

            for pair in ((0, 1), (2, 3)):
                gens = [qchain(pair[0], 0), qchain(pair[1], 1)]
                while gens:
                    for g_ in list(gens):
                        try:
                            next(g_)
                        except StopIteration:
                            gens.remove(g_)
            self.attn_core(P, G, 4,
                           lambda h, kb: [(kaT[:, h, kb * 128:(kb + 1) * 128], qa_g[:, h, :])],
                           lambda kb: mkT[:, kb, :],
                           lambda h, kb: va[:, kb, h, :],
                           a_scale, pT, o_tm, identb, rs, ["kaT", "qa_g", "va", "identb"] + [("mkT", i_) for i_ in range(4)])
            self.store_mixT(P, G, o_tm, 4, 0, stage, identb)
        P.barrier()

    def phase_mla(self, P, A, l):
        d = self.d
        A.off = self.base_off
        identb = A.take(128, BF16)
        knT = A.take(8 * S, BF16).rearrange("p (h t) -> p h t", h=8)
        krT = A.take(S, BF16)
        vb = A.take(32 * 8 * 129, BF16).rearrange("p (k h c) -> p k h c", k=32, h=8)
        qn_g = A.take(8 * 512, BF16).rearrange("p (h t) -> p h t", h=8)
        qr_g = A.take(8 * 512, BF16).rearrange("p (h t) -> p h t", h=8)
        cmf = A.take(4 * 512)
        cm = A.take(4 * 512, BF16).rearrange("p (j t) -> p j t", j=4)
        pT = [A.take(512, BF16) for _ in range(2)]
        o_tm = A.take(4 * 1024, BF16).rearrange("p (s c) -> p s c", s=4)
        stage = A.take(8 * 512, BF16).rearrange("p (c t) -> p c t", c=8)
        rs = A.take(4)
        P.op("dve", lambda e: e.tensor_copy(out=identb, in_=self.ident), reads=["ident"], writes=["identb"])
        P.dma("sp", knT, d["knT"].rearrange("h p t -> p h t"), writes=["knT"])
        P.dma("sp", krT[0:64], d["krT"][0], writes=["krT"])
        P.dma("sp", vb, d["vb"].rearrange("(k p) (h c) -> p k h c", p=128, h=8), writes=["vb"])
        P.dma("sp", cmf, d["cmaskB"], writes=["cmf"])
        P.op("dve", lambda e: e.tensor_copy(out=cm, in_=cmf.rearrange("p (j t) -> p j t", j=4)), reads=["cmf"], writes=["cm"])
        b_scale = 192.0 ** -0.5
        for G in range(self.cfg.get("ng", NG)):
            rows = slice(G * 512, (G + 1) * 512)
            P.dma("sp", qn_g, d["qnT"][:, :, rows].rearrange("h p t -> p h t"), writes=["qn_g"])
            P.dma("sp", qr_g[0:64], d["qrT"][:, :, rows].rearrange("h p t -> p h t"), writes=["qr_g"])
            self.attn_core(P, G, 8,
                           lambda h, kb: [(knT[:, h, kb * 128:(kb + 1) * 128], qn_g[:, h, :]),
                                          (krT[0:64, kb * 128:(kb + 1) * 128], qr_g[0:64, h, :])],
                           lambda kb, G=G: (cm[:, kb - 4 * G, :] if kb >= 4 * G else None),
                           lambda h, kb: vb[:, kb, h, :],
                           b_scale, pT, o_tm, identb, rs, ["knT", "krT", "qn_g", "qr_g", "cm", "vb", "identb"])
            self.store_mixT(P, G, o_tm, 8, 4, stage, identb)
        P.barrier()

    def phase_sb(self, P, A, l):
        d = self.d
        ps = self.ps
        A.off = self.base_off
        identb = A.take(128, BF16)
        kcT = A.take(4 * S, BF16).rearrange("p (h t) -> p h t", h=4)
        vc = A.take(32 * 512, BF16).rearrange("p (k c) -> p k c", k=32)
        qc_g = A.take(4 * 512, BF16).rearrange("p (h t) -> p h t", h=4)
        tri = A.take(4 * 512).rearrange("p (j t) -> p j t", j=4)
        trib = A.take(4 * 512, BF16).rearrange("p (j t) -> p j t", j=4)
        ustr = A.take(128)
        ones = A.take(128)
        eb = [A.take(512) for _ in range(3)]
        spb = [A.take(512) for _ in range(3)]
        t1b = [A.take(512) for _ in range(3)]
        acc = A.take(512)
        wT = [A.take(512, BF16) for _ in range(3)]
        o_tm = A.take(4 * 512, BF16).rearrange("p (s c) -> p s c", s=4)
        stage = A.take(4 * 512, BF16).rearrange("p (c t) -> p c t", c=4)
        P.op("dve", lambda e: e.tensor_copy(out=identb, in_=self.ident), reads=["ident"], writes=["identb"])
        P.dma("sp", kcT, d["kcT"].rearrange("h p t -> p h t"), writes=["kcT"])
        P.dma("sp", vc, d["vc"].rearrange("(k p) c -> p k c", p=128), writes=["vc"])
        P.dma("sp", tri, d["tri"].rearrange("p (j t) -> p j t", j=4), writes=["tri"])
        P.dma("sp", ustr, d["ustrict"], writes=["ustr"])
        P.op("dve", lambda e: e.tensor_copy(out=trib, in_=tri), reads=["tri"], writes=["trib"])
        P.op("dve", lambda e: e.memset(ones, 1.0), writes=["ones"])
        c_scale = 128.0 ** -0.5
        gi = 0
        for G in range(self.cfg.get("ng", NG)):
            rows = slice(G * 512, (G + 1) * 512)
            P.dma("sp", qc_g, d["qcT"][:, :, rows].rearrange("h p t -> p h t"), writes=["qc_g"])
            seq = [(h, kb) for h in range(4) for kb in range(4 * G + 3, -1, -1)]
            n = len(seq)

            def bufs(idx):
                g3 = (gi + idx) % 3
                g2 = (gi + idx) % 2
                return g3, g2

            def stageA(idx):
                h, kb = seq[idx]
                g3, g2 = bufs(idx)
                pz = ps[:, g2 * 512:(g2 + 1) * 512]
                e_, sp_, t1_ = eb[g3], spb[g3], t1b[g3]
                j = kb - 4 * G
                P.op("pe", lambda e, pz=pz, h=h, kb=kb: e.matmul(pz, lhsT=kcT[:, h, kb * 128:(kb + 1) * 128], rhs=qc_g[:, h, :], start=True, stop=True),
                     reads=["kcT", "qc_g"], writes=[("psb", g2)])
                P.op("act", lambda e, e_=e_, pz=pz: e.activation(out=e_, in_=pz, func=AF.Exp, scale=c_scale),
                     reads=[("psb", g2)], writes=[("eb", g3), ("psb", g2)])
                P.op("act", lambda e, e_=e_, sp_=sp_: e.activation(out=sp_, in_=e_, func=AF.Ln, bias=1.0),
                     reads=[("eb", g3)], writes=[("sp", g3)])
                P.op("dve", lambda e, t1_=t1_, pz=pz, sp_=sp_: e.scalar_tensor_tensor(out=t1_, in0=pz, scalar=c_scale, in1=sp_, op0=ALU.mult, op1=ALU.subtract),
                     reads=[("psb", g2), ("sp", g3)], writes=[("t1", g3), ("psb", g2)])
                if j >= 0:
                    P.op("pool", lambda e, sp_=sp_, j=j: e.tensor_tensor(out=sp_, in0=sp_, in1=tri[:, j, :], op=ALU.mult),
                         reads=[("sp", g3), "tri"], writes=[("sp", g3)])

            def stageB(idx):
                h, kb = seq[idx]
                g3, g2 = bufs(idx)
                first = (kb == 4 * G + 3)
                pl = ps[:, (2 + g2) * 512:(3 + g2) * 512]
                sp_, t1_, w_ = spb[g3], t1b[g3], wT[g3]
                j = kb - 4 * G
                P.op("pe", lambda e, pl=pl, sp_=sp_, first=first: e.matmul(pl, lhsT=ustr, rhs=sp_, start=True, stop=first),
                     reads=["ustr", ("sp", g3)], writes=[("psb", 2 + g2)])
                if not first:
                    P.op("pe", lambda e, pl=pl: e.matmul(pl, lhsT=ones, rhs=acc, start=False, stop=True),
                         reads=["ones", "acc"], writes=[("psb", 2 + g2)])
                P.op("dve", lambda e, t1_=t1_, pl=pl: e.tensor_tensor(out=t1_, in0=t1_, in1=pl, op=ALU.subtract),
                     reads=[("t1", g3), ("psb", 2 + g2)], writes=[("t1", g3), ("psb", 2 + g2)])
                if first:
                    P.op("pool", lambda e, sp_=sp_: e.tensor_copy(out=acc, in_=sp_), reads=[("sp", g3)], writes=["acc"])
                else:
                    P.op("pool", lambda e, sp_=sp_: e.tensor_tensor(out=acc, in0=acc, in1=sp_, op=ALU.add), reads=[("sp", g3), "acc"], writes=["acc"])
                P.op("act", lambda e, w_=w_, t1_=t1_: e.activation(out=w_, in_=t1_, func=AF.Exp), reads=[("t1", g3)], writes=[("wT", g3)])
                if j >= 0:
                    P.op("pool", lambda e, w_=w_, j=j: e.tensor_tensor(out=w_, in0=w_, in1=trib[:, j, :], op=ALU.mult),
                         reads=[("wT", g3), "trib"], writes=[("wT", g3)])

            def stageC(idx):
                h, kb = seq[idx]
                g3, g2 = bufs(idx)
                w_ = wT[g3]
                for i in range(4):
                    if kb > 4 * G + i:
                        continue
                    P.op("pe", lambda e, i=i, w_=w_, kb=kb, h=h, G=G: e.matmul(
                        ps[:, (4 + i) * 512:(4 + i) * 512 + 128], lhsT=w_[:, i * 128:(i + 1) * 128], rhs=vc[:, kb, h * 128:(h + 1) * 128],
                        start=(kb == 4 * G + i), stop=(kb == 0)), reads=[("wT", g3), "vc"], writes=[("psb", 4 + i)])
                if kb == 0:
                    for i in range(4):
                        P.op("act", lambda e, i=i, h=h: e.copy(out=o_tm[:, i, h * 128:(h + 1) * 128], in_=ps[:, (4 + i) * 512:(4 + i) * 512 + 128]),
                             reads=[("psb", 4 + i)], writes=["o_tm", ("psb", 4 + i)])

            for it in range(n + 2):
                if it < n:
                    stageA(it)
                if 0 <= it - 1 < n:
                    stageB(it - 1)
                if 0 <= it - 2 < n:
                    stageC(it - 2)
            gi += n
            self.store_mixT(P, G, o_tm, 4, 12, stage, identb)
        P.barrier()

    def phase_outproj(self, P, A, l, src):
        d = self.d
        ps = self.ps
        A.off = self.base_off
        _, _, gb = self.sublayer_setup(P, A, l, 1, 1.0)
        wo = A.take(16 * D, BF16).rearrange("p (k n) -> p k n", k=16)
        mx = [A.take(16 * 512, BF16).rearrange("p (k t) -> p k t", k=16) for _ in range(2)]
        xr = [A.take(512) for _ in range(8)]
        y = d["y"]
        P.dma("sp", wo, d["wout_b"][l].rearrange("p (k n) -> p k n", k=16), writes=["wo"])
        pi = 0
        for G in range(self.cfg.get("ng", NG)):
            rows = slice(G * 512, (G + 1) * 512)
            m_ = mx[G % 2]
            P.dma("sp", m_, d["mixT"][:, :, rows].rearrange("c p t -> p c t"), writes=[("mx", G % 2)])
            for s in range(4):
                t = G * 4 + s
                for n in range(4):
                    pb = pi % 8
                    xp = xr[pi % 8]
                    kxp = ("xrb", pi % 8)
                    pi += 1
                    P.dma("pool", xp, src[t * 128:(t + 1) * 128, n * 512:(n + 1) * 512], reads=[("xr", t, n)], writes=[kxp])
                    pd = ps[:, pb * 512:(pb + 1) * 512]
                    for k in range(16):
                        P.op("pe", lambda e, pd=pd, m_=m_, k=k, s=s, n=n: e.matmul(pd, lhsT=m_[:, k, s * 128:(s + 1) * 128], rhs=wo[:, k, n * 512:(n + 1) * 512],
                                                                                    start=(k == 0), stop=(k == 15)),
                             reads=[("mx", G % 2), "wo"], writes=[("psb", pb)])
                    P.op("dve", lambda e, pd=pd, n=n: e.tensor_tensor(out=pd, in0=pd, in1=gb[:, n * 512:(n + 1) * 512], op=ALU.mult),
                         reads=[("psb", pb), "gb"], writes=[("psb", pb)])
                    P.op("dve", lambda e, pd=pd, xp=xp: e.tensor_tensor(out=xp, in0=pd, in1=xp, op=ALU.add),
                         reads=[("psb", pb), kxp], writes=[kxp, ("psb", pb)])
                    P.dma("pool", y[t * 128:(t + 1) * 128, n * 512:(n + 1) * 512], xp, reads=[kxp], writes=[("xr", t, n)])
        P.barrier()

    def phase_ffn(self, P, A, l, f, src):
        d = self.d
        ps = self.ps
        A.off = self.base_off
        sub = 0 if f == 0 else 2
        shc, gsc, gb = self.sublayer_setup(P, A, l, sub, 0.5)
        xb = [A.take(D) for _ in range(4)]
        xr = [A.take(512) for _ in range(8)]
        junk = A.take(D, BF16)
        st = [A.take(4) for _ in range(2)]
        hT = A.take(16 * 512, BF16).rearrange("p (k t) -> p k t", k=16)
        actT = A.take(44 * 512, BF16).rearrange("p (c t) -> p c t", c=44)
        wgb = [A.take(16 * 256, BF16).rearrange("p (k c) -> p k c", k=16) for _ in range(2)]
        wub = [A.take(16 * 256, BF16).rearrange("p (k c) -> p k c", k=16) for _ in range(2)]
        wdb = [A.take(22 * 512, BF16).rearrange("p (c n) -> p c n", c=22) for _ in range(2)]
        sg = [A.take(512) for _ in range(2)]
        wt0 = (l * 2 + f) * 22
        wd0 = (l * 2 + f) * 8
        hTk = [("hT", s) for s in range(4)]
        y = d["y"]
        pi = 0
        ng = self.cfg.get("ng", NG)
        stop = self.cfg.get("ffn_stop", 9)
        if stop <= 1:
            P.barrier()
            return
        self.norm_loads(P, src, 0, xb)
        self.norm_group(P, src, 0, hT, shc, gsc, xb, junk, st)
        if stop <= 2:
            P.barrier()
            return
        for g in range(ng):
            for jb in range(22):
                b = jb % 2
                P.dma("sp", wgb[b], d["wg_b"][wt0 + jb].rearrange("p (k c) -> p k c", k=16), writes=[("wg", b)])
                P.dma("sp", wub[b], d["wu_b"][wt0 + jb].rearrange("p (k c) -> p k c", k=16), writes=[("wu", b)])
                for cc in range(2):
                    c = jb * 2 + cc
                    pb = c % 2
                    pg = ps[:, (0 + pb) * 512:(1 + pb) * 512]
                    pu = ps[:, (2 + pb) * 512:(3 + pb) * 512]
                    for k in range(16):
                        P.op("pe", lambda e, pg=pg, b=b, k=k, cc=cc: e.matmul(pg, lhsT=wgb[b][:, k, cc * 128:(cc + 1) * 128], rhs=hT[:, k, :],
                                                                              start=(k == 0), stop=(k == 15)),
                             reads=[("wg", b)] + hTk, writes=[("psb", pb)])
                    for k in range(16):
                        P.op("pe", lambda e, pu=pu, b=b, k=k, cc=cc: e.matmul(pu, lhsT=wub[b][:, k, cc * 128:(cc + 1) * 128], rhs=hT[:, k, :],
                                                                              start=(k == 0), stop=(k == 15)),
                             reads=[("wu", b)] + hTk, writes=[("psb", 2 + pb)])
                    sgt = sg[c % 2]
                    P.op("act", lambda e, sgt=sgt, pg=pg: e.activation(out=sgt, in_=pg, func=AF.Silu),
                         reads=[("psb", pb)], writes=[("sg", c % 2), ("psb", pb)])
                    P.op("dve", lambda e, sgt=sgt, pu=pu, c=c: e.tensor_tensor(out=actT[:, c, :], in0=pu, in1=sgt, op=ALU.mult),
                         reads=[("psb", 2 + pb), ("sg", c % 2)], writes=[("actT", c), ("psb", 2 + pb)])
            if stop <= 3:
                continue
            if g + 1 < ng:
                self.norm_loads(P, src, g + 1, xb)
            for n in range(4):
                if n == 2 and g + 1 < ng:
                    self.norm_group(P, src, g + 1, hT, shc, gsc, xb, junk, st)
                xps = []
                for s in range(4):
                    t = g * 4 + s
                    xp = xr[pi % 8]
                    kxp = ("xrb", pi % 8)
                    pi += 1
                    xps.append((xp, kxp))
                    P.dma("pool", xp, src[t * 128:(t + 1) * 128, n * 512:(n + 1) * 512], reads=[("xr", t, n)], writes=[kxp])
                for hf in range(2):
                    wi = n * 2 + hf
                    b = wi % 2
                    P.dma("sp", wdb[b], d["wd_b"][wd0 + wi].rearrange("p (c n) -> p c n", c=22), writes=[("wd", b)])
                    for s in range(4):
                        pd = ps[:, (4 + s) * 512:(5 + s) * 512]
                        for c in range(22):
                            ca = hf * 22 + c
                            P.op("pe", lambda e, pd=pd, b=b, c=c, ca=ca, s=s, hf=hf: e.matmul(
                                pd, lhsT=actT[:, ca, s * 128:(s + 1) * 128], rhs=wdb[b][:, c, :],
                                start=(hf == 0 and c == 0), stop=(hf == 1 and c == 21)),
                                 reads=[("wd", b), ("actT", ca)], writes=[("psb", 4 + s)])
                for s in range(4):
                    t = g * 4 + s
                    pd = ps[:, (4 + s) * 512:(5 + s) * 512]
                    xp, kxp = xps[s]
                    P.op("dve", lambda e, pd=pd, n=n: e.tensor_tensor(out=pd, in0=pd, in1=gb[:, n * 512:(n + 1) * 512], op=ALU.mult),
                         reads=[("psb", 4 + s), "gb"], writes=[("psb", 4 + s)])
                    P.op("dve", lambda e, pd=pd, xp=xp: e.tensor_tensor(out=xp, in0=pd, in1=xp, op=ALU.add),
                         reads=[("psb", 4 + s), kxp], writes=[kxp, ("psb", 4 + s)])
                    P.dma("pool", y[t * 128:(t + 1) * 128, n * 512:(n + 1) * 512], xp, reads=[kxp], writes=[("xr", t, n)])
        P.barrier()

    def build(self):
        nc = self.nc
        self.declare()
        d = self.d
        with ExitStack() as es:
            NB = 211968
            big = es.enter_context(nc.sbuf_tensor("big", [128, NB // 4], F32))
            self.ps = es.enter_context(nc.psum_tensor("ps", [128, 4096], F32))
            A = Alloc(big, NB)
            self.A = A
            self._alloc_offsets = A.registry
            P = Prog(nc)
            self.ident = A.take(128)
            self.gcol = A.take(NL * 3 * 16)
            P.dma("sp", self.ident, d["ident"], writes=["ident"])
            P.dma("sp", self.gcol, d["gcol"], writes=["gcol"])
            self.base_off = A.off
            P.barrier()
            if self.cfg.get("do_adaln", True):
                self.phase_adaln(P, A)
            if self.cfg.get("do_prep", True):
                self.phase_prep(P, A)
            src = d["x"]
            for l in range(self.cfg["nl"] if self.cfg.get("do_ffn", True) else 0):
                if self.cfg["ffn"][0]:
                    self.phase_ffn(P, A, l, 0, src)
                    src = d["y"]
                if self.cfg.get("mix", False):
                    mp = self.cfg.get("mixparts", "pabco")
                    if "p" in mp:
                        self.phase_mixproj(P, A, l, src)
                    if "a" in mp:
                        self.phase_dsa(P, A, l)
                    if "b" in mp:
                        self.phase_mla(P, A, l)
                    if "c" in mp:
                        self.phase_sb(P, A, l)
                    if "o" in mp:
                        self.phase_outproj(P, A, l, src)
                        src = d["y"]
                if self.cfg["ffn"][1]:
                    self.phase_ffn(P, A, l, 1, src)
                    src = d["y"]
            P.barrier()
            P.emit(es)
            self.P = P
        return nc


def host_layout(inp):
    sh = {}
    w_ada = np.asarray(inp["w_ada"], dtype=np.float32)
    sh["wada"] = np.ascontiguousarray(
        w_ada.reshape(NL, 16, 128, 36, 512).transpose(0, 3, 2, 1, 4)).reshape(NL * 36, 128, 16 * 512)
    sh["bada"] = np.ascontiguousarray(np.asarray(inp["b_ada"], dtype=np.float32))
    g = np.stack([np.asarray(inp[k], dtype=np.float32) for k in ("g_ffn1", "g_mix", "g_ffn2")], axis=1)
    sh["gcol"] = np.ascontiguousarray(g.reshape(NL, 3, 16, 128).transpose(3, 0, 1, 2)).reshape(128, NL * 3 * 16)

    def gu(a, b):
        w = np.stack([np.asarray(inp[a], dtype=np.float32), np.asarray(inp[b], dtype=np.float32)], axis=1)
        w = w.reshape(NL, 2, 16, 128, 22, 256).transpose(0, 1, 4, 3, 2, 5)
        return np.ascontiguousarray(w).reshape(NL * 2 * 22, 128, 16 * 256)

    sh["wg"] = gu("w1_gate", "w2_gate")
    sh["wu"] = gu("w1_up", "w2_up")
    w = np.stack([np.asarray(inp["w1_down"], dtype=np.float32), np.asarray(inp["w2_down"], dtype=np.float32)], axis=1)
    w = w.reshape(NL, 2, 2, 22, 128, 4, 512).transpose(0, 1, 5, 2, 4, 3, 6)
    sh["wd"] = np.ascontiguousarray(w).reshape(NL * 2 * 8, 128, 22 * 512)
    sh["ident"] = np.eye(128, dtype=np.float32)
    w_in = np.asarray(inp["w_in"], dtype=np.float32)
    sp = np.cumsum([0, 512, 512, 512, 1024, 64, 16, 448, 128, 64, 512, 512, 512])
    qa, ka, va, qi, ki, wi, cq, ckv, kr, qc, kc, vc = [np.arange(sp[i], sp[i + 1]) for i in range(12)]
    tm = np.concatenate([qa, ka, va, vc, cq, kr, ckv, wi])
    fm = np.concatenate([qi, ki, ki, qc, kc])
    sh["wtm"] = np.ascontiguousarray(w_in[:, :, tm].reshape(NL, 16, 128, TM_COLS).transpose(0, 2, 1, 3)).reshape(NL, 128, 16 * TM_COLS)
    sh["wfm"] = np.ascontiguousarray(w_in[:, :, fm].reshape(NL, 16, 128, FM_COLS).transpose(0, 2, 1, 3)).reshape(NL, 128, 16 * FM_COLS)
    wuq = np.zeros((NL, 512, 1536), np.float32)
    wuq[:, :448] = np.asarray(inp["w_uq"], dtype=np.float32)
    sh["wuq"] = np.ascontiguousarray(wuq.reshape(NL, 4, 128, 1536).transpose(0, 2, 1, 3)).reshape(NL, 128, 4 * 1536)
    sh["wukv"] = np.ascontiguousarray(np.asarray(inp["w_ukv"], dtype=np.float32))
    sh["wout"] = np.ascontiguousarray(np.asarray(inp["w_out"], dtype=np.float32).reshape(NL, 16, 128, D).transpose(0, 2, 1, 3)).reshape(NL, 128, 16 * D)
    gt = np.concatenate([np.asarray(inp[k], dtype=np.float32) for k in
                         ("g_qa", "g_ka", "g_cq", "g_ckv", "g_q_nope", "g_k_nope", "g_q_rope", "g_k_rope")], axis=1)
    sh["gtm"] = np.ascontiguousarray(np.broadcast_to(gt.reshape(1, NL * GT), (128, NL * GT)))
    def tables(dim):
        inv = (1.0 / (np.float32(500000.0) ** (np.arange(0, dim, 2, dtype=np.float32) / np.float32(dim)))).astype(np.float32)
        ang = (np.arange(S, dtype=np.float32)[:, None] * inv[None, :]).astype(np.float32)
        return np.cos(ang).astype(np.float32), np.sin(ang).astype(np.float32)
    ca, sa = tables(32)
    cm_, sm_ = tables(64)
    sh["ropeA"] = np.ascontiguousarray(np.concatenate([np.tile(ca, (1, 4)), np.tile(sa, (1, 4))], axis=1))
    sh["ropeQ"] = np.ascontiguousarray(np.concatenate([np.tile(cm_, (1, 8)), np.tile(sm_, (1, 8))], axis=1))
    sh["ropeK"] = np.ascontiguousarray(np.concatenate([cm_, sm_], axis=1))
    cmask = np.zeros((128, 4, 4, 128), np.float32)
    tri = np.zeros((128, 4, 4, 128), np.float32)
    kk = np.arange(128)[:, None]
    qq = np.arange(128)[None, :]
    for j in range(4):
        for i in range(4):
            if i < j:
                cmask[:, j, i, :] = BIGNEG
            elif i == j:
                cmask[:, j, i, :] = np.where((kk // 64) <= (qq // 64), 0.0, BIGNEG)
                tri[:, j, i, :] = (kk < qq)
            else:
                tri[:, j, i, :] = 1.0
    sh["cmaskB"] = cmask.reshape(128, 2048)
    sh["tri"] = tri.reshape(128, 2048)
    sh["ustrict"] = (np.arange(128)[:, None] > np.arange(128)[None, :]).astype(np.float32)
    sh["pow2"] = np.ascontiguousarray(np.broadcast_to((0.5 ** np.arange(1, NBIS + 2)).astype(np.float32)[None, :], (128, NBIS + 1)))
    return sh


def core_inputs(inp, sh, b):
    m = dict(sh)
    m["x"] = np.ascontiguousarray(np.asarray(inp["x"][b], dtype=np.float32))
    m["cT"] = np.ascontiguousarray(np.asarray(inp["c"][b], dtype=np.float32).reshape(16, 128).T)
    return m


DEFAULT_CFG = {"nl": 2, "ffn": (True, True), "mix": True}


def kernel(**inputs):
    nc = bass.Bass("TRN2", target_bir_lowering=False)
    bld = Builder(nc, DEFAULT_CFG)
    bld.build()
    sh = host_layout(inputs)
    in_maps = [core_inputs(inputs, sh, b) for b in range(8)]
    res = run_bass_kernel_spmd(nc, in_maps, core_ids=list(range(8)))
    return np.stack([np.asarray(r["y"], dtype=np.float32) for r in res.results], axis=0)
```

```python
import numpy as np
from contextlib import ExitStack
import concourse.bass as bass
import concourse.mybir as mybir
from concourse.bass_utils import run_bass_kernel_spmd

F32 = mybir.dt.float32
BF16 = mybir.dt.bfloat16
AF = mybir.ActivationFunctionType
ALU = mybir.AluOpType
AX = mybir.AxisListType

S = 4096
D = 2048
DFF = 5632
NL = 2
NT = S // 128
NG = S // 512
EPS = 1e-6
NIN = 4816
TM_COLS = 2704
FM_COLS = 2176
BIGNEG = -30000.0
GT = 1216
NBIS = 20

ENGS = ("pe", "act", "dve", "pool", "sp")
CHUNK = 12000
NPOOL = 40


class Op:
    __slots__ = ("eng", "fn", "deps", "sig", "seq", "dma", "dsem", "dval", "name")

    def __init__(self, eng, fn, dma=False, name=""):
        self.eng = eng
        self.fn = fn
        self.deps = []
        self.sig = False
        self.seq = 0
        self.dma = dma
        self.dsem = -1
        self.dval = 0
        self.name = name


class Prog:
    def __init__(self, nc):
        self.nc = nc
        self.q = {e: [] for e in ENGS}
        self.last_w = {}
        self.readers = {}
        self.dma_cnt = [0] * NPOOL
        self.dma_rr = 0
        self.live_dma = []
        self.last_real = {}

    def _add_dep(self, op, d, raw):
        if d is None or d is op:
            return
        if (not d.dma) and (not op.dma) and d.eng == op.eng and not raw:
            return
        if d in op.deps:
            return
        op.deps.append(d)
        d.sig = True

    def _track(self, op, reads, writes):
        for r in reads:
            self._add_dep(op, self.last_w.get(r), True)
        for w in writes:
            self._add_dep(op, self.last_w.get(w), False)
            for rd in self.readers.get(w, ()):
                self._add_dep(op, rd, False)
        for r in reads:
            lst = self.readers.setdefault(r, [])
            if not op.dma:
                lst[:] = [o for o in lst if o.dma or o.eng != op.eng]
            lst.append(op)
        for w in writes:
            self.last_w[w] = op
            self.readers[w] = []

    def op(self, eng, fn, reads=(), writes=(), name=""):
        o = Op(eng, fn, name=name)
        self._track(o, reads, writes)
        self.q[eng].append(o)
        self.last_real[eng] = o
        return o

    def dma(self, queue, out, in_, reads=(), writes=(), **kw):
        o = Op(queue, lambda e: e.dma_start(out=out, in_=in_, **kw), dma=True)
        i = self.dma_rr
        self.dma_rr = (self.dma_rr + 1) % NPOOL
        self.dma_cnt[i] += 1
        o.dsem = i
        o.dval = 16 * self.dma_cnt[i]
        self._track(o, reads, writes)
        self.q[queue].append(o)
        self.live_dma.append(o)
        return o

    def barrier(self):
        lasts = list(self.last_real.values())
        dmas = list(self.live_dma)
        self.live_dma = []
        for e in ENGS:
            b = Op(e, None, name="barrier")
            for d in lasts:
                if d.eng != e:
                    b.deps.append(d)
                    d.sig = True
            for d in dmas:
                b.deps.append(d)
            self.q[e].append(b)
        self.last_w = {}
        self.readers = {}

    def emit(self, es):
        nc = self.nc
        nsig = {}
        for e in ENGS:
            n = 0
            for o in self.q[e]:
                if o.sig and not o.dma:
                    n += 1
                    o.seq = n
            nsig[e] = n
        sems = {}
        for e in ENGS:
            k = (nsig[e] + CHUNK - 1) // CHUNK
            sems[e] = [es.enter_context(nc.semaphore(f"s_{e}{i}")) for i in range(k)]
        dsems = [es.enter_context(nc.semaphore(f"s_dma{i}")) for i in range(NPOOL)]
        self.nsig = nsig

        def run(ename):
            def body(eng):
                seen_eng = {}
                seen_dma = {}
                for o in self.q[ename]:
                    for d in o.deps:
                        if d.dma:
                            if seen_dma.get(d.dsem, 0) >= d.dval:
                                continue
                            eng.wait_ge(dsems[d.dsem], d.dval)
                            seen_dma[d.dsem] = d.dval
                        else:
                            if seen_eng.get(d.eng, 0) >= d.seq:
                                continue
                            c = (d.seq - 1) // CHUNK
                            eng.wait_ge(sems[d.eng][c], d.seq - c * CHUNK)
                            seen_eng[d.eng] = d.seq
                    if o.fn is None:
                        continue
                    if o.dma:
                        prev = o.dval - 16
                        if prev > 0 and seen_dma.get(o.dsem, 0) < prev:
                            eng.wait_ge(dsems[o.dsem], prev)
                            seen_dma[o.dsem] = prev
                        ins = o.fn(eng)
                        ins.then_inc(dsems[o.dsem], 16)
                    else:
                        ins = o.fn(eng)
                        if o.sig:
                            c = (o.seq - 1) // CHUNK
                            ins.then_inc(sems[ename][c], 1)
            return body

        with nc.Block() as block:
            block.tensor(run("pe"))
            block.scalar(run("act"))
            block.vector(run("dve"))
            block.gpsimd(run("pool"))
            block.sync(run("sp"))


class Alloc:
    def __init__(self, big, nbytes):
        self.big = big
        self.nbytes = nbytes
        self.off = 0
        self.registry = {}
        self.keep = []

    def take(self, cols, dtype=F32, parts=128):
        esz = 4 if dtype == F32 else 2
        nb = (cols * esz + 63) // 64 * 64
        assert self.off + nb <= self.nbytes, (self.off, nb, self.nbytes)
        v = self.big[0:parts, self.off // 4:(self.off + nb) // 4]
        off0 = self.off
        self.off += nb
        if dtype != F32:
            v = v.bitcast(dtype)
        r = v[:, 0:cols]
        if self.registry is not None:
            self.registry[id(r)] = off0
            self.keep.append(r)
        return r


class Builder:
    def __init__(self, nc, cfg):
        self.nc = nc
        self.cfg = cfg
        self.uid = 0

    def big_flat(self, ap0, ncols):
        off = self._alloc_offsets[id(ap0)]
        return self.A.big[:, off // 4:off // 4 + ncols]

    def dram_in(self, name, shape, dt=F32):
        return self.nc.dram_tensor(name, list(shape), dt, kind="ExternalInput").ap()

    def dram_scr(self, name, shape, dt):
        kind = "ExternalOutput" if name in self.cfg.get("dbg", ()) else "Internal"
        return self.nc.dram_tensor(name, list(shape), dt, kind=kind).ap()

    def declare(self):
        nl = self.cfg["nl"]
        d = {}
        d["x"] = self.dram_in("x", [S, D])
        d["cT"] = self.dram_in("cT", [128, 16])
        d["wada"] = self.dram_in("wada", [NL * 36, 128, 16 * 512])
        d["bada"] = self.dram_in("bada", [NL, 9 * D])
        d["gcol"] = self.dram_in("gcol", [128, NL * 3 * 16])
        d["wg"] = self.dram_in("wg", [NL * 2 * 22, 128, 16 * 256])
        d["wu"] = self.dram_in("wu", [NL * 2 * 22, 128, 16 * 256])
        d["wd"] = self.dram_in("wd", [NL * 2 * 8, 128, 22 * 512])
        d["ident"] = self.dram_in("ident", [128, 128])
        d["wtm"] = self.dram_in("wtm", [NL, 128, 16 * TM_COLS])
        d["wfm"] = self.dram_in("wfm", [NL, 128, 16 * FM_COLS])
        d["wuq"] = self.dram_in("wuq", [NL, 128, 4 * 1536])
        d["wukv"] = self.dram_in("wukv", [NL, 128, 2048])
        d["wout"] = self.dram_in("wout", [NL, 128, 16 * D])
        d["gtm"] = self.dram_in("gtm", [128, NL * GT])
        d["ropeA"] = self.dram_in("ropeA", [S, 128])
        d["ropeQ"] = self.dram_in("ropeQ", [S, 512])
        d["ropeK"] = self.dram_in("ropeK", [S, 64])
        d["cmaskB"] = self.dram_in("cmaskB", [128, 4 * 512])
        d["tri"] = self.dram_in("tri", [128, 4 * 512])
        d["ustrict"] = self.dram_in("ustrict", [128, 128])
        d["pow2"] = self.dram_in("pow2", [128, NBIS + 1])
        d["y"] = self.nc.dram_tensor("y", [S, D], F32, kind="ExternalOutput").ap()
        d["modv"] = self.dram_scr("modv", [NL, 9 * D], F32)
        d["wg_b"] = self.dram_scr("wg_b", [NL * 2 * 22, 128, 16 * 256], BF16)
        d["wu_b"] = self.dram_scr("wu_b", [NL * 2 * 22, 128, 16 * 256], BF16)
        d["wd_b"] = self.dram_scr("wd_b", [NL * 2 * 8, 128, 22 * 512], BF16)
        d["wtm_b"] = self.dram_scr("wtm_b", [NL, 128, 16 * TM_COLS], BF16)
        d["wfm_b"] = self.dram_scr("wfm_b", [NL, 128, 16 * FM_COLS], BF16)
        d["wuq_b"] = self.dram_scr("wuq_b", [NL, 128, 4 * 1536], BF16)
        d["wukv_b"] = self.dram_scr("wukv_b", [NL, 128, 2048], BF16)
        d["wout_b"] = self.dram_scr("wout_b", [NL, 128, 16 * D], BF16)
        for nm, shp in (("qaT", [4, 128, S]), ("kaT", [4, 128, S]), ("qiT", [8, 128, S]), ("kiT", [1, 128, S]),
                        ("qnT", [8, 128, S]), ("qrT", [8, 64, S]), ("knT", [8, 128, S]), ("krT", [1, 64, S]),
                        ("qcT", [4, 128, S]), ("kcT", [4, 128, S]), ("mixT", [16, 128, S]),
                        ("va", [S, 4 * 129]), ("vb", [S, 8 * 129]), ("vc", [S, 512])):
            d[nm] = self.dram_scr(nm + "_d", shp, BF16)
        d["wi"] = self.dram_scr("wi_d", [S, 16], F32)
        self.d = d

    def cast_tiles(self, P, src, dst, tiles, cols, bufs, step=4096):
        i = self.uid
        for t in tiles:
            for c0 in range(0, cols, step):
                c1 = min(cols, c0 + step)
                fb, bb = bufs[i % len(bufs)]
                kf = ("castf", i % len(bufs))
                kb = ("castb", i % len(bufs))
                P.dma("sp", fb[:, 0:c1 - c0], src[t, :, c0:c1], writes=[kf])
                o_, i_ = bb[:, 0:c1 - c0], fb[:, 0:c1 - c0]
                if i % 2 == 0:
                    P.op("dve", lambda e, o_=o_, i_=i_: e.tensor_copy(out=o_, in_=i_), reads=[kf], writes=[kb])
                else:
                    P.op("act", lambda e, o_=o_, i_=i_: e.copy(out=o_, in_=i_), reads=[kf], writes=[kb])
                P.dma("pool", dst[t, :, c0:c1], bb[:, 0:c1 - c0], reads=[kb], writes=[])
                i += 1
        self.uid = i

    def phase_prep(self, P, A):
        d = self.d
        nl = self.cfg["nl"]
        A.off = self.base_off
        bufs = [(A.take(4096, F32), A.take(4096, BF16)) for _ in range(4)]
        for l in range(nl):
            for f in range(2):
                if not self.cfg["ffn"][f]:
                    continue
                t0 = (l * 2 + f) * 22
                self.cast_tiles(P, d["wg"], d["wg_b"], range(t0, t0 + 22), 16 * 256, bufs)
                self.cast_tiles(P, d["wu"], d["wu_b"], range(t0, t0 + 22), 16 * 256, bufs)
                t0 = (l * 2 + f) * 8
                self.cast_tiles(P, d["wd"], d["wd_b"], range(t0, t0 + 8), 22 * 512, bufs, step=2816)
        if self.cfg.get("mix", False):
            for l in range(nl):
                for nm, cols in (("wtm", 16 * TM_COLS), ("wfm", 16 * FM_COLS), ("wuq", 4 * 1536), ("wukv", 2048), ("wout", 16 * D)):
                    self.cast_tiles(P, d[nm], d[nm + "_b"], [l], cols, bufs)
        P.barrier()

    def phase_adaln(self, P, A):
        d = self.d
        nl = self.cfg["nl"]
        ps = self.ps
        A.off = self.base_off
        cact = A.take(16)
        wt = [A.take(16 * 512) for _ in range(2)]
        bt = [A.take(512, parts=1) for _ in range(2)]
        rt = [A.take(512, parts=1) for _ in range(2)]
        P.dma("sp", cact, d["cT"], writes=["cact"])
        P.op("act", lambda e: e.activation(out=cact, in_=cact, func=AF.Silu), reads=["cact"], writes=["cact"])
        for l in range(nl):
            for j in range(36):
                i = l * 36 + j
                w = wt[i % 2]
                w3 = w.rearrange("p (k c) -> p k c", k=16)
                P.dma("sp", w, d["wada"][i], writes=[("wt", i % 2)])
                P.dma("pool", bt[i % 2], d["bada"][l:l + 1, j * 512:(j + 1) * 512], writes=[("bt", i % 2)])
                pso = ps[0:1, (i % 2) * 512:(i % 2) * 512 + 512]
                for k in range(16):
                    P.op("pe", lambda e, k=k, pso=pso, w3=w3: e.matmul(pso, lhsT=cact[:, k:k + 1], rhs=w3[:, k, :],
                                                                         start=(k == 0), stop=(k == 15)),
                         reads=["cact", ("wt", i % 2)], writes=[("psb", i % 2)])
                r = rt[i % 2]
                b = bt[i % 2]
                P.op("dve", lambda e, r=r, pso=pso, b=b: e.tensor_tensor(out=r, in0=pso, in1=b, op=ALU.add),
                     reads=[("psb", i % 2), ("bt", i % 2)], writes=[("rt", i % 2), ("psb", i % 2)])
                P.dma("pool", d["modv"][l:l + 1, j * 512:(j + 1) * 512], r, reads=[("rt", i % 2)], writes=["modv"])
        P.barrier()

    def sublayer_setup(self, P, A, l, sub, gate_scale, want_cols=True):
        d = self.d
        shc = A.take(16)
        scc = A.take(16)
        if gate_scale is None:
            gb = None
        else:
            gb = A.take(D)
        mv = d["modv"]
        P.dma("sp", shc, mv[l, (3 * sub) * D:(3 * sub + 1) * D].rearrange("(k p) -> p k", p=128),
              writes=["shc"], allow_slow_non_contiguous=True)
        P.dma("sp", scc, mv[l, (3 * sub + 1) * D:(3 * sub + 2) * D].rearrange("(k p) -> p k", p=128),
              writes=["scc"], allow_slow_non_contiguous=True)
        if gb is not None:
            P.dma("sp", gb, mv[l, (3 * sub + 2) * D:(3 * sub + 3) * D].partition_broadcast(128), writes=["gb"])
        gc = self.gcol[:, (l * 3 + sub) * 16:(l * 3 + sub + 1) * 16]
        P.op("dve", lambda e: e.scalar_tensor_tensor(out=scc, in0=scc, scalar=1.0, in1=gc, op0=ALU.add, op1=ALU.mult),
             reads=["scc", "gcol"], writes=["scc"])
        if gb is not None:
            P.op("dve", lambda e: e.tensor_scalar(out=gb, in0=gb, scalar1=1.0, scalar2=gate_scale, op0=ALU.add, op1=ALU.mult),
                 reads=["gb"], writes=["gb"])
        return shc, scc, gb

    def norm_loads(self, P, src, g, xb):
        for s in range(4):
            t = g * 4 + s
            P.dma("pool", xb[s % len(xb)], src[t * 128:(t + 1) * 128, :], reads=[("xr", t, n) for n in range(4)], writes=[("xb", s % len(xb))])

    def norm_group(self, P, src, g, hT, shc, gsc, xb, junk, st, inline_load=False, junk_key="junk"):
        ps = self.ps
        for s in range(4):
            t = g * 4 + s
            xt = xb[s % len(xb)]
            kx = ("xb", s % len(xb))
            if inline_load:
                P.dma("pool", xt, src[t * 128:(t + 1) * 128, :], reads=[("xr", t, n) for n in range(4)], writes=[kx])
            ss = st[t % 2]
            kss = ("st", t % 2)
            P.op("act", lambda e, xt=xt, ss=ss: e.activation(out=junk, in_=xt, func=AF.Square, accum_out=ss[:, 0:1]),
                 reads=[kx], writes=[junk_key, kss])
            P.op("dve", lambda e, ss=ss: e.tensor_scalar(out=ss[:, 1:2], in0=ss[:, 0:1], scalar1=1.0 / D, scalar2=EPS,
                                                         op0=ALU.mult, op1=ALU.add), reads=[kss], writes=[kss])
            P.op("act", lambda e, ss=ss: e.activation(out=ss[:, 2:3], in_=ss[:, 1:2], func=AF.Sqrt), reads=[kss], writes=[kss])
            P.op("dve", lambda e, ss=ss: e.reciprocal(out=ss[:, 3:4], in_=ss[:, 2:3]), reads=[kss], writes=[kss])
            P.op("dve", lambda e, xt=xt, ss=ss: e.tensor_scalar(out=xt, in0=xt, scalar1=ss[:, 3:4], scalar2=None, op0=ALU.mult),
                 reads=[kx, kss], writes=[kx])
            for kg in range(4):
                bank = kg % 2
                kp = ("psb", bank)
                for q in range(4):
                    k = kg * 4 + q
                    pt = ps[:, bank * 512 + q * 128: bank * 512 + (q + 1) * 128]
                    P.op("pe", lambda e, pt=pt, xt=xt, k=k: e.transpose(out=pt, in_=xt[:, k * 128:(k + 1) * 128], identity=self.ident),
                         reads=[kx, "ident"], writes=[kp])
                for q in range(4):
                    k = kg * 4 + q
                    pt = ps[:, bank * 512 + q * 128: bank * 512 + (q + 1) * 128]
                    P.op("act", lambda e, pt=pt, k=k, s=s: e.activation(out=hT[:, k, s * 128:(s + 1) * 128], in_=pt, func=AF.Identity,
                                                                         scale=gsc[:, k:k + 1], bias=shc[:, k:k + 1]),
                         reads=[kp, "scc", "shc"], writes=[("hT", s), kp])


    def headnorm(self, P, x3, H, dh, gain, tmp3, stat, kx):
        kt, ks = ("hn_tmp", kx), ("hn_stat", kx)
        P.op("dve", lambda e: e.tensor_tensor(out=tmp3, in0=x3, in1=x3, op=ALU.mult), reads=[kx], writes=[kt])
        yield
        P.op("dve", lambda e: e.tensor_reduce(out=stat[:, 0:H], in_=tmp3, axis=AX.X, op=ALU.add), reads=[kt], writes=[ks])
        yield
        P.op("dve", lambda e: e.tensor_scalar(out=stat[:, H:2 * H], in0=stat[:, 0:H], scalar1=1.0 / dh, scalar2=EPS,
                                              op0=ALU.mult, op1=ALU.add), reads=[ks], writes=[ks])
        yield
        P.op("act", lambda e: e.activation(out=stat[:, H:2 * H], in_=stat[:, H:2 * H], func=AF.Sqrt), reads=[ks], writes=[ks])
        yield
        P.op("dve", lambda e: e.reciprocal(out=stat[:, 2 * H:3 * H], in_=stat[:, H:2 * H]), reads=[ks], writes=[ks])
        yield
        P.op("dve", lambda e: e.tensor_tensor(out=x3, in0=x3, in1=stat[:, 2 * H:3 * H].unsqueeze(2).to_broadcast([128, H, dh]),
                                              op=ALU.mult), reads=[kx, ks], writes=[kx])
        yield
        P.op("dve", lambda e: e.tensor_tensor(out=x3, in0=x3, in1=gain.unsqueeze(1).to_broadcast([128, H, dh]), op=ALU.mult),
             reads=[kx, "gt"], writes=[kx])
        yield

    def rope(self, P, x1, x2, cos3, sin3, rt, kx, krope):
        H, r2 = x1.shape[1], x1.shape[2]
        t = [r[:, 0:H * r2].rearrange("p (h r) -> p h r", h=H) for r in rt]
        kr = ("rope_tmp", kx)
        P.op("dve", lambda e: e.tensor_tensor(out=t[0], in0=x1, in1=cos3, op=ALU.mult), reads=[kx, krope], writes=[kr])
        P.op("dve", lambda e: e.tensor_tensor(out=t[1], in0=x2, in1=sin3, op=ALU.mult), reads=[kx, krope], writes=[kr])
        yield
        P.op("dve", lambda e: e.tensor_tensor(out=t[2], in0=x1, in1=sin3, op=ALU.mult), reads=[kx, krope], writes=[kr])
        P.op("dve", lambda e: e.tensor_tensor(out=t[3], in0=x2, in1=cos3, op=ALU.mult), reads=[kx, krope], writes=[kr])
        yield
        P.op("dve", lambda e: e.tensor_tensor(out=x1, in0=t[0], in1=t[1], op=ALU.subtract), reads=[kr], writes=[kx])
        P.op("dve", lambda e: e.tensor_tensor(out=x2, in0=t[2], in1=t[3], op=ALU.add), reads=[kr], writes=[kx])
        yield

    def phase_mixproj(self, P, A, l, src):
        d = self.d
        ps = self.ps
        psb = self.ps.bitcast(BF16)
        A.off = self.base_off
        shc, gsc, _ = self.sublayer_setup(P, A, l, 1, None)
        xb = [A.take(D) for _ in range(2)]
        st = [A.take(4) for _ in range(2)]
        hT = A.take(16 * 512, BF16).rearrange("p (k t) -> p k t", k=16)
        wblk = [A.take(16 * 512, BF16) for _ in range(2)]
        wblk.append(self.big_flat(xb[0], 2 * D).bitcast(BF16)[:, 0:16 * 512])
        wuq = A.take(4 * 1536, BF16).rearrange("p (j c) -> p j c", j=4)
        wukv = A.take(2048, BF16)
        gt = A.take(GT)
        identb = A.take(128, BF16)
        rAg = A.take(4 * 128).rearrange("p (s c) -> p s c", s=4)
        rQg = A.take(4 * 512).rearrange("p (s c) -> p s c", s=4)
        rKg = A.take(4 * 64).rearrange("p (s c) -> p s c", s=4)
        xsL = [A.take(1536) for _ in range(4)]
        tmpL = [A.take(1024) for _ in range(4)]
        statL = [A.take(32) for _ in range(4)]
        xbfL = [A.take(1536, BF16) for _ in range(4)]
        cqTL = [A.take(4 * 128, BF16).rearrange("p (j t) -> p j t", j=4) for _ in range(4)]
        ckvTL = [A.take(128, BF16) for _ in range(4)]
        qaS = A.take(4 * 512, BF16).rearrange("p (h t) -> p h t", h=4)
        kaS = A.take(4 * 512, BF16).rearrange("p (h t) -> p h t", h=4)
        qnS_flat = A.take(8 * 512, BF16)
        qnS = qnS_flat.rearrange("p (h t) -> p h t", h=8)
        junk = qnS_flat[:, 0:D]
        qrS = A.take(8 * 512, BF16).rearrange("p (h t) -> p h t", h=8)
        knS = A.take(8 * 512, BF16).rearrange("p (h t) -> p h t", h=8)
        krS = A.take(512, BF16)
        fmS = [A.take(512, BF16) for _ in range(2)]
        vaS = [A.take(4 * 129, BF16).rearrange("p (h c) -> p h c", h=4) for _ in range(4)]
        vbS = [A.take(8 * 129, BF16).rearrange("p (h c) -> p h c", h=8) for _ in range(4)]
        vcS = [A.take(512, BF16) for _ in range(4)]
        wiS = [A.take(16) for _ in range(4)]

        P.dma("sp", wuq, d["wuq_b"][l].rearrange("p (j c) -> p j c", j=4), writes=["wuq"])
        P.dma("sp", wukv, d["wukv_b"][l], writes=["wukv"])
        P.dma("sp", gt, d["gtm"][:, l * GT:(l + 1) * GT], writes=["gt"])
        P.op("dve", lambda e: e.tensor_copy(out=identb, in_=self.ident), reads=["ident"], writes=["identb"])
        for b in range(4):
            P.op("pool", lambda e, b=b: e.memset(cqTL[b][64:128, 3, :], 0.0), writes=[("cqT", b)])
            P.op("dve", lambda e, b=b: e.memset(vaS[b][:, :, 128:129], 1.0), writes=[("vaS", b)])
            P.op("dve", lambda e, b=b: e.memset(vbS[b][:, :, 128:129], 1.0), writes=[("vbS", b)])
        g_qa, g_ka = gt[:, 0:128], gt[:, 128:256]
        g_cq, g_ckv = gt[:, 256:704], gt[:, 704:832]
        g_qn, g_kn = gt[:, 832:960], gt[:, 960:1088]
        g_qr, g_kr = gt[:, 1088:1152], gt[:, 1152:1216]
        hTk = [("hT", s) for s in range(4)]
        tm_blocks = [(0, 512), (512, 1024), (1024, 1536), (1536, 2048), (2048, 2560), (2560, 2704)]
        wtm_v = d["wtm_b"][l].rearrange("p (k c) -> p k c", k=16)
        wfm_v = d["wfm_b"][l].rearrange("p (k c) -> p k c", k=16)
        fm_tiles = [(0, 512), (512, 1024), (1024, 1536), (1536, 2048), (2048, 2176)]
        fm_dest = ([("qiT", i) for i in range(8)] + [("kiT", 0)] + [("qcT", i) for i in range(4)] + [("kcT", i) for i in range(4)])
        cnt = {"w": 0, "pb": 0, "fm": 0}

        def wkeys(wb):
            return [("wblk", wb)] + ([("xb", 0), ("xb", 1)] if wb == 2 else [])

        ng = self.cfg.get("ng", NG)
        for g in range(ng):
            self.norm_group(P, src, g, hT, shc, gsc, xb, junk, st, inline_load=True, junk_key="qnS")
            rows = slice(g * 512, (g + 1) * 512)
            P.dma("pool", rAg, d["ropeA"][rows, :].rearrange("(s p) c -> p s c", p=128), writes=["ropeA"])
            P.dma("pool", rQg, d["ropeQ"][rows, :].rearrange("(s p) c -> p s c", p=128), writes=["ropeQ"])
            P.dma("pool", rKg, d["ropeK"][rows, :].rearrange("(s p) c -> p s c", p=128), writes=["ropeK"])

            def tm_chain(bi, s, w3, wb, nc_):
                t = g * 4 + s
                trows = slice(t * 128, (t + 1) * 128)
                tcol = slice(s * 128, (s + 1) * 128)
                xs, tmp, stat, xbf, cqT, ckvT = xsL[s], tmpL[s], statL[s], xbfL[s], cqTL[s], ckvTL[s]
                rt = [tmp[:, 256 * q:256 * (q + 1)] for q in range(4)]
                kxs, kxbf = ("xs", s), ("xbf", s)
                pb = s
                tb = 7

                def evac_xs(pbank, ncols, off=0):
                    P.op("act", lambda e: e.copy(out=xs[:, off:off + ncols], in_=ps[:, pbank * 512:pbank * 512 + ncols]),
                         reads=[("psb", pbank)], writes=[kxs, ("psb", pbank)])

                def tr(items):
                    for in_ap, off, w in items:
                        P.op("pe", lambda e, in_ap=in_ap, off=off, w=w: e.transpose(out=psb[0:w, tb * 1024 + off:tb * 1024 + off + 128], in_=in_ap,
                                                                                    identity=identb),
                             reads=[kxbf, "identb"], writes=[("psb", tb)])

                def evac7(out_ap, parts, off, ncols, wkey, view=None):
                    i_ = psb[0:parts, tb * 1024 + off:tb * 1024 + off + ncols]
                    if view is not None:
                        i_ = i_.rearrange("p (h t) -> p h t", h=view)
                    P.op("act", lambda e: e.copy(out=out_ap, in_=i_), reads=[("psb", tb)], writes=[wkey, ("psb", tb)])

                for k in range(16):
                    P.op("pe", lambda e, k=k: e.matmul(ps[:, pb * 512:pb * 512 + nc_], lhsT=hT[:, k, tcol], rhs=w3[:, k, :],
                                                       start=(k == 0), stop=(k == 15)),
                         reads=wkeys(wb) + [("hT", s)], writes=[("psb", pb)])
                yield
                if bi in (0, 1):
                    evac_xs(pb, 512)
                    yield
                    x3 = xs[:, 0:512].rearrange("p (h c) -> p h c", h=4)
                    yield from self.headnorm(P, x3, 4, 128, g_qa if bi == 0 else g_ka, tmp[:, 0:512].rearrange("p (h c) -> p h c", h=4), stat, kxs)
                    cos3 = rAg[:, s, 0:64].rearrange("p (h r) -> p h r", h=4)
                    sin3 = rAg[:, s, 64:128].rearrange("p (h r) -> p h r", h=4)
                    yield from self.rope(P, x3[:, :, 0:16], x3[:, :, 16:32], cos3, sin3, rt, kxs, "ropeA")
                    P.op("dve", lambda e: e.tensor_copy(out=xbf[:, 0:512], in_=xs[:, 0:512]), reads=[kxs], writes=[kxbf])
                    yield
                    tr([(xbf[:, h * 128:(h + 1) * 128], h * 128, 128) for h in range(4)])
                    evac7((qaS if bi == 0 else kaS)[:, :, tcol], 128, 0, 512, "qaS" if bi == 0 else "kaS", view=4)
                    yield
                elif bi == 2:
                    P.op("act", lambda e: e.copy(out=vaS[s][:, :, 0:128], in_=ps[:, pb * 512:(pb + 1) * 512].rearrange("p (h c) -> p h c", h=4)),
                         reads=[("psb", pb)], writes=[("vaS", s), ("psb", pb)])
                    P.dma("pool", d["va"][trows, :].rearrange("p (h c) -> p h c", h=4), vaS[s], reads=[("vaS", s)])
                    yield
                elif bi == 3:
                    P.op("act", lambda e: e.copy(out=vcS[s], in_=ps[:, pb * 512:(pb + 1) * 512]),
                         reads=[("psb", pb)], writes=[("vcS", s), ("psb", pb)])
                    P.dma("pool", d["vc"][trows, :], vcS[s], reads=[("vcS", s)])
                    yield
                elif bi == 4:
                    evac_xs(pb, 512)
                    yield
                    yield from self.headnorm(P, xs[:, 0:448].unsqueeze(1), 1, 448, g_cq, tmp[:, 0:448].unsqueeze(1), stat, kxs)
                    xk = xs[:, 448:512].unsqueeze(1)
                    yield from self.headnorm(P, xk, 1, 64, g_kr, tmp[:, 448:512].unsqueeze(1), stat, kxs)
                    yield from self.rope(P, xk[:, :, 0:32], xk[:, :, 32:64], rKg[:, s, 0:32].unsqueeze(1), rKg[:, s, 32:64].unsqueeze(1),
                                         rt, kxs, "ropeK")
                    P.op("dve", lambda e: e.tensor_copy(out=xbf[:, 0:512], in_=xs[:, 0:512]), reads=[kxs], writes=[kxbf])
                    yield
                    tr([(xbf[:, 0:128], 0, 128), (xbf[:, 128:256], 128, 128), (xbf[:, 256:384], 256, 128),
                        (xbf[:, 384:448], 384, 64), (xbf[:, 448:512], 512, 64)])
                    kcq = ("cqT", s)
                    evac7(cqT[:, 0:3, :], 128, 0, 384, kcq, view=3)
                    evac7(cqT[0:64, 3, :], 64, 384, 128, kcq)
                    evac7(krS[0:64, tcol], 64, 512, 128, "krS")
                    yield
                    for nb in range(3):
                        for j in range(4):
                            kk = 128
                            P.op("pe", lambda e, nb=nb, j=j, kk=kk: e.matmul(
                                ps[:, (4 + nb) * 512:(5 + nb) * 512], lhsT=cqT[0:kk, j, :], rhs=wuq[0:kk, j, nb * 512:(nb + 1) * 512],
                                start=(j == 0), stop=(j == 3)), reads=[kcq, "wuq"], writes=[("psb", 4 + nb)])
                        evac_xs(4 + nb, 512, off=nb * 512)
                        yield
                    q3 = xs[:, 0:1536].rearrange("p (h c) -> p h c", h=8)
                    yield from self.headnorm(P, q3[:, :, 0:128], 8, 128, g_qn, tmp[:, 0:1024].rearrange("p (h c) -> p h c", h=8), stat, kxs)
                    yield from self.headnorm(P, q3[:, :, 128:192], 8, 64, g_qr, tmp[:, 0:512].rearrange("p (h c) -> p h c", h=8), stat, kxs)
                    cosq = rQg[:, s, 0:256].rearrange("p (h r) -> p h r", h=8)
                    sinq = rQg[:, s, 256:512].rearrange("p (h r) -> p h r", h=8)
                    yield from self.rope(P, q3[:, :, 128:160], q3[:, :, 160:192], cosq, sinq, rt, kxs, "ropeQ")
                    P.op("dve", lambda e: e.tensor_copy(out=xbf[:, 0:1536], in_=xs[:, 0:1536]), reads=[kxs], writes=[kxbf])
                    yield
                    tr([(xbf[:, h * 192:h * 192 + 128], h * 128, 128) for h in range(8)])
                    evac7(qnS[:, :, tcol], 128, 0, 1024, "qnS", view=8)
                    yield
                    tr([(xbf[:, h * 192 + 128:h * 192 + 192], h * 128, 64) for h in range(8)])
                    evac7(qrS[0:64, :, tcol], 64, 0, 1024, "qrS", view=8)
                    yield
                else:
                    evac_xs(pb, 144)
                    yield
                    P.op("dve", lambda e: e.tensor_scalar(out=wiS[s], in0=xs[:, 128:144], scalar1=1.0 / 32.0, scalar2=None, op0=ALU.mult),
                         reads=[kxs], writes=[("wiS", s)])
                    P.dma("pool", d["wi"][trows, :], wiS[s], reads=[("wiS", s)])
                    yield
                    yield from self.headnorm(P, xs[:, 0:128].unsqueeze(1), 1, 128, g_ckv, tmp[:, 0:128].unsqueeze(1), stat, kxs)
                    P.op("dve", lambda e: e.tensor_copy(out=xbf[:, 0:128], in_=xs[:, 0:128]), reads=[kxs], writes=[kxbf])
                    yield
                    tr([(xbf[:, 0:128], 0, 128)])
                    kckv = ("ckvT", s)
                    evac7(ckvT, 128, 0, 128, kckv)
                    yield
                    for hh in range(2):
                        for nb in range(2):
                            P.op("pe", lambda e, hh=hh, nb=nb: e.matmul(
                                ps[:, (4 + nb) * 512:(5 + nb) * 512], lhsT=ckvT, rhs=wukv[:, hh * 1024 + nb * 512:hh * 1024 + (nb + 1) * 512],
                                start=True, stop=True), reads=[kckv, "wukv"], writes=[("psb", 4 + nb)])
                            evac_xs(4 + nb, 512, off=nb * 512)
                        yield
                        kv3 = xs[:, 0:1024].rearrange("p (h c) -> p h c", h=4)
                        P.op("dve", lambda e, hh=hh: e.tensor_copy(out=vbS[s][:, hh * 4:(hh + 1) * 4, 0:128], in_=kv3[:, :, 128:256]),
                             reads=[kxs], writes=[("vbS", s)])
                        yield
                        yield from self.headnorm(P, kv3[:, :, 0:128], 4, 128, g_kn, tmp[:, 0:512].rearrange("p (h c) -> p h c", h=4), stat, kxs)
                        P.op("dve", lambda e: e.tensor_copy(out=xbf[:, 0:512].rearrange("p (h c) -> p h c", h=4), in_=kv3[:, :, 0:128]),
                             reads=[kxs], writes=[kxbf])
                        yield
                        tr([(xbf[:, h * 128:(h + 1) * 128], h * 128, 128) for h in range(4)])
                        evac7(knS[:, hh * 4:(hh + 1) * 4, tcol], 128, 0, 512, "knS", view=4)
                        yield
                    P.dma("pool", d["vb"][trows, :].rearrange("p (h c) -> p h c", h=8), vbS[s], reads=[("vbS", s)])
                    yield

            def fm_gen(fi, banks):
                c0, c1 = fm_tiles[fi]
                nc_ = c1 - c0
                wb = cnt["w"] % 3
                cnt["w"] += 1
                w3 = wblk[wb][:, 0:16 * nc_].rearrange("p (k c) -> p k c", k=16)
                P.dma("sp", w3, wfm_v[:, :, c0:c1], writes=wkeys(wb))
                yield
                for cc in range(nc_ // 128):
                    ci = fi * 4 + cc
                    pb = banks[cnt["pb"] % len(banks)]
                    cnt["pb"] += 1
                    for k in range(16):
                        P.op("pe", lambda e, pb=pb, k=k, cc=cc: e.matmul(
                            ps[:, pb * 512:(pb + 1) * 512], lhsT=w3[:, k, cc * 128:(cc + 1) * 128], rhs=hT[:, k, :],
                            start=(k == 0), stop=(k == 15)), reads=wkeys(wb) + hTk, writes=[("psb", pb)])
                        if k % 4 == 3:
                            yield
                    fb = cnt["fm"] % 2
                    cnt["fm"] += 1
                    P.op("act", lambda e, pb=pb, fb=fb: e.copy(out=fmS[fb], in_=ps[:, pb * 512:(pb + 1) * 512]),
                         reads=[("psb", pb)], writes=[("fmS", fb), ("psb", pb)])
                    nm, idx = fm_dest[ci]
                    P.dma("pool", d[nm][idx, :, rows], fmS[fb], reads=[("fmS", fb)])
                    yield

            for bi, (c0, c1) in enumerate(tm_blocks):
                nc_ = c1 - c0
                wb = cnt["w"] % 3
                cnt["w"] += 1
                w3 = wblk[wb][:, 0:16 * nc_].rearrange("p (k c) -> p k c", k=16)
                P.dma("sp", w3, wtm_v[:, :, c0:c1], writes=wkeys(wb))
                gens = [tm_chain(bi, s, w3, wb, nc_) for s in range(4)]
                if bi < 4:
                    gens.append(fm_gen(bi, (4, 5)))
                while gens:
                    for g_ in list(gens):
                        try:
                            next(g_)
                        except StopIteration:
                            gens.remove(g_)
            for _ in fm_gen(4, (0, 1, 2, 3)):
                pass
            P.dma("pool", d["qaT"][:, :, rows].rearrange("h p t -> p h t"), qaS, reads=["qaS"])
            P.dma("pool", d["kaT"][:, :, rows].rearrange("h p t -> p h t"), kaS, reads=["kaS"])
            P.dma("pool", d["qnT"][:, :, rows].rearrange("h p t -> p h t"), qnS, reads=["qnS"])
            P.dma("pool", d["knT"][:, :, rows].rearrange("h p t -> p h t"), knS, reads=["knS"])
            P.dma("pool", d["qrT"][:, :, rows].rearrange("h p t -> p h t"), qrS[0:64], reads=["qrS"])
            P.dma("pool", d["krT"][0, :, rows], krS[0:64], reads=["krS"])
        P.barrier()


    def attn_core(self, P, G, heads, s_mms, mask_rhs, v_ap, scale, pT, o_tm, identb, rs, rkeys):
        ps = self.ps
        nkb = 4 * G + 4
        seq = [(h, kb) for h in range(heads) for kb in range(nkb)]

        def emit_S(idx):
            h, kb = seq[idx]
            sb_ = idx % 2
            pS = ps[:, sb_ * 512:(sb_ + 1) * 512]
            mms = list(s_mms(h, kb))
            m = mask_rhs(kb)
            if m is not None:
                mms.append((identb, m))
            for i, (lt, rh) in enumerate(mms):
                P.op("pe", lambda e, pS=pS, lt=lt, rh=rh, i=i, n=len(mms): e.matmul(pS, lhsT=lt, rhs=rh, start=(i == 0), stop=(i == n - 1)),
                     reads=rkeys, writes=[("psb", sb_)])

        emit_S(0)
        for idx, (h, kb) in enumerate(seq):
            sb_ = idx % 2
            pS = ps[:, sb_ * 512:(sb_ + 1) * 512]
            if idx + 1 < len(seq):
                emit_S(idx + 1)
            p_ = pT[sb_]
            P.op("act", lambda e, p_=p_, pS=pS: e.activation(out=p_, in_=pS, func=AF.Exp, scale=scale),
                 reads=[("psb", sb_)], writes=[("pT", sb_), ("psb", sb_)])
            for i in range(4):
                last = 4 * G + i
                if kb > last:
                    continue
                P.op("pe", lambda e, i=i, p_=p_, h=h, kb=kb, last=last: e.matmul(
                    ps[:, (4 + i) * 512:(4 + i) * 512 + 129], lhsT=p_[:, i * 128:(i + 1) * 128], rhs=v_ap(h, kb),
                    start=(kb == 0), stop=(kb == last)), reads=[("pT", sb_)] + rkeys, writes=[("psb", 4 + i)])
            if kb == nkb - 1:
                for i in range(4):
                    acc = ps[:, (4 + i) * 512:(4 + i) * 512 + 129]
                    r_ = rs[:, i:i + 1]
                    P.op("dve", lambda e, acc=acc, r_=r_: e.reciprocal(out=r_, in_=acc[:, 128:129]), reads=[("psb", 4 + i)], writes=[("rs", i), ("psb", 4 + i)])
                    P.op("dve", lambda e, acc=acc, i=i, h=h, r_=r_: e.tensor_scalar(out=o_tm[:, i, h * 128:(h + 1) * 128], in0=acc[:, 0:128],
                                                                                     scalar1=r_, scalar2=None, op0=ALU.mult),
                         reads=[("psb", 4 + i), ("rs", i)], writes=["o_tm", ("psb", 4 + i)])

    def store_mixT(self, P, G, o_tm, nch, c0, stage, identb):
        psb = self.ps.bitcast(BF16)
        d = self.d
        rows = slice(G * 512, (G + 1) * 512)
        for c in range(nch):
            bank = 2 + c % 2
            for i in range(4):
                P.op("pe", lambda e, c=c, i=i, bank=bank: e.transpose(out=psb[:, bank * 1024 + i * 128:bank * 1024 + (i + 1) * 128],
                                                                       in_=o_tm[:, i, c * 128:(c + 1) * 128], identity=identb),
                     reads=["o_tm", "identb"], writes=[("psb", bank)])
            P.op("act", lambda e, c=c, bank=bank: e.copy(out=stage[:, c, :], in_=psb[:, bank * 1024:bank * 1024 + 512]),
                 reads=[("psb", bank)], writes=["stage", ("psb", bank)])
        P.dma("pool", d["mixT"][c0:c0 + nch, :, rows].rearrange("c p t -> p c t"), stage[:, 0:nch, :], reads=["stage"], writes=[])

    def phase_dsa(self, P, A, l):
        d = self.d
        ps = self.ps
        psb = self.ps.bitcast(BF16)
        A.off = self.base_off
        identb = A.take(128, BF16)
        kiT = A.take(S, BF16)
        kaT = A.take(4 * S, BF16).rearrange("p (h t) -> p h t", h=4)
        va = A.take(32 * 516, BF16).rearrange("p (k h c) -> p k h c", k=32, h=4)
        qi_g = A.take(16 * 512, BF16).rearrange("p (h t) -> p h t", h=16)
        qi_4 = qi_g.rearrange("p (c two) t -> p c two t", two=2)
        qa_g = A.take(4 * 512, BF16).rearrange("p (h t) -> p h t", h=4)
        wi_g = A.take(64).rearrange("p (s h) -> p s h", s=4)
        scores = [A.take(S) for _ in range(2)]
        rl = [[A.take(512, BF16) for _ in range(3)] for _ in range(2)]
        dg = [A.take(16 * 128, BF16).rearrange("p (h q) -> p h q", h=16) for _ in range(2)]
        mks = [A.take(S, BF16) for _ in range(2)]
        mkT = A.take(32 * 512, BF16).rearrange("p (k t) -> p k t", k=32)
        pT = [A.take(512, BF16) for _ in range(2)]
        o_tm = A.take(4 * 512, BF16).rearrange("p (s c) -> p s c", s=4)
        stage = A.take(4 * 512, BF16).rearrange("p (c t) -> p c t", c=4)
        pw2 = A.take(NBIS + 1)
        Ws = [A.take(NBIS + 1) for _ in range(2)]
        W2s = [A.take(NBIS + 1) for _ in range(2)]
        sms = [A.take(16) for _ in range(2)]
        rs = A.take(4)
        P.op("dve", lambda e: e.tensor_copy(out=identb, in_=self.ident), reads=["ident"], writes=["identb"])
        P.dma("sp", kiT, d["kiT"][0], writes=["kiT"])
        P.op("pool", lambda e: e.memset(qi_g, 0.0), writes=["qi_g"])
        P.dma("sp", kaT, d["kaT"].rearrange("h p t -> p h t"), writes=["kaT"])
        P.dma("sp", va, d["va"].rearrange("(k p) (h c) -> p k h c", p=128, h=4), writes=["va"])
        P.dma("sp", pw2, d["pow2"], writes=["pw2"])
        a_scale = 128.0 ** -0.5
        cnt = {"s": 0, "l": 0}
        for G in range(self.cfg.get("ng", NG)):
            rows = slice(G * 512, (G + 1) * 512)
            P.dma("sp", qi_4[0:64, :, 0, :], d["qiT"][:, 0:64, rows].rearrange("c p t -> p c t"), writes=["qi_g"])
            P.dma("sp", qi_4[64:128, :, 1, :], d["qiT"][:, 64:128, rows].rearrange("c p t -> p c t"), writes=["qi_g"])
            P.dma("sp", qa_g, d["qaT"][:, :, rows].rearrange("h p t -> p h t"), writes=["qa_g"])
            P.dma("sp", wi_g, d["wi"][rows, :].rearrange("(s p) h -> p s h", p=128), writes=["wi_g"])
            P.op("pool", lambda e, G=G: e.memset(mkT[:, 4 * G:4 * G + 4, :], BIGNEG), writes=[("mkT", i_) for i_ in range(4)])
            def qchain(i, sl):
                qt = 4 * G + i
                n2 = 128 * (qt + 1)
                n1 = n2 - 64
                score = scores[sl]
                sm = sms[sl]
                W, W2 = Ws[sl], W2s[sl]
                lo, w0, mid, cn, upd, m8 = sm[:, 0:1], sm[:, 2:3], sm[:, 3:4], sm[:, 4:5], sm[:, 5:6], sm[:, 8:16]
                ksc, ksm, kW = ("score", sl), ("sm", sl), ("W", sl)
                lb0 = 4 * sl
                sbank = lb0 + 2
                tbank = lb0 + 3
                dg_ = dg[sl]
                kdg = ("dg", sl)
                P.op("pool", lambda e: e.tensor_tensor(out=dg_, in0=identb.unsqueeze(1).to_broadcast([128, 16, 128]),
                                                       in1=wi_g[:, i, :].unsqueeze(2).to_broadcast([128, 16, 128]), op=ALU.mult),
                     reads=["identb", "wi_g"], writes=[kdg])
                yield
                for k5 in range((n2 + 511) // 512):
                    wd_ = min(512, n2 - k5 * 512)
                    pacc = ps[:, sbank * 512:sbank * 512 + wd_]

                    def logit(h, wd_=wd_, k5=k5):
                        lb = lb0 + h % 2
                        P.op("pe", lambda e, lb=lb, h=h: e.matmul(
                            ps[:, lb * 512:lb * 512 + wd_], lhsT=qi_g[:, h, i * 128:(i + 1) * 128],
                            rhs=kiT[:, k5 * 512:k5 * 512 + wd_], start=True, stop=True),
                             reads=["qi_g", "kiT"], writes=[("psb", lb)])

                    logit(0)
                    for h in range(16):
                        if h + 1 < 16:
                            logit(h + 1)
                        lb = lb0 + h % 2
                        rb = (sl, h % 3)
                        r_ = rl[sl][h % 3][:, 0:wd_]
                        pin = ps[:, lb * 512:lb * 512 + wd_]
                        if h not in (1, 3, 6, 8, 10, 13, 15):
                            P.op("act", lambda e, r_=r_, pin=pin: e.activation(out=r_, in_=pin, func=AF.Relu),
                                 reads=[("psb", lb)], writes=[("rl", rb), ("psb", lb)])
                        else:
                            P.op("dve", lambda e, r_=r_, pin=pin: e.tensor_scalar(out=r_, in0=pin, scalar1=0.0, scalar2=None, op0=ALU.max),
                                 reads=[("psb", lb)], writes=[("rl", rb), ("psb", lb)])
                        P.op("pe", lambda e, pacc=pacc, h=h, r_=r_: e.matmul(pacc, lhsT=dg_[:, h, :], rhs=r_, start=(h == 0), stop=(h == 15)),
                             reads=[kdg, ("rl", rb)], writes=[("psb", sbank)])
                        yield
                    sc_blk = score[:, k5 * 512:k5 * 512 + wd_]
                    P.op("act", lambda e, sc_blk=sc_blk, pacc=pacc: e.copy(out=sc_blk, in_=pacc),
                         reads=[("psb", sbank)], writes=[ksc, ("psb", sbank)])
                    yield
                sc = score[:, 0:n2]
                P.op("dve", lambda e: e.tensor_reduce(out=lo, in_=sc, axis=AX.X, op=ALU.min), reads=[ksc], writes=[ksm])
                yield
                P.op("dve", lambda e: e.memset(score[0:64, n1:n2], BIGNEG), reads=[ksm], writes=[ksc])
                yield
                if qt >= 2:
                    P.op("dve", lambda e: e.max(out=m8, in_=sc), reads=[ksc], writes=[ksm])
                    yield
                    P.op("dve", lambda e: e.tensor_tensor(out=w0, in0=m8[:, 0:1], in1=lo, op=ALU.subtract), reads=[ksm], writes=[ksm])
                    yield
                    P.op("dve", lambda e: e.tensor_scalar(out=W, in0=pw2, scalar1=w0, scalar2=None, op0=ALU.mult), reads=[ksm, "pw2"], writes=[kW])
                    jk = mks[sl][:, 0:n2]
                    if sl == 0:
                        P.op("dve", lambda e: e.tensor_scalar(out=W2, in0=pw2, scalar1=w0, scalar2=2.0, op0=ALU.mult, op1=ALU.mult), reads=[ksm, "pw2"], writes=[kW])
                        yield
                        P.op("dve", lambda e: e.tensor_tensor(out=mid, in0=lo, in1=W[:, 0:1], op=ALU.add), reads=[ksm, kW], writes=[ksm])
                        yield
                        for k in range(NBIS):
                            P.op("dve", lambda e: e.tensor_scalar(out=jk, in0=sc, scalar1=mid, scalar2=None, op0=ALU.is_ge,
                                                                  op1=ALU.add, accum_out=cn), reads=[ksc, ksm], writes=[ksm, ("mk", sl)])
                            yield
                            P.op("dve", lambda e, k=k: e.tensor_scalar(out=upd, in0=cn, scalar1=255.5, scalar2=W2[:, k + 1:k + 2], op0=ALU.is_ge, op1=ALU.mult),
                                 reads=[ksm, kW], writes=[ksm])
                            yield
                            P.op("dve", lambda e, k=k: e.scalar_tensor_tensor(out=mid, in0=upd, scalar=W[:, k + 1:k + 2], in1=mid, op0=ALU.subtract, op1=ALU.add),
                                 reads=[ksm, kW], writes=[ksm])
                            yield
                        P.op("dve", lambda e: e.tensor_tensor(out=lo, in0=mid, in1=W[:, NBIS:NBIS + 1], op=ALU.subtract), reads=[ksm, kW], writes=[ksm])
                        yield
                    else:
                        P.op("dve", lambda e: e.tensor_scalar(out=W2, in0=pw2, scalar1=w0, scalar2=-2.0, op0=ALU.mult, op1=ALU.mult), reads=[ksm, "pw2"], writes=[kW])
                        yield
                        P.op("dve", lambda e: e.tensor_scalar(out=mid, in0=lo, scalar1=W[:, 0:1], scalar2=-1.0, op0=ALU.add, op1=ALU.mult), reads=[ksm, kW], writes=[ksm])
                        yield
                        cthr = float(511 - n2)
                        for k in range(NBIS):
                            P.op("act", lambda e: e.activation(out=jk, in_=sc, func=AF.Sign, bias=mid, scale=1.0, accum_out=cn),
                                 reads=[ksc, ksm], writes=[ksm, ("mk", sl)])
                            yield
                            P.op("dve", lambda e, k=k: e.tensor_scalar(out=upd, in0=cn, scalar1=cthr, scalar2=W2[:, k + 1:k + 2], op0=ALU.is_ge, op1=ALU.mult),
                                 reads=[ksm, kW], writes=[ksm])
                            yield
                            P.op("dve", lambda e, k=k: e.scalar_tensor_tensor(out=mid, in0=upd, scalar=W[:, k + 1:k + 2], in1=mid, op0=ALU.add, op1=ALU.add),
                                 reads=[ksm, kW], writes=[ksm])
                            yield
                        P.op("dve", lambda e: e.tensor_scalar(out=lo, in0=mid, scalar1=-1.0, scalar2=W[:, NBIS:NBIS + 1], op0=ALU.mult, op1=ALU.subtract),
                             reads=[ksm, kW], writes=[ksm])
                        yield
                mk_ = mks[sl]
                kmk = ("mk", sl)
                P.op("dve", lambda e: e.tensor_scalar(out=mk_[:, 0:n2], in0=sc, scalar1=lo, scalar2=1.0, op0=ALU.is_ge, op1=ALU.subtract),
                     reads=[ksc, ksm], writes=[kmk])
                yield
                for kb0 in range(0, qt + 1, 8):
                    nb_ = min(8, qt + 1 - kb0)
                    for j in range(nb_):
                        kb = kb0 + j
                        P.op("pe", lambda e, j=j, kb=kb: e.transpose(out=psb[:, tbank * 1024 + j * 128:tbank * 1024 + (j + 1) * 128],
                                                                      in_=mk_[:, kb * 128:(kb + 1) * 128], identity=identb),
                             reads=[kmk, "identb"], writes=[("psb", tbank)])
                    P.op("act", lambda e, nb_=nb_, kb0=kb0: e.activation(
                        out=mkT[:, kb0:kb0 + nb_, i * 128:(i + 1) * 128],
                        in_=psb[:, tbank * 1024:tbank * 1024 + nb_ * 128].rearrange("p (k t) -> p k t", k=nb_), func=AF.Copy, scale=-BIGNEG),
                         reads=[("psb", tbank)], writes=[("mkT", i), ("psb", tbank)])
                    yield

            for pair in ((0, 1), (2, 3)):
                gens = [qchain(pair[0], 0), qchain(pair[1], 1)]
                while gens:
                    for g_ in list(gens):
                        try:
                            next(g_)
                        except StopIteration:
                            gens.remove(g_)
            self.attn_core(P, G, 4,
                           lambda h, kb: [(kaT[:, h, kb * 128:(kb + 1) * 128], qa_g[:, h, :])],
                           lambda kb: mkT[:, kb, :],
                           lambda h, kb: va[:, kb, h, :],
                           a_scale, pT, o_tm, identb, rs, ["kaT", "qa_g", "va", "identb"] + [("mkT", i_) for i_ in range(4)])
            self.store_mixT(P, G, o_tm, 4, 0, stage, identb)
        P.barrier()

    def phase_mla(self, P, A, l):
        d = self.d
        A.off = self.base_off
        identb = A.take(128, BF16)
        knT = A.take(8 * S, BF16).rearrange("p (h t) -> p h t", h=8)
        krT = A.take(S, BF16)
        vb = A.take(32 * 8 * 129, BF16).rearrange("p (k h c) -> p k h c", k=32, h=8)
        qn_g = A.take(8 * 512, BF16).rearrange("p (h t) -> p h t", h=8)
        qr_g = A.take(8 * 512, BF16).rearrange("p (h t) -> p h t", h=8)
        cmf = A.take(4 * 512)
        cm = A.take(4 * 512, BF16).rearrange("p (j t) -> p j t", j=4)
        pT = [A.take(512, BF16) for _ in range(2)]
        o_tm = A.take(4 * 1024, BF16).rearrange("p (s c) -> p s c", s=4)
        stage = A.take(8 * 512, BF16).rearrange("p (c t) -> p c t", c=8)
        rs = A.take(4)
        P.op("dve", lambda e: e.tensor_copy(out=identb, in_=self.ident), reads=["ident"], writes=["identb"])
        P.dma("sp", knT, d["knT"].rearrange("h p t -> p h t"), writes=["knT"])
        P.op("pool", lambda e: e.memset(krT[64:128, :], 0.0), writes=["krT"])
        P.op("pool", lambda e: e.memset(qr_g[64:128, :, :], 0.0), writes=["qr_g"])
        P.dma("sp", krT[0:64], d["krT"][0], writes=["krT"])
        P.dma("sp", vb, d["vb"].rearrange("(k p) (h c) -> p k h c", p=128, h=8), writes=["vb"])
        P.dma("sp", cmf, d["cmaskB"], writes=["cmf"])
        P.op("dve", lambda e: e.tensor_copy(out=cm, in_=cmf.rearrange("p (j t) -> p j t", j=4)), reads=["cmf"], writes=["cm"])
        b_scale = 192.0 ** -0.5
        for G in range(self.cfg.get("ng", NG)):
            rows = slice(G * 512, (G + 1) * 512)
            P.dma("sp", qn_g, d["qnT"][:, :, rows].rearrange("h p t -> p h t"), writes=["qn_g"])
            P.dma("sp", qr_g[0:64], d["qrT"][:, :, rows].rearrange("h p t -> p h t"), writes=["qr_g"])
            self.attn_core(P, G, 8,
                           lambda h, kb: [(knT[:, h, kb * 128:(kb + 1) * 128], qn_g[:, h, :]),
                                          (krT[:, kb * 128:(kb + 1) * 128], qr_g[:, h, :])],
                           lambda kb, G=G: (cm[:, kb - 4 * G, :] if kb >= 4 * G else None),
                           lambda h, kb: vb[:, kb, h, :],
                           b_scale, pT, o_tm, identb, rs, ["knT", "krT", "qn_g", "qr_g", "cm", "vb", "identb"])
            self.store_mixT(P, G, o_tm, 8, 4, stage, identb)
        P.barrier()

    def phase_sb(self, P, A, l):
        d = self.d
        ps = self.ps
        A.off = self.base_off
        identb = A.take(128, BF16)
        kcT = A.take(4 * S, BF16).rearrange("p (h t) -> p h t", h=4)
        vc = A.take(32 * 512, BF16).rearrange("p (k c) -> p k c", k=32)
        qc_g = A.take(4 * 512, BF16).rearrange("p (h t) -> p h t", h=4)
        tri = A.take(4 * 512).rearrange("p (j t) -> p j t", j=4)
        trib = A.take(4 * 512, BF16).rearrange("p (j t) -> p j t", j=4)
        ustr = A.take(128)
        ones = A.take(128)
        eb = [A.take(512) for _ in range(3)]
        spb = [A.take(512) for _ in range(3)]
        t1b = [A.take(512) for _ in range(3)]
        acc = A.take(512)
        wT = [A.take(512, BF16) for _ in range(3)]
        o_tm = A.take(4 * 512, BF16).rearrange("p (s c) -> p s c", s=4)
        stage = A.take(4 * 512, BF16).rearrange("p (c t) -> p c t", c=4)
        P.op("dve", lambda e: e.tensor_copy(out=identb, in_=self.ident), reads=["ident"], writes=["identb"])
        P.dma("sp", kcT, d["kcT"].rearrange("h p t -> p h t"), writes=["kcT"])
        P.dma("sp", vc, d["vc"].rearrange("(k p) c -> p k c", p=128), writes=["vc"])
        P.dma("sp", tri, d["tri"].rearrange("p (j t) -> p j t", j=4), writes=["tri"])
        P.dma("sp", ustr, d["ustrict"], writes=["ustr"])
        P.op("dve", lambda e: e.tensor_copy(out=trib, in_=tri), reads=["tri"], writes=["trib"])
        P.op("dve", lambda e: e.memset(ones, 1.0), writes=["ones"])
        c_scale = 128.0 ** -0.5
        gi = 0
        for G in range(self.cfg.get("ng", NG)):
            rows = slice(G * 512, (G + 1) * 512)
            P.dma("sp", qc_g, d["qcT"][:, :, rows].rearrange("h p t -> p h t"), writes=["qc_g"])
            seq = [(h, kb) for h in range(4) for kb in range(4 * G + 3, -1, -1)]
            n = len(seq)

            def bufs(idx):
                g3 = (gi + idx) % 3
                g2 = (gi + idx) % 2
                return g3, g2

            def stageA(idx):
                h, kb = seq[idx]
                g3, g2 = bufs(idx)
                pz = ps[:, g2 * 512:(g2 + 1) * 512]
                e_, sp_, t1_ = eb[g3], spb[g3], t1b[g3]
                j = kb - 4 * G
                P.op("pe", lambda e, pz=pz, h=h, kb=kb: e.matmul(pz, lhsT=kcT[:, h, kb * 128:(kb + 1) * 128], rhs=qc_g[:, h, :], start=True, stop=True),
                     reads=["kcT", "qc_g"], writes=[("psb", g2)])
                P.op("act", lambda e, e_=e_, pz=pz: e.activation(out=e_, in_=pz, func=AF.Exp, scale=c_scale),
                     reads=[("psb", g2)], writes=[("eb", g3), ("psb", g2)])
                P.op("act", lambda e, e_=e_, sp_=sp_: e.activation(out=sp_, in_=e_, func=AF.Ln, bias=1.0),
                     reads=[("eb", g3)], writes=[("sp", g3)])
                P.op("dve", lambda e, t1_=t1_, pz=pz, sp_=sp_: e.scalar_tensor_tensor(out=t1_, in0=pz, scalar=c_scale, in1=sp_, op0=ALU.mult, op1=ALU.subtract),
                     reads=[("psb", g2), ("sp", g3)], writes=[("t1", g3), ("psb", g2)])
                if j >= 0:
                    P.op("pool", lambda e, sp_=sp_, j=j: e.tensor_tensor(out=sp_, in0=sp_, in1=tri[:, j, :], op=ALU.mult),
                         reads=[("sp", g3), "tri"], writes=[("sp", g3)])

            def stageB(idx):
                h, kb = seq[idx]
                g3, g2 = bufs(idx)
                first = (kb == 4 * G + 3)
                pl = ps[:, (2 + g2) * 512:(3 + g2) * 512]
                sp_, t1_, w_ = spb[g3], t1b[g3], wT[g3]
                j = kb - 4 * G
                P.op("pe", lambda e, pl=pl, sp_=sp_, first=first: e.matmul(pl, lhsT=ustr, rhs=sp_, start=True, stop=first),
                     reads=["ustr", ("sp", g3)], writes=[("psb", 2 + g2)])
                if not first:
                    P.op("pe", lambda e, pl=pl: e.matmul(pl, lhsT=ones, rhs=acc, start=False, stop=True),
                         reads=["ones", "acc"], writes=[("psb", 2 + g2)])
                P.op("dve", lambda e, t1_=t1_, pl=pl: e.tensor_tensor(out=t1_, in0=t1_, in1=pl, op=ALU.subtract),
                     reads=[("t1", g3), ("psb", 2 + g2)], writes=[("t1", g3), ("psb", 2 + g2)])
                if first:
                    P.op("pool", lambda e, sp_=sp_: e.tensor_copy(out=acc, in_=sp_), reads=[("sp", g3)], writes=["acc"])
                else:
                    P.op("pool", lambda e, sp_=sp_: e.tensor_tensor(out=acc, in0=acc, in1=sp_, op=ALU.add), reads=[("sp", g3), "acc"], writes=["acc"])
                P.op("act", lambda e, w_=w_, t1_=t1_: e.activation(out=w_, in_=t1_, func=AF.Exp), reads=[("t1", g3)], writes=[("wT", g3)])
                if j >= 0:
                    P.op("pool", lambda e, w_=w_, j=j: e.tensor_tensor(out=w_, in0=w_, in1=trib[:, j, :], op=ALU.mult),
                         reads=[("wT", g3), "trib"], writes=[("wT", g3)])

            def stageC(idx):
                h, kb = seq[idx]
                g3, g2 = bufs(idx)
                w_ = wT[g3]
                for i in range(4):
                    if kb > 4 * G + i:
                        continue
                    P.op("pe", lambda e, i=i, w_=w_, kb=kb, h=h, G=G: e.matmul(
                        ps[:, (4 + i) * 512:(4 + i) * 512 + 128], lhsT=w_[:, i * 128:(i + 1) * 128], rhs=vc[:, kb, h * 128:(h + 1) * 128],
                        start=(kb == 4 * G + i), stop=(kb == 0)), reads=[("wT", g3), "vc"], writes=[("psb", 4 + i)])
                if kb == 0:
                    for i in range(4):
                        P.op("act", lambda e, i=i, h=h: e.copy(out=o_tm[:, i, h * 128:(h + 1) * 128], in_=ps[:, (4 + i) * 512:(4 + i) * 512 + 128]),
                             reads=[("psb", 4 + i)], writes=["o_tm", ("psb", 4 + i)])

            for it in range(n + 2):
                if it < n:
                    stageA(it)
                if 0 <= it - 1 < n:
                    stageB(it - 1)
                if 0 <= it - 2 < n:
                    stageC(it - 2)
            gi += n
            self.store_mixT(P, G, o_tm, 4, 12, stage, identb)
        P.barrier()

    def phase_outproj(self, P, A, l, src):
        d = self.d
        ps = self.ps
        A.off = self.base_off
        _, _, gb = self.sublayer_setup(P, A, l, 1, 1.0)
        wo = A.take(16 * D, BF16).rearrange("p (k n) -> p k n", k=16)
        mx = [A.take(16 * 512, BF16).rearrange("p (k t) -> p k t", k=16) for _ in range(2)]
        xr = [A.take(512) for _ in range(8)]
        y = d["y"]
        P.dma("sp", wo, d["wout_b"][l].rearrange("p (k n) -> p k n", k=16), writes=["wo"])
        pi = 0
        for G in range(self.cfg.get("ng", NG)):
            rows = slice(G * 512, (G + 1) * 512)
            m_ = mx[G % 2]
            P.dma("sp", m_, d["mixT"][:, :, rows].rearrange("c p t -> p c t"), writes=[("mx", G % 2)])
            for s in range(4):
                t = G * 4 + s
                for n in range(4):
                    pb = pi % 8
                    xp = xr[pi % 8]
                    kxp = ("xrb", pi % 8)
                    pi += 1
                    P.dma("pool", xp, src[t * 128:(t + 1) * 128, n * 512:(n + 1) * 512], reads=[("xr", t, n)], writes=[kxp])
                    pd = ps[:, pb * 512:(pb + 1) * 512]
                    for k in range(16):
                        P.op("pe", lambda e, pd=pd, m_=m_, k=k, s=s, n=n: e.matmul(pd, lhsT=m_[:, k, s * 128:(s + 1) * 128], rhs=wo[:, k, n * 512:(n + 1) * 512],
                                                                                    start=(k == 0), stop=(k == 15)),
                             reads=[("mx", G % 2), "wo"], writes=[("psb", pb)])
                    P.op("dve", lambda e, pd=pd, n=n: e.tensor_tensor(out=pd, in0=pd, in1=gb[:, n * 512:(n + 1) * 512], op=ALU.mult),
                         reads=[("psb", pb), "gb"], writes=[("psb", pb)])
                    P.op("dve", lambda e, pd=pd, xp=xp: e.tensor_tensor(out=xp, in0=pd, in1=xp, op=ALU.add),
                         reads=[("psb", pb), kxp], writes=[kxp, ("psb", pb)])
                    P.dma("pool", y[t * 128:(t + 1) * 128, n * 512:(n + 1) * 512], xp, reads=[kxp], writes=[("xr", t, n)])
        P.barrier()

    def phase_ffn(self, P, A, l, f, src):
        d = self.d
        ps = self.ps
        A.off = self.base_off
        sub = 0 if f == 0 else 2
        shc, gsc, gb = self.sublayer_setup(P, A, l, sub, 0.5)
        xb = [A.take(D) for _ in range(4)]
        xr = [A.take(512) for _ in range(8)]
        junk = A.take(D, BF16)
        st = [A.take(4) for _ in range(2)]
        hT = A.take(16 * 512, BF16).rearrange("p (k t) -> p k t", k=16)
        actT = A.take(44 * 512, BF16).rearrange("p (c t) -> p c t", c=44)
        wgb = [A.take(16 * 256, BF16).rearrange("p (k c) -> p k c", k=16) for _ in range(2)]
        wub = [A.take(16 * 256, BF16).rearrange("p (k c) -> p k c", k=16) for _ in range(2)]
        wdb = [A.take(22 * 512, BF16).rearrange("p (c n) -> p c n", c=22) for _ in range(2)]
        sg = [A.take(512) for _ in range(2)]
        wt0 = (l * 2 + f) * 22
        wd0 = (l * 2 + f) * 8
        hTk = [("hT", s) for s in range(4)]
        y = d["y"]
        pi = 0
        ng = self.cfg.get("ng", NG)
        stop = self.cfg.get("ffn_stop", 9)
        if stop <= 1:
            P.barrier()
            return
        self.norm_loads(P, src, 0, xb)
        self.norm_group(P, src, 0, hT, shc, gsc, xb, junk, st)
        if stop <= 2:
            P.barrier()
            return
        for g in range(ng):
            for jb in range(22):
                b = jb % 2
                P.dma("sp", wgb[b], d["wg_b"][wt0 + jb].rearrange("p (k c) -> p k c", k=16), writes=[("wg", b)])
                P.dma("sp", wub[b], d["wu_b"][wt0 + jb].rearrange("p (k c) -> p k c", k=16), writes=[("wu", b)])
                for cc in range(2):
                    c = jb * 2 + cc
                    pb = c % 2
                    pg = ps[:, (0 + pb) * 512:(1 + pb) * 512]
                    pu = ps[:, (2 + pb) * 512:(3 + pb) * 512]
                    for k in range(16):
                        P.op("pe", lambda e, pg=pg, b=b, k=k, cc=cc: e.matmul(pg, lhsT=wgb[b][:, k, cc * 128:(cc + 1) * 128], rhs=hT[:, k, :],
                                                                              start=(k == 0), stop=(k == 15)),
                             reads=[("wg", b)] + hTk, writes=[("psb", pb)])
                    for k in range(16):
                        P.op("pe", lambda e, pu=pu, b=b, k=k, cc=cc: e.matmul(pu, lhsT=wub[b][:, k, cc * 128:(cc + 1) * 128], rhs=hT[:, k, :],
                                                                              start=(k == 0), stop=(k == 15)),
                             reads=[("wu", b)] + hTk, writes=[("psb", 2 + pb)])
                    sgt = sg[c % 2]
                    P.op("act", lambda e, sgt=sgt, pg=pg: e.activation(out=sgt, in_=pg, func=AF.Silu),
                         reads=[("psb", pb)], writes=[("sg", c % 2), ("psb", pb)])
                    P.op("dve", lambda e, sgt=sgt, pu=pu, c=c: e.tensor_tensor(out=actT[:, c, :], in0=pu, in1=sgt, op=ALU.mult),
                         reads=[("psb", 2 + pb), ("sg", c % 2)], writes=[("actT", c), ("psb", 2 + pb)])
            if stop <= 3:
                continue
            if g + 1 < ng:
                self.norm_loads(P, src, g + 1, xb)
            for n in range(4):
                if n == 2 and g + 1 < ng:
                    self.norm_group(P, src, g + 1, hT, shc, gsc, xb, junk, st)
                xps = []
                for s in range(4):
                    t = g * 4 + s
                    xp = xr[pi % 8]
                    kxp = ("xrb", pi % 8)
                    pi += 1
                    xps.append((xp, kxp))
                    P.dma("pool", xp, src[t * 128:(t + 1) * 128, n * 512:(n + 1) * 512], reads=[("xr", t, n)], writes=[kxp])
                for hf in range(2):
                    wi = n * 2 + hf
                    b = wi % 2
                    P.dma("sp", wdb[b], d["wd_b"][wd0 + wi].rearrange("p (c n) -> p c n", c=22), writes=[("wd", b)])
                    for s in range(4):
                        pd = ps[:, (4 + s) * 512:(5 + s) * 512]
                        for c in range(22):
                            ca = hf * 22 + c
                            P.op("pe", lambda e, pd=pd, b=b, c=c, ca=ca, s=s, hf=hf: e.matmul(
                                pd, lhsT=actT[:, ca, s * 128:(s + 1) * 128], rhs=wdb[b][:, c, :],
                                start=(hf == 0 and c == 0), stop=(hf == 1 and c == 21)),
                                 reads=[("wd", b), ("actT", ca)], writes=[("psb", 4 + s)])
                for s in range(4):
                    t = g * 4 + s
                    pd = ps[:, (4 + s) * 512:(5 + s) * 512]
                    xp, kxp = xps[s]
                    P.op("dve", lambda e, pd=pd, n=n: e.tensor_tensor(out=pd, in0=pd, in1=gb[:, n * 512:(n + 1) * 512], op=ALU.mult),
                         reads=[("psb", 4 + s), "gb"], writes=[("psb", 4 + s)])
                    P.op("dve", lambda e, pd=pd, xp=xp: e.tensor_tensor(out=xp, in0=pd, in1=xp, op=ALU.add),
                         reads=[("psb", 4 + s), kxp], writes=[kxp, ("psb", 4 + s)])
                    P.dma("pool", y[t * 128:(t + 1) * 128, n * 512:(n + 1) * 512], xp, reads=[kxp], writes=[("xr", t, n)])
        P.barrier()

    def build(self):
        nc = self.nc
        self.declare()
        d = self.d
        with ExitStack() as es:
            NB = 211968
            big = es.enter_context(nc.sbuf_tensor("big", [128, NB // 4], F32))
            self.ps = es.enter_context(nc.psum_tensor("ps", [128, 4096], F32))
            A = Alloc(big, NB)
            self.A = A
            self._alloc_offsets = A.registry
            P = Prog(nc)
            self.ident = A.take(128)
            self.gcol = A.take(NL * 3 * 16)
            P.dma("sp", self.ident, d["ident"], writes=["ident"])
            P.dma("sp", self.gcol, d["gcol"], writes=["gcol"])
            self.base_off = A.off
            P.barrier()
            if self.cfg.get("do_adaln", True):
                self.phase_adaln(P, A)
            if self.cfg.get("do_prep", True):
                self.phase_prep(P, A)
            src = d["x"]
            for l in range(self.cfg["nl"] if self.cfg.get("do_ffn", True) else 0):
                if self.cfg["ffn"][0]:
                    self.phase_ffn(P, A, l, 0, src)
                    src = d["y"]
                if self.cfg.get("mix", False):
                    mp = self.cfg.get("mixparts", "pabco")
                    if "p" in mp:
                        self.phase_mixproj(P, A, l, src)
                    if "a" in mp:
                        self.phase_dsa(P, A, l)
                    if "b" in mp:
                        self.phase_mla(P, A, l)
                    if "c" in mp:
                        self.phase_sb(P, A, l)
                    if "o" in mp:
                        self.phase_outproj(P, A, l, src)
                        src = d["y"]
                if self.cfg["ffn"][1]:
                    self.phase_ffn(P, A, l, 1, src)
                    src = d["y"]
            P.barrier()
            P.emit(es)
            self.P = P
        return nc


def host_layout(inp):
    sh = {}
    w_ada = np.asarray(inp["w_ada"], dtype=np.float32)
    sh["wada"] = np.ascontiguousarray(
        w_ada.reshape(NL, 16, 128, 36, 512).transpose(0, 3, 2, 1, 4)).reshape(NL * 36, 128, 16 * 512)
    sh["bada"] = np.ascontiguousarray(np.asarray(inp["b_ada"], dtype=np.float32))
    g = np.stack([np.asarray(inp[k], dtype=np.float32) for k in ("g_ffn1", "g_mix", "g_ffn2")], axis=1)
    sh["gcol"] = np.ascontiguousarray(g.reshape(NL, 3, 16, 128).transpose(3, 0, 1, 2)).reshape(128, NL * 3 * 16)

    def gu(a, b):
        w = np.stack([np.asarray(inp[a], dtype=np.float32), np.asarray(inp[b], dtype=np.float32)], axis=1)
        w = w.reshape(NL, 2, 16, 128, 22, 256).transpose(0, 1, 4, 3, 2, 5)
        return np.ascontiguousarray(w).reshape(NL * 2 * 22, 128, 16 * 256)

    sh["wg"] = gu("w1_gate", "w2_gate")
    sh["wu"] = gu("w1_up", "w2_up")
    w = np.stack([np.asarray(inp["w1_down"], dtype=np.float32), np.asarray(inp["w2_down"], dtype=np.float32)], axis=1)
    w = w.reshape(NL, 2, 2, 22, 128, 4, 512).transpose(0, 1, 5, 2, 4, 3, 6)
    sh["wd"] = np.ascontiguousarray(w).reshape(NL * 2 * 8, 128, 22 * 512)
    sh["ident"] = np.eye(128, dtype=np.float32)
    w_in = np.asarray(inp["w_in"], dtype=np.float32)
    sp = np.cumsum([0, 512, 512, 512, 1024, 64, 16, 448, 128, 64, 512, 512, 512])
    qa, ka, va, qi, ki, wi, cq, ckv, kr, qc, kc, vc = [np.arange(sp[i], sp[i + 1]) for i in range(12)]
    tm = np.concatenate([qa, ka, va, vc, cq, kr, ckv, wi])
    fm = np.concatenate([qi, ki, ki, qc, kc])
    sh["wtm"] = np.ascontiguousarray(w_in[:, :, tm].reshape(NL, 16, 128, TM_COLS).transpose(0, 2, 1, 3)).reshape(NL, 128, 16 * TM_COLS)
    sh["wfm"] = np.ascontiguousarray(w_in[:, :, fm].reshape(NL, 16, 128, FM_COLS).transpose(0, 2, 1, 3)).reshape(NL, 128, 16 * FM_COLS)
    wuq = np.zeros((NL, 512, 1536), np.float32)
    wuq[:, :448] = np.asarray(inp["w_uq"], dtype=np.float32)
    sh["wuq"] = np.ascontiguousarray(wuq.reshape(NL, 4, 128, 1536).transpose(0, 2, 1, 3)).reshape(NL, 128, 4 * 1536)
    sh["wukv"] = np.ascontiguousarray(np.asarray(inp["w_ukv"], dtype=np.float32))
    sh["wout"] = np.ascontiguousarray(np.asarray(inp["w_out"], dtype=np.float32).reshape(NL, 16, 128, D).transpose(0, 2, 1, 3)).reshape(NL, 128, 16 * D)
    gt = np.concatenate([np.asarray(inp[k], dtype=np.float32) for k in
                         ("g_qa", "g_ka", "g_cq", "g_ckv", "g_q_nope", "g_k_nope", "g_q_rope", "g_k_rope")], axis=1)
    sh["gtm"] = np.ascontiguousarray(np.broadcast_to(gt.reshape(1, NL * GT), (128, NL * GT)))
    def tables(dim):
        inv = (1.0 / (np.float32(500000.0) ** (np.arange(0, dim, 2, dtype=np.float32) / np.float32(dim)))).astype(np.float32)
        ang = (np.arange(S, dtype=np.float32)[:, None] * inv[None, :]).astype(np.float32)
        return np.cos(ang).astype(np.float32), np.sin(ang).astype(np.float32)
    ca, sa = tables(32)
    cm_, sm_ = tables(64)
    sh["ropeA"] = np.ascontiguousarray(np.concatenate([np.tile(ca, (1, 4)), np.tile(sa, (1, 4))], axis=1))
    sh["ropeQ"] = np.ascontiguousarray(np.concatenate([np.tile(cm_, (1, 8)), np.tile(sm_, (1, 8))], axis=1))
    sh["ropeK"] = np.ascontiguousarray(np.concatenate([cm_, sm_], axis=1))
    cmask = np.zeros((128, 4, 4, 128), np.float32)
    tri = np.zeros((128, 4, 4, 128), np.float32)
    kk = np.arange(128)[:, None]
    qq = np.arange(128)[None, :]
    for j in range(4):
        for i in range(4):
            if i < j:
                cmask[:, j, i, :] = BIGNEG
            elif i == j:
                cmask[:, j, i, :] = np.where((kk // 64) <= (qq // 64), 0.0, BIGNEG)
                tri[:, j, i, :] = (kk < qq)
            else:
                tri[:, j, i, :] = 1.0
    sh["cmaskB"] = cmask.reshape(128, 2048)
    sh["tri"] = tri.reshape(128, 2048)
    sh["ustrict"] = (np.arange(128)[:, None] > np.arange(128)[None, :]).astype(np.float32)
    sh["pow2"] = np.ascontiguousarray(np.broadcast_to((0.5 ** np.arange(1, NBIS + 2)).astype(np.float32)[None, :], (128, NBIS + 1)))
    return sh


def core_inputs(inp, sh, b):
    m = dict(sh)
    m["x"] = np.ascontiguousarray(np.asarray(inp["x"][b], dtype=np.float32))
    m["cT"] = np.ascontiguousarray(np.asarray(inp["c"][b], dtype=np.float32).reshape(16, 128).T)
    return m


DEFAULT_CFG = {"nl": 2, "ffn": (True, True), "mix": True}


def kernel(**inputs):
    nc = bass.Bass("TRN2", target_bir_lowering=False)
    bld = Builder(nc, DEFAULT_CFG)
    bld.build()
    sh = host_layout(inputs)
    in_maps = [core_inputs(inputs, sh, b) for b in range(8)]
    res = run_bass_kernel_spmd(nc, in_maps, core_ids=list(range(8)))
    return np.stack([np.asarray(r["y"], dtype=np.float32) for r in res.results], axis=0)
```
